# Optimizing a Trainium2 kernel written in Bass

```python
import math
import jax, jax.numpy as jnp
from jax import lax
import numpy as np

D_MODEL = 2048
BATCH = 2
SEQ = 4096
DEPTH = 1

D_MIX = D_MODEL
D_ATTN = D_MIX // 2
D_CONV = D_MIX - D_ATTN
HEAD_DIM = 128
N_Q_HEADS = D_ATTN // HEAD_DIM
N_KV_HEADS = 2
GQA = N_Q_HEADS // N_KV_HEADS
D_KV = N_KV_HEADS * HEAD_DIM
L_CMP = 32
STRIDE_CMP = 16
L_SEL = 64
N_SEL = 16
WINDOW = 512
Q_BLOCK = 128
FORCED_SCORE = float(GQA + 1)
CONV_WIDTH = 3
CONV_GROUPS = 8
NUM_BUCKETS = 32
MAX_DISTANCE = 128
PEER_HEADS = 8
N_KEYS = 128
N_EXPERTS = N_KEYS * N_KEYS
PEER_TOPK = 16
D_KEY = 256
D_KEY_HALF = D_KEY // 2
TOK_BLOCK = 128
EPS = 1e-6
IN_SPLITS = (D_ATTN, D_KV, D_KV, D_KV, D_KV, D_KV, D_KV, 3 * N_Q_HEADS, D_CONV, D_CONV, D_CONV)
D_IN_PROJ = sum(IN_SPLITS)

kernel_name = 'hymba_nsa_shortconv_peer_layer'


def rmsnorm(x, w):
    xf = x.astype(jnp.float32)
    y = xf * lax.rsqrt(jnp.mean(xf * xf, axis=-1, keepdims=True) + EPS)
    return y.astype(x.dtype) * w


def group_rmsnorm(x, w, n_groups):
    shp = x.shape
    xg = x.reshape(shp[:-1] + (n_groups, shp[-1] // n_groups)).astype(jnp.float32)
    y = xg * lax.rsqrt(jnp.mean(xg * xg, axis=-1, keepdims=True) + EPS)
    return y.reshape(shp).astype(x.dtype) * w


def masked_softmax(s, mask):
    s = jnp.where(mask, s, -jnp.inf)
    m = jnp.max(s, axis=-1, keepdims=True)
    m = jnp.where(jnp.isfinite(m), m, 0.0)
    e = jnp.exp(s - m)
    d = jnp.sum(e, axis=-1, keepdims=True)
    return e / jnp.where(d > 0, d, 1.0)


def t5_bucket(dist):
    n = jnp.maximum(dist, 0)
    max_exact = NUM_BUCKETS // 2
    nf = jnp.maximum(n, 1).astype(jnp.float32)
    large = max_exact + (jnp.log(nf / max_exact) / math.log(MAX_DISTANCE / max_exact)
                         * (NUM_BUCKETS - max_exact)).astype(jnp.int32)
    large = jnp.minimum(large, NUM_BUCKETS - 1)
    return jnp.where(n < max_exact, n, large)


def selection_overlap(S):
    n_c = (S - L_CMP) // STRIDE_CMP + 1
    n_sel = S // L_SEL
    pos = np.arange(n_c)[:, None] * STRIDE_CMP + np.arange(L_CMP)[None, :]
    m = np.zeros((n_c, n_sel), np.float32)
    np.add.at(m, (np.repeat(np.arange(n_c), L_CMP), (pos // L_SEL).reshape(-1)), 1.0 / L_CMP)
    return jnp.asarray(m)


def nsa_mixer(q, k_cmp_in, v_cmp_in, k_sel, v_sel, k_win, v_win, gate_logits,
              w_cmp_k, w_cmp_v, cmp_pos, rel_bias):
    B, S = q.shape[:2]
    scale = HEAD_DIM ** -0.5
    q = q.reshape(B, S, N_KV_HEADS, GQA, HEAD_DIM)
    kvs = lambda a: a.reshape(B, S, N_KV_HEADS, HEAD_DIM)
    k_cmp_in, v_cmp_in, k_sel, v_sel, k_win, v_win = map(kvs, (k_cmp_in, v_cmp_in, k_sel, v_sel, k_win, v_win))
    t = np.arange(S)

    n_c = (S - L_CMP) // STRIDE_CMP + 1
    blk_pos = np.arange(n_c)[:, None] * STRIDE_CMP + np.arange(L_CMP)[None, :]

    def compress(k, w):
        blocks = k[:, blk_pos] + cmp_pos[None, None, :, None, :]
        return jnp.einsum('bnlhd,lde->bnhe', blocks, w)

    kc = compress(k_cmp_in, w_cmp_k)
    vc = compress(v_cmp_in, w_cmp_v)
    dist_c = t[:, None] - blk_pos[:, -1][None, :]
    bias_c = rel_bias[t5_bucket(dist_c)].reshape(S, n_c, N_KV_HEADS, GQA).transpose(2, 3, 0, 1)
    s_c = jnp.einsum('bqhgd,bnhd->bhgqn', q, kc).astype(jnp.float32) * scale + bias_c
    p_c = masked_softmax(s_c, dist_c >= 0)
    o_cmp = jnp.einsum('bhgqn,bnhd->bqhgd', p_c.astype(vc.dtype), vc)

    n_sel = S // L_SEL
    n_top = min(N_SEL, n_sel)
    imp = jnp.einsum('bhgqn,nj->bhqj', p_c, selection_overlap(S))
    j = np.arange(n_sel)[None, :]
    blk_t = (t // L_SEL)[:, None]
    forced = (j == 0) | (j == blk_t) | (j == blk_t - 1)
    causal_blk = j * L_SEL <= t[:, None]
    score = jnp.where(forced, FORCED_SCORE, jnp.where(causal_blk, imp, -1.0))
    _, sel_idx = lax.top_k(score, n_top)

    n_qb = S // Q_BLOCK
    q_ch = q.reshape(B, n_qb, Q_BLOCK, N_KV_HEADS, GQA, HEAD_DIM).transpose(1, 0, 2, 3, 4, 5)
    idx_ch = sel_idx.reshape(B, N_KV_HEADS, n_qb, Q_BLOCK, n_top).transpose(2, 0, 1, 3, 4)
    ks_blk = k_sel.reshape(B, n_sel, L_SEL, N_KV_HEADS, HEAD_DIM).transpose(0, 3, 1, 2, 4)
    vs_blk = v_sel.reshape(B, n_sel, L_SEL, N_KV_HEADS, HEAD_DIM).transpose(0, 3, 1, 2, 4)
    pad = ((0, 0), (WINDOW, 0), (0, 0), (0, 0))
    kw_pad = jnp.pad(k_win, pad)
    vw_pad = jnp.pad(v_win, pad)
    rel_bias_g = rel_bias.reshape(NUM_BUCKETS, N_KV_HEADS, GQA)
    gather = jax.vmap(jax.vmap(lambda blocks, ids: blocks[ids]))
    h_ar = jnp.arange(N_KV_HEADS)[None, :, None, None]
    win_off = jnp.arange(WINDOW + Q_BLOCK) - WINDOW
    n_k = n_top * L_SEL

    def block_fn(args):
        c, qc, ic = args
        t_q = c * Q_BLOCK + jnp.arange(Q_BLOCK)
        kg = gather(ks_blk, ic).reshape(B, N_KV_HEADS, Q_BLOCK, n_k, HEAD_DIM)
        vg = gather(vs_blk, ic).reshape(B, N_KV_HEADS, Q_BLOCK, n_k, HEAD_DIM)
        pos = (ic[..., None] * L_SEL + jnp.arange(L_SEL)).reshape(B, N_KV_HEADS, Q_BLOCK, n_k)
        dist = t_q[None, None, :, None] - pos
        bias = rel_bias_g[t5_bucket(dist), h_ar].transpose(0, 1, 4, 2, 3)
        s = jnp.einsum('bqhgd,bhqkd->bhgqk', qc, kg).astype(jnp.float32) * scale + bias
        p = masked_softmax(s, (dist >= 0)[:, :, None])
        o_s = jnp.einsum('bhgqk,bhqkd->bqhgd', p.astype(vg.dtype), vg)
        kw = lax.dynamic_slice_in_dim(kw_pad, c * Q_BLOCK, WINDOW + Q_BLOCK, axis=1)
        vw = lax.dynamic_slice_in_dim(vw_pad, c * Q_BLOCK, WINDOW + Q_BLOCK, axis=1)
        pos_w = c * Q_BLOCK + win_off
        dist_w = t_q[:, None] - pos_w[None, :]
        valid_w = (dist_w >= 0) & (dist_w < WINDOW) & (pos_w >= 0)[None, :]
        bias_w = rel_bias[t5_bucket(dist_w)].reshape(Q_BLOCK, WINDOW + Q_BLOCK, N_KV_HEADS, GQA).transpose(2, 3, 0, 1)
        s_w = jnp.einsum('bqhgd,bkhd->bhgqk', qc, kw).astype(jnp.float32) * scale + bias_w
        p_w = masked_softmax(s_w, valid_w)
        o_w = jnp.einsum('bhgqk,bkhd->bqhgd', p_w.astype(vw.dtype), vw)
        return o_s, o_w

    o_sel, o_win = lax.map(block_fn, (jnp.arange(n_qb), q_ch, idx_ch))
    unblock = lambda o: o.transpose(1, 0, 2, 3, 4, 5).reshape(B, S, N_KV_HEADS, GQA, HEAD_DIM)
    o_sel, o_win = unblock(o_sel), unblock(o_win)

    g = jax.nn.sigmoid(gate_logits.astype(jnp.float32)).reshape(B, S, N_KV_HEADS, GQA, 3).astype(q.dtype)
    o = g[..., 0:1] * o_cmp + g[..., 1:2] * o_sel + g[..., 2:3] * o_win
    return o.reshape(B, S, D_ATTN)


def short_conv_mixer(b_gate, c_gate, h, conv_w, conv_b):
    u = c_gate * h
    y = lax.conv_general_dilated(u, conv_w[:, None, :].astype(u.dtype), window_strides=(1,),
                                 padding=[(CONV_WIDTH - 1, 0)],
                                 dimension_numbers=('NWC', 'WIO', 'NWC'),
                                 feature_group_count=D_CONV) + conv_b
    return b_gate * y


def peer_ffn(x, wq, subkeys, u_tab, v_tab):
    B, S, D = x.shape
    T = B * S
    xt = x.reshape(T, D)
    q = (xt @ wq).reshape(T, PEER_HEADS, 2, D_KEY_HALF)
    s = jnp.einsum('thcd,hcnd->thcn', q, subkeys).astype(jnp.float32)
    sv, si = lax.top_k(s, PEER_TOPK)
    cand = (sv[:, :, 0, :, None] + sv[:, :, 1, None, :]).reshape(T, PEER_HEADS, PEER_TOPK * PEER_TOPK)
    cv, ci = lax.top_k(cand, PEER_TOPK)
    i1 = jnp.take_along_axis(si[:, :, 0], ci // PEER_TOPK, axis=-1)
    i2 = jnp.take_along_axis(si[:, :, 1], ci % PEER_TOPK, axis=-1)
    experts = i1 * N_KEYS + i2
    gates = jax.nn.softmax(cv, axis=-1)
    n_tb = T // TOK_BLOCK

    def block_fn(args):
        xc, ec, gc = args
        u = u_tab[ec]
        a = jax.nn.gelu(jnp.einsum('thkd,td->thk', u, xc).astype(jnp.float32))
        w = (gc * a).astype(xc.dtype)
        return jnp.einsum('thk,thkd->td', w, v_tab[ec])

    out = lax.map(block_fn, (xt.reshape(n_tb, TOK_BLOCK, D),
                             experts.reshape(n_tb, TOK_BLOCK, PEER_HEADS, PEER_TOPK),
                             gates.reshape(n_tb, TOK_BLOCK, PEER_HEADS, PEER_TOPK)))
    return out.reshape(B, S, D)


def setup_inputs(seed: int = 0) -> dict:
    key = jax.random.key(seed)
    ks = jax.random.split(key, 20)
    nrm = lambda k, shp, sc: jax.random.normal(k, shp, jnp.float32) * sc
    return {
        'x': nrm(ks[0], (BATCH, SEQ, D_MODEL), 1.0),
        'attn_norm_w': 1.0 + nrm(ks[1], (DEPTH, D_MODEL), 0.02),
        'w_in': nrm(ks[2], (DEPTH, D_MODEL, D_IN_PROJ), D_MODEL ** -0.5),
        'w_cmp_k': nrm(ks[3], (DEPTH, L_CMP, HEAD_DIM, HEAD_DIM), (L_CMP * HEAD_DIM) ** -0.5),
        'w_cmp_v': nrm(ks[4], (DEPTH, L_CMP, HEAD_DIM, HEAD_DIM), (L_CMP * HEAD_DIM) ** -0.5),
        'cmp_pos': nrm(ks[5], (DEPTH, L_CMP, HEAD_DIM), 0.1),
        'conv_w': nrm(ks[6], (DEPTH, CONV_WIDTH, D_CONV), CONV_WIDTH ** -0.5),
        'conv_b': nrm(ks[7], (DEPTH, D_CONV), 0.02),
        'attn_group_norm_w': 1.0 + nrm(ks[8], (DEPTH, D_ATTN), 0.02),
        'conv_group_norm_w': 1.0 + nrm(ks[9], (DEPTH, D_CONV), 0.02),
        'w_out': nrm(ks[10], (DEPTH, D_MIX, D_MODEL), D_MIX ** -0.5),
        'rel_bias': nrm(ks[11], (NUM_BUCKETS, N_Q_HEADS), 0.2),
        'ffn_norm_w': 1.0 + nrm(ks[12], (DEPTH, D_MODEL), 0.02),
        'peer_wq': nrm(ks[13], (DEPTH, D_MODEL, PEER_HEADS * D_KEY), D_MODEL ** -0.5),
        'peer_subkeys': nrm(ks[14], (DEPTH, PEER_HEADS, 2, N_KEYS, D_KEY_HALF), D_KEY_HALF ** -0.5),
        'peer_u': nrm(ks[15], (DEPTH, N_EXPERTS, D_MODEL), D_MODEL ** -0.5),
        'peer_v': nrm(ks[16], (DEPTH, N_EXPERTS, D_MODEL), (PEER_HEADS * PEER_TOPK) ** -0.5),
        'final_norm_w': 1.0 + nrm(ks[17], (D_MODEL,), 0.02),
    }


def reference(x, attn_norm_w, w_in, w_cmp_k, w_cmp_v, cmp_pos, conv_w, conv_b,
              attn_group_norm_w, conv_group_norm_w, w_out, rel_bias, ffn_norm_w,
              peer_wq, peer_subkeys, peer_u, peer_v, final_norm_w):
    split_at = list(np.cumsum(IN_SPLITS)[:-1])
    h = x
    for l in range(DEPTH):
        xn = rmsnorm(h, attn_norm_w[l])
        proj = xn @ w_in[l]
        (q, k_c, v_c, k_s, v_s, k_w, v_w, gate_logits,
         b_gate, c_gate, h_conv) = jnp.split(proj, split_at, axis=-1)
        o_attn = nsa_mixer(q, k_c, v_c, k_s, v_s, k_w, v_w, gate_logits,
                           w_cmp_k[l], w_cmp_v[l], cmp_pos[l], rel_bias)
        o_conv = short_conv_mixer(b_gate, c_gate, h_conv, conv_w[l], conv_b[l])
        mixed = jnp.concatenate([group_rmsnorm(o_attn, attn_group_norm_w[l], N_Q_HEADS),
                                 group_rmsnorm(o_conv, conv_group_norm_w[l], CONV_GROUPS)], axis=-1)
        h = h + mixed @ w_out[l]
        h = h + peer_ffn(rmsnorm(h, ffn_norm_w[l]), peer_wq[l], peer_subkeys[l], peer_u[l], peer_v[l])
    return rmsnorm(h, final_norm_w)
```

```python
import math
from contextlib import ExitStack
import numpy as np
import ml_dtypes
import concourse.bass as bass
import concourse.mybir as mybir
from concourse.bass_utils import run_bass_kernel_spmd

F32 = mybir.dt.float32
BF16 = mybir.dt.bfloat16
U32 = mybir.dt.uint32
ALU = mybir.AluOpType
AF = mybir.ActivationFunctionType
AX = mybir.AxisListType

D = 2048
S = 4096
NQ = 1024
NEG = -30000.0
EPS = 1e-6
SCALE = 128 ** -0.5
DIN = 5656
MSEG0, MSEG1, MSEG2 = 384, 256, 3072
MTOT = MSEG0 + MSEG1 + MSEG2
NDBG = []


class T:
    __slots__ = ("name", "w", "r", "dsem")

    def __init__(self, name):
        self.name = name
        self.w = {}
        self.r = {}
        self.dsem = None


class Prog:
    ENG = ("pe", "dve", "act", "pool", "sp")

    def __init__(self, nc, stack):
        self.nc = nc
        self.stack = stack
        self.stream = {k: [] for k in self.ENG}
        self.cnt = {}
        self.sems = {}
        self.known = {k: {} for k in self.ENG}
        for k in self.ENG:
            self.sems[k] = stack.enter_context(nc.semaphore("s_" + k))
            self.cnt[k] = 0
        self.ndsem = 0

    def _dsem(self, t):
        if t.dsem is None:
            key = "d%d" % self.ndsem
            self.ndsem += 1
            self.sems[key] = self.stack.enter_context(self.nc.semaphore(key))
            self.cnt[key] = 0
            t.dsem = key
        return t.dsem

    def _waits(self, eng, reads, writes, nowaw=False):
        need = {}

        def add(k, v):
            if v > need.get(k, 0):
                need[k] = v
        for t in reads:
            for k, v in t.w.items():
                add(k, v)
        for t in writes:
            if not nowaw:
                for k, v in t.w.items():
                    add(k, v)
            for k, v in t.r.items():
                add(k, v)
        kn = self.known[eng]
        out = []
        for k, v in need.items():
            if kn.get(k, 0) < v:
                kn[k] = v
                out.append((k, v))
        return out

    def op(self, eng, name, kw, reads=(), writes=()):
        waits = self._waits(eng, reads, writes)
        self.cnt[eng] += 1
        v = self.cnt[eng]
        self.stream[eng].append((waits, (name, kw), eng, 1))
        for t in reads:
            t.r[eng] = v
        for t in writes:
            t.w[eng] = v

    def dma(self, eng, kw, reads=(), writes=(), nowaw=False, name="dma_start"):
        assert len(writes) == 1
        t = writes[0]
        key = self._dsem(t)
        waits = self._waits(eng, reads, writes, nowaw=nowaw)
        self.cnt[key] += 16
        v = self.cnt[key]
        self.stream[eng].append((waits, (name, kw), key, 16))
        for s in reads:
            s.r[key] = v
        t.w[key] = v

    def barrier(self):
        snap = dict(self.cnt)
        for e in self.ENG:
            kn = self.known[e]
            waits = []
            for k, v in snap.items():
                if v > 0 and kn.get(k, 0) < v:
                    kn[k] = v
                    waits.append((k, v))
            self.stream[e].append((waits, None, None, 0))

    def final_wait(self, eng, tiles):
        waits = self._waits(eng, tiles, ())
        self.stream[eng].append((waits, None, None, 0))

    def emit(self):
        nc = self.nc
        with nc.Block() as block:
            def mk(name):
                def body(e):
                    for waits, fn, key, inc in self.stream[name]:
                        for k, v in waits:
                            e.wait_ge(self.sems[k], v)
                        if fn is not None:
                            getattr(e, fn[0])(**fn[1]).then_inc(self.sems[key], inc)
                return body
            block.tensor(mk("pe"))
            block.vector(mk("dve"))
            block.scalar(mk("act"))
            block.gpsimd(mk("pool"))
            block.sync(mk("sp"))


def _t5_bucket_np(n):
    n = np.maximum(n, 0)
    nf = np.maximum(n, 1).astype(np.float32)
    large = 16 + (np.log(nf / np.float32(16)) / np.float32(math.log(8.0)) * np.float32(16)).astype(np.int32)
    large = np.minimum(large, 31)
    return np.where(n < 16, n, large)


def _consts():
    oh = np.zeros((33, MTOT), np.float32)
    m = np.arange(MSEG0)
    d = m - 127
    bk = _t5_bucket_np(d)
    for i in range(MSEG0):
        if d[i] < 0:
            oh[32, i] = NEG
        else:
            oh[bk[i], i] = 1.0
    for i in range(MSEG1):
        if i - 127 < 0:
            oh[31, MSEG0 + i] = 1.0
        else:
            oh[32, MSEG0 + i] = NEG
    m = np.arange(MSEG2)
    d = m - 1023
    bk = _t5_bucket_np(d)
    base = MSEG0 + MSEG1
    for i in range(MSEG2):
        if d[i] < 0:
            oh[32, base + i] = NEG
        else:
            oh[bk[i], base + i] = 1.0
    ident = np.eye(128, dtype=np.float32)
    jmat = ident[::-1].copy()
    eexp = np.zeros((64, 4096), np.float32)
    eexp[np.arange(4096) // 64, np.arange(4096)] = 1.0
    ov = np.zeros((256, 64), np.float32)
    for n in range(255):
        pos = n * 16 + np.arange(32)
        for p in pos:
            ov[n + 1, p // 64] += 1.0 / 32
    ovs = ov.reshape(2, 128, 64).transpose(1, 0, 2).copy()
    iota16 = np.tile(np.arange(16, dtype=np.float32)[None, :], (128, 1))
    return dict(iota16=iota16, oh33=oh, ident=ident, jmat=jmat, identb=ident.astype(ml_dtypes.bfloat16),
                eexp=eexp.astype(ml_dtypes.bfloat16), ovs=ovs)


def _core_consts(c):
    q0 = 1024 * c
    ws = q0 - 3072
    kvalid = np.zeros((128, 32), np.float32)
    for s in range(32):
        if ws + 128 * s < 0:
            kvalid[:, s] = NEG
    cvalid = np.zeros((128, 2), np.float32)
    for tl in range(2):
        for p in range(128):
            n_rel = tl * 128 + p - 1
            n_abs = n_rel + ws // 16
            if n_rel < 0 or n_abs < 0:
                cvalid[p, tl] = NEG
    t_abs = q0 + np.arange(1024)
    j_abs = np.arange(64) + ws // 64
    exists = j_abs >= 0
    causal = exists[None, :] & (j_abs[None, :] * 64 <= t_abs[:, None])
    blk_t = t_abs // 64
    forced = exists[None, :] & ((j_abs[None, :] == 0) | (j_abs[None, :] == blk_t[:, None]) | (j_abs[None, :] == blk_t[:, None] - 1))
    cbnf = (causal & ~forced).astype(np.float32)
    sadd = np.where(forced, 5.0, np.where(causal, 0.0, -1.0)).astype(np.float32)
    cb = causal.astype(np.float32)
    f = lambda a: a.reshape(8, 128, 64).transpose(1, 0, 2).copy()
    return dict(kvalid=kvalid, cvalid=cvalid, cbnf=f(cbnf), sadd=f(sadd), cb=f(cb))


def build(stop_after=None, dbg=False):
    nc = bass.Bass("TRN2", target_bir_lowering=False)
    din = lambda n, shp, dt=F32: nc.dram_tensor(n, list(shp), dt, kind="ExternalInput").ap()
    xs = din("xs", [S, D])
    attn_norm_w = din("attn_norm_w", [D])
    w_in = din("w_in", [D, DIN])
    w_cmp_k = din("w_cmp_k", [32, 128, 128])
    w_cmp_v = din("w_cmp_v", [32, 128, 128])
    cmp_pos = din("cmp_pos", [32, 128])
    conv_w = din("conv_w", [3, 1024])
    conv_b = din("conv_b", [1024])
    agnw = din("attn_group_norm_w", [1024])
    cgnw = din("conv_group_norm_w", [1024])
    w_out = din("w_out", [D, D])
    rel_bias = din("rel_bias", [32, 8])
    ffn_norm_w = din("ffn_norm_w", [D])
    peer_wq = din("peer_wq", [D, D])
    peer_sk = din("peer_subkeys", [16, 128, 128])
    peer_u = din("peer_u", [16384, D])
    peer_v = din("peer_v", [16384, D])
    final_norm_w = din("final_norm_w", [D])
    oh33_d = din("oh33", [33, MTOT])
    ident_d = din("ident", [128, 128])
    jmat_d = din("jmat", [128, 128])
    identb_d = din("identb", [128, 128], BF16)
    eexp_d = din("eexp", [64, 4096], BF16)
    ovs_d = din("ovs", [128, 2, 64])
    kvalid_d = din("kvalid", [128, 32])
    cvalid_d = din("cvalid", [128, 2])
    cbnf_d = din("cbnf", [128, 8, 64])
    sadd_d = din("sadd", [128, 8, 64])
    cb_d = din("cb", [128, 8, 64])
    iota16_d = din("iota16", [128, 16])
    y_out = nc.dram_tensor("y", [NQ, D], F32, kind="ExternalOutput").ap()
    gtd = nc.dram_tensor("gtd", [8, MTOT], F32, kind="Internal").ap()
    uv16 = nc.dram_tensor("uv16", [16384, 2 * D], BF16, kind="Internal").ap()
    Tuv16 = T("uv16")
    dbg_outs = {}

    with ExitStack() as st:
        P = Prog(nc, st)
        ntile = [0]

        def sb(stk, shape, dt=F32, name=None):
            ntile[0] += 1
            nm = (name or "t") + "_%d" % ntile[0]
            return stk.enter_context(nc.sbuf_tensor(nm, list(shape), dt)), T(nm)

        def psb(stk, shape, dt=F32, name=None):
            ntile[0] += 1
            nm = (name or "p") + "_%d" % ntile[0]
            return stk.enter_context(nc.psum_tensor(nm, list(shape), dt)), T(nm)

        Tout = T("y_out")

        def dump(name, ap, t, shape, dt=F32):
            if not dbg:
                return
            o = nc.dram_tensor("dbg_" + name, list(shape), dt, kind="ExternalOutput").ap()
            to = T("dbg_" + name)
            dbg_outs[name] = to
            P.dma("sp", dict(out=o, in_=ap), reads=[t], writes=[to])

        def finish():
            P.final_wait("sp", [Tout] + list(dbg_outs.values()))
            P.emit()

        def OP(eng, name, reads, writes, **kw):
            P.op(eng, name, kw, reads, writes)

        def DMA(eng, out, in_, reads, writes, **kw):
            P.dma(eng, dict(out=out, in_=in_, **kw), reads, writes)

        evac_rr = [0]

        def evac(out_ap, in_ap, reads, writes):
            evac_rr[0] += 1
            if evac_rr[0] % 2:
                OP("act", "activation", reads, writes, out=out_ap, in_=in_ap, func=AF.Copy)
            else:
                OP("dve", "tensor_copy", reads, writes, out=out_ap, in_=in_ap)

        ident, Tident = sb(st, [128, 128], F32, "ident")
        identb, Tidentb = sb(st, [128, 128], BF16, "identb")
        jmat, Tjmat = sb(st, [128, 128], F32, "jmat")
        onesf, Tonesf = sb(st, [128, 128], F32, "onesf")
        gcol, Tgcol = sb(st, [128, 16], F32, "gcol")
        chcol, Tchcol = sb(st, [128, 8], F32, "chcol")
        kvalid, Tkvalid = sb(st, [128, 32], F32, "kvalid")
        cvalid, Tcvalid = sb(st, [128, 2], F32, "cvalid")
        ccol0, Tccol0 = sb(st, [128, 8], F32, "ccol0")
        cw, Tcw = sb(st, [128, 3, 8], F32, "cw")
        cbias, Tcbias = sb(st, [128, 8], F32, "cbias")
        agw, Tagw = sb(st, [128, 8], F32, "agw")
        cgw, Tcgw = sb(st, [128, 8], F32, "cgw")
        DMA("sp", ident[:], ident_d, [], [Tident])
        DMA("sp", identb[:], identb_d, [], [Tidentb])
        DMA("sp", jmat[:], jmat_d, [], [Tjmat])
        DMA("sp", kvalid[:], kvalid_d, [], [Tkvalid])
        DMA("sp", cvalid[:], cvalid_d, [], [Tcvalid])
        OP("dve", "memset", [], [Tonesf], ap=onesf[:], constant=1.0)
        NCD = dict(allow_slow_non_contiguous=True)
        DMA("sp", gcol[:], attn_norm_w.rearrange("(k p) -> p k", p=128), [], [Tgcol], **NCD)
        for wi in range(3):
            DMA("sp", cw[:, wi, :], conv_w[wi].rearrange("(c p) -> p c", p=128), [], [Tcw], **NCD)
        DMA("sp", cbias[:], conv_b.rearrange("(c p) -> p c", p=128), [], [Tcbias], **NCD)
        DMA("sp", agw[:], agnw.rearrange("(c p) -> p c", p=128), [], [Tagw], **NCD)
        DMA("sp", cgw[:], cgnw.rearrange("(c p) -> p c", p=128), [], [Tcgw], **NCD)
        DMA("sp", chcol[:], rel_bias[31:32, :].partition_broadcast(128), [], [Tchcol], **NCD)
        OP("dve", "tensor_scalar", [Tchcol, Tcvalid], [Tccol0], out=ccol0[:], in0=chcol[:], scalar1=cvalid[:, 0:1],
           scalar2=None, op0=ALU.add)

        mixT, TmixT = sb(st, [128, 16, NQ], BF16, "mixT")
        gsig, Tgsig = sb(st, [128, 8, 24], F32, "gsig")
        mixF = mixT[:].rearrange("p a b -> p (a b)").bitcast(F32)
        sA = ExitStack()
        ksT, TksT = sb(sA, [128, 2, S], BF16, "ksT")
        vs, Tvs = sb(sA, [128, 32, 2, 130], BF16, "vs")
        kwT, TkwT = sb(sA, [128, 2, 1536], BF16, "kwT")
        vw, Tvw = sb(sA, [128, 12, 2, 130], BF16, "vw")
        kcT, TkcT = sb(sA, [128, 2, 256], BF16, "kcT")
        vcaug, Tvcaug = sb(sA, [128, 2, 2, 194], F32, "vcaug")
        xnT, _ = sb(sA, [128, 2, 16, 512], BF16, "xnT")
        xnTA, TxnTA = xnT[:, 0], T("xnTA")
        xnTB, TxnTB = xnT[:, 1], T("xnTB")
        xnTh, TxnTh = sb(sA, [128, 16, 2], BF16, "xnTh")
        OP("pool", "memset", [], [Tvs], ap=vs[:], constant=1.0)
        OP("pool", "memset", [], [Tvw], ap=vw[:], constant=1.0)
        OP("pool", "memset", [], [Tvcaug], ap=vcaug[:], constant=0.0)
        OP("pool", "memset", [], [Tvcaug], ap=vcaug[:, :, :, 128:129], constant=1.0)
        for tl in range(2):
            for kvh in range(2):
                DMA("sp", vcaug[:, tl, kvh, 130:194], ovs_d[:, tl, :], [], [Tvcaug])

        def rstd_from_sumsq(sqt, Tsq, n):
            OP("dve", "tensor_scalar", [Tsq], [Tsq], out=sqt[:, 1:2], in0=sqt[:, 0:1], scalar1=1.0 / n, scalar2=EPS,
               op0=ALU.mult, op1=ALU.add)
            OP("act", "activation", [Tsq], [Tsq], out=sqt[:, 1:2], in_=sqt[:, 1:2], func=AF.Sqrt)
            OP("dve", "reciprocal", [Tsq], [Tsq], out=sqt[:, 0:1], in_=sqt[:, 1:2])

        with ExitStack() as s1:
            wkv, Twkv = sb(s1, [128, 16, 1536], BF16, "wkv")
            wck, Twck = sb(s1, [128, 32, 128], BF16, "wck")
            wcv, Twcv = sb(s1, [128, 32, 128], BF16, "wcv")
            cposT, TcposT = sb(s1, [128, 32], BF16, "cposT")
            constk, Tconstk = sb(s1, [128, 2], F32, "constk")
            kcvbuf, Tkcv = sb(s1, [128, 4, 528], BF16, "kcvbuf")
            vcT, TvcT = sb(s1, [128, 2, 256], F32, "vcT")
            xst = [(mixF[:, 4096 + i * 2048:4096 + (i + 1) * 2048], T("xst%d" % i)) for i in range(2)]
            xnb = [sb(s1, [128, D], BF16, "xnb") for _ in range(2)]
            junk, Tjunk = sb(s1, [128, D], BF16, "junk")
            sq = [sb(s1, [128, 2], F32, "sq") for _ in range(2)]
            wst = [(mixF[:, i * 2048:(i + 1) * 2048].rearrange("p (k c) -> p k c", k=16), T("wst%d" % i)) for i in range(2)]
            pT = [psb(s1, [128, 8, 128], BF16, "pT") for _ in range(2)]
            pM = [psb(s1, [128, 512], F32, "pM") for _ in range(4)]
            pC, TpC = psb(s1, [128, 512], F32, "pC")

            for ci in range(12):
                stg, Tstg = wst[ci % 2]
                c0 = 1024 + ci * 128
                DMA("sp", stg, w_in[:, c0:c0 + 128].rearrange("(k p) c -> p k c", p=128), [], [Tstg])
                OP("pool", "tensor_tensor", [Tstg, Tgcol], [Twkv], out=wkv[:, :, ci * 128:(ci + 1) * 128], in0=stg,
                   in1=gcol[:].unsqueeze(2).to_broadcast([128, 16, 128]), op=ALU.mult)
            DMA("pool", wck[:], w_cmp_k.rearrange("l d e -> d l e"), [], [Twck])
            DMA("pool", wcv[:], w_cmp_v.rearrange("l d e -> d l e"), [], [Twcv])
            DMA("pool", cposT[:], cmp_pos.rearrange("l d -> d l"), [], [TcposT], **NCD)
            OP("dve", "memset", [], [Tkcv], ap=kcvbuf[:], constant=0.0)
            for ti, (wc, Twc) in enumerate(((wck, Twck), (wcv, Twcv))):
                for l in range(32):
                    OP("pe", "matmul", [Twc, TcposT], [TpC], out=pC[:, ti:ti + 1], lhsT=wc[:, l, :], rhs=cposT[:, l:l + 1],
                       start=(l == 0), stop=(l == 31))
            OP("dve", "tensor_copy", [TpC], [Tconstk], out=constk[:], in_=pC[:, 0:2])

            xcount = [0]

            def norm_transpose(row0, dst, Tdst, col0):
                i = xcount[0] % 2
                xcount[0] += 1
                (xt, Txt), (xb, Txb), (sqt, Tsq) = xst[i], xnb[i], sq[i]
                DMA("sp", xt, xs[row0:row0 + 128, :], [], [Txt])
                OP("act", "activation", [Txt], [Tjunk, Tsq], out=junk[:], in_=xt, func=AF.Square, accum_out=sqt[:, 0:1])
                rstd_from_sumsq(sqt, Tsq, D)
                OP("dve", "tensor_scalar", [Txt, Tsq], [Txb], out=xb[:], in0=xt, scalar1=sqt[:, 0:1], scalar2=None, op0=ALU.mult)
                for half in range(2):
                    pt, Tpt = pT[half]
                    for k8 in range(8):
                        kc = half * 8 + k8
                        OP("pe", "transpose", [Txb, Tidentb], [Tpt], out=pt[:, k8, :], in_=xb[:, kc * 128:(kc + 1) * 128],
                           identity=identb[:])
                    evac(dst[:, half * 8:(half + 1) * 8, col0:col0 + 128], pt[:], [Tpt], [Tdst])

            mrr = [0]

            def nextpm():
                mrr[0] += 1
                return pM[mrr[0] % 4]

            def NT(g):
                xg, Txg = (xnTA, TxnTA) if g % 2 == 0 else (xnTB, TxnTB)
                for j in range(4):
                    norm_transpose((4 * g + j) * 128, xg, Txg, j * 128)
                if g == 5:
                    OP("dve", "tensor_copy", [TxnTB], [TxnTh], out=xnTh[:], in_=xnTB[:, :, 510:512])

            NT(0)
            for g in range(8):
                xg, Txg = (xnTA, TxnTA) if g % 2 == 0 else (xnTB, TxnTB)
                if g < 7:
                    NT(g + 1)
                fm = [(0, kcvbuf[:, 0, 16:528], Tkcv), (128, kcvbuf[:, 1, 16:528], Tkcv),
                      (256, kcvbuf[:, 2, 16:528], Tkcv), (384, kcvbuf[:, 3, 16:528], Tkcv),
                      (512, ksT[:, 0, g * 512:(g + 1) * 512], TksT), (640, ksT[:, 1, g * 512:(g + 1) * 512], TksT)]
                if g >= 5:
                    fm += [(1024, kwT[:, 0, (g - 5) * 512:(g - 4) * 512], TkwT),
                           (1152, kwT[:, 1, (g - 5) * 512:(g - 4) * 512], TkwT)]
                for (co, dst, Td) in fm:
                    pm, Tpm = nextpm()
                    for kc in range(16):
                        OP("pe", "matmul", [Twkv, Txg], [Tpm], out=pm[:], lhsT=wkv[:, kc, co:co + 128], rhs=xg[:, kc, :],
                           start=(kc == 0), stop=(kc == 15))
                    evac(dst, pm[:], [Tpm], [Td])
                for j in range(4):
                    slot = 4 * g + j
                    pm, Tpm = nextpm()
                    for kc in range(16):
                        OP("pe", "matmul", [Twkv, Txg], [Tpm], out=pm[:, 0:256], lhsT=xg[:, kc, j * 128:(j + 1) * 128],
                           rhs=wkv[:, kc, 768:1024], start=(kc == 0), stop=(kc == 15))
                    evac(vs[:, slot, :, 0:128], pm[:, 0:256].rearrange("p (a b) -> p a b", a=2), [Tpm], [Tvs])
                    if g >= 5:
                        pm, Tpm = nextpm()
                        for kc in range(16):
                            OP("pe", "matmul", [Twkv, Txg], [Tpm], out=pm[:, 0:256], lhsT=xg[:, kc, j * 128:(j + 1) * 128],
                               rhs=wkv[:, kc, 1280:1536], start=(kc == 0), stop=(kc == 15))
                        evac(vw[:, slot - 20, :, 0:128], pm[:, 0:256].rearrange("p (a b) -> p a b", a=2), [Tpm], [Tvw])
                for ti in range(2):
                    wc, Twc = (wck, Twck) if ti == 0 else (wcv, Twcv)
                    for l in range(32):
                        OP("pe", "matmul", [Twc, Tkcv], [TpC], out=pC[:, 64 * ti:64 * ti + 64].rearrange("p (a b) -> p a b", a=2), lhsT=wc[:, l, :],
                           rhs=kcvbuf[:, 2 * ti:2 * ti + 2, l:l + 497:16], start=(l == 0), stop=(l == 31))
                for kvh in range(2):
                    OP("dve", "tensor_scalar", [TpC, Tconstk], [TkcT], out=kcT[:, kvh, 32 * g:32 * g + 32],
                       in0=pC[:, 32 * kvh:32 * kvh + 32], scalar1=constk[:, 0:1], scalar2=None, op0=ALU.add)
                    OP("dve", "tensor_scalar", [TpC, Tconstk], [TvcT], out=vcT[:, kvh, 32 * g:32 * g + 32],
                       in0=pC[:, 64 + 32 * kvh:96 + 32 * kvh], scalar1=constk[:, 1:2], scalar2=None, op0=ALU.add)
                OP("dve", "tensor_copy", [Tkcv], [Tkcv], out=kcvbuf[:, :, 0:16], in_=kcvbuf[:, :, 512:528])
            for tl in range(2):
                for kvh in range(2):
                    pm, Tpm = nextpm()
                    OP("pe", "transpose", [TvcT, Tident], [Tpm], out=pm[:, 0:128], in_=vcT[:, kvh, tl * 128:(tl + 1) * 128],
                       identity=ident[:])
                    OP("dve", "tensor_copy", [Tpm], [Tvcaug], out=vcaug[:, tl, kvh, 0:128], in_=pm[:, 0:128])
            if stop_after == 1:
                dump("ksT", ksT[:], TksT, [128, 2, S], BF16)
                dump("vs", vs[:], Tvs, [128, 32, 2, 130], BF16)
                dump("kwT", kwT[:], TkwT, [128, 2, 1536], BF16)
                dump("kcT", kcT[:], TkcT, [128, 2, 256], BF16)
                dump("vcaug", vcaug[:], Tvcaug, [128, 2, 2, 194], F32)
            P.barrier()
        if stop_after == 1:
            finish()
            return nc, dbg_outs

        qT, TqT = sb(sA, [128, 8, NQ], BF16, "qT")
        xh = [(xnTA, TxnTA), (xnTB, TxnTB)]
        with ExitStack() as s2:
            wst2 = [sb(s2, [128, 16, 128], F32, "wst2") for _ in range(2)]
            wb2 = [sb(s2, [128, 16, 128], BF16, "wb2") for _ in range(4)]
            bT, TbT = sb(s2, [128, 1026], F32, "bT")
            cT, TcT = sb(s2, [128, 1026], F32, "cT")
            hT, ThT = sb(s2, [128, 1026], F32, "hT")
            uu, Tuu = sb(s2, [128, 1026], F32, "uu")
            yy, Tyy = sb(s2, [128, 1024], F32, "yy")
            oo, Too = sb(s2, [128, 1024], F32, "oo")
            osq, Tosq = sb(s2, [128, 1024], F32, "osq")
            rs, Trs = sb(s2, [128, 1024], F32, "rs")
            pM2 = [psb(s2, [128, 512], F32, "pM2") for _ in range(4)]
            pH, TpH = psb(s2, [128, 512], F32, "pH")
            wrr = [0]
            mr2 = [0]

            def nextpm2():
                mr2[0] += 1
                return pM2[mr2[0] % 4]

            def load_w(c0, width=128):
                i = wrr[0]
                wrr[0] += 1
                stg, Tstg = wst2[i % 2]
                wb, Twb = wb2[i % 4]
                DMA("sp", stg[:, :, 0:width], w_in[:, c0:c0 + width].rearrange("(k p) c -> p k c", p=128), [], [Tstg])
                OP("pool", "tensor_tensor", [Tstg, Tgcol], [Twb], out=wb[:, :, 0:width], in0=stg[:, :, 0:width],
                   in1=gcol[:].unsqueeze(2).to_broadcast([128, 16, width]), op=ALU.mult)
                return wb, Twb

            def proj_fm(wb, Twb, dst_fn, Tdst):
                for half in range(2):
                    xg, Txg = xh[half]
                    pm, Tpm = nextpm2()
                    for kc in range(16):
                        OP("pe", "matmul", [Twb, Txg], [Tpm], out=pm[:], lhsT=wb[:, kc, :], rhs=xg[:, kc, :],
                           start=(kc == 0), stop=(kc == 15))
                    evac(dst_fn(half), pm[:], [Tpm], [Tdst])

            for h in range(8):
                wb, Twb = load_w(h * 128)
                proj_fm(wb, Twb, lambda half, h=h: qT[:, h, half * 512:(half + 1) * 512], TqT)
            wg, Twg = load_w(2560, 24)
            for tile in range(8):
                xg, Txg = xh[tile // 4]
                cs = (tile % 4) * 128
                pm, Tpm = nextpm2()
                for kc in range(16):
                    OP("pe", "matmul", [Twg, Txg], [Tpm], out=pm[:, 0:24], lhsT=xg[:, kc, cs:cs + 128], rhs=wg[:, kc, 0:24],
                       start=(kc == 0), stop=(kc == 15))
                OP("act", "activation", [Tpm], [Tgsig], out=gsig[:, tile, :], in_=pm[:, 0:24], func=AF.Sigmoid)
            for ch in range(8):
                wb_b, Twb_b = load_w(2584 + ch * 128)
                wb_c, Twb_c = load_w(3608 + ch * 128)
                wb_h, Twb_h = load_w(4632 + ch * 128)
                for (wb, Twb, dT, TdT) in ((wb_b, Twb_b, bT, TbT), (wb_c, Twb_c, cT, TcT), (wb_h, Twb_h, hT, ThT)):
                    proj_fm(wb, Twb, lambda half, dT=dT: dT[:, 2 + half * 512:2 + (half + 1) * 512], TdT)
                for hi, (wb, Twb, dT, TdT) in enumerate(((wb_c, Twb_c, cT, TcT), (wb_h, Twb_h, hT, ThT))):
                    for kc in range(16):
                        OP("pe", "matmul", [Twb, TxnTh], [TpH], out=pH[:, 2 * hi:2 * hi + 2], lhsT=wb[:, kc, :], rhs=xnTh[:, kc, :],
                           start=(kc == 0), stop=(kc == 15))
                    OP("dve", "tensor_copy", [TpH], [TdT], out=dT[:, 0:2], in_=pH[:, 2 * hi:2 * hi + 2])
                OP("dve", "tensor_tensor", [TcT, ThT], [Tuu], out=uu[:], in0=cT[:], in1=hT[:], op=ALU.mult)
                OP("dve", "tensor_scalar", [Tuu, Tcw, Tcbias], [Tyy], out=yy[:], in0=uu[:, 2:1026], scalar1=cw[:, 2, ch:ch + 1],
                   scalar2=cbias[:, ch:ch + 1], op0=ALU.mult, op1=ALU.add)
                OP("dve", "scalar_tensor_tensor", [Tuu, Tcw, Tyy], [Tyy], out=yy[:], in0=uu[:, 1:1025], scalar=cw[:, 1, ch:ch + 1],
                   in1=yy[:], op0=ALU.mult, op1=ALU.add)
                OP("dve", "scalar_tensor_tensor", [Tuu, Tcw, Tyy], [Tyy], out=yy[:], in0=uu[:, 0:1024], scalar=cw[:, 0, ch:ch + 1],
                   in1=yy[:], op0=ALU.mult, op1=ALU.add)
                OP("dve", "tensor_tensor", [TbT, Tyy], [Too], out=oo[:], in0=bT[:, 2:1026], in1=yy[:], op=ALU.mult)
                OP("pool", "tensor_tensor", [Too], [Tosq], out=osq[:], in0=oo[:], in1=oo[:], op=ALU.mult)
                for half in range(2):
                    pm, Tpm = nextpm2()
                    OP("pe", "matmul", [Tonesf, Tosq], [Tpm], out=pm[:], lhsT=onesf[:], rhs=osq[:, half * 512:(half + 1) * 512],
                       start=True, stop=True)
                    OP("dve", "tensor_scalar", [Tpm], [Trs], out=rs[:, half * 512:(half + 1) * 512], in0=pm[:], scalar1=1.0 / 128,
                       scalar2=EPS, op0=ALU.mult, op1=ALU.add)
                OP("act", "activation", [Trs], [Trs], out=rs[:], in_=rs[:], func=AF.Sqrt)
                OP("dve", "reciprocal", [Trs], [Trs], out=rs[:], in_=rs[:])
                OP("dve", "scalar_tensor_tensor", [Too, Tcgw, Trs], [TmixT], out=mixT[:, 8 + ch, :], in0=oo[:], scalar=cgw[:, ch:ch + 1],
                   in1=rs[:], op0=ALU.mult, op1=ALU.mult)
            if stop_after == 2:
                dump("qT", qT[:], TqT, [128, 8, NQ], BF16)
                dump("gsig", gsig[:], Tgsig, [128, 8, 24], F32)
                dump("mixc", mixT[:, 8:16, :], TmixT, [128, 8, NQ], BF16)
            P.barrier()
        if stop_after == 2:
            finish()
            return nc, dbg_outs
        with ExitStack() as s3:
            BT0, TBT0 = sb(s3, [128, 8, 128], F32, "BT0")
            BT1, TBT1 = sb(s3, [128, 8, 128], F32, "BT1")
            BT4, TBT4 = sb(s3, [128, 8, 128], F32, "BT4")
            BTC, TBTC = sb(s3, [128, 8, 128], F32, "BTC")
            CB, TCB = xnT[:].rearrange("p a b c -> p (a b c)").bitcast(F32).rearrange("p (h t) -> p h t", h=8), T("CB")
            eexp, Teexp = sb(s3, [128, 4096], BF16, "eexp")
            cbnf, Tcbnf = sb(s3, [128, 8, 64], F32, "cbnf")
            sadd, Tsadd = sb(s3, [128, 8, 64], F32, "sadd")
            cbm, Tcbm = sb(s3, [128, 8, 64], F32, "cbm")
            PT = [sb(s3, [128, 512], BF16, "PT") for _ in range(4)]
            ec, Tec = sb(s3, [128, 2, 512], F32, "ec")
            tmpS = [sb(s3, [128, 512], F32, "tmpS") for _ in range(2)]
            ocmp2 = [sb(s3, [128, 4, 194], F32, "ocmp") for _ in range(2)]
            osel, Tosel = sb(s3, [128, 4, 130], F32, "osel")
            owin, Towin = sb(s3, [128, 4, 130], F32, "owin")
            sm, Tsm = sb(s3, [128, 64], F32, "sm")
            imp, Timp = sb(s3, [128, 64], F32, "imp")
            score, Tscore = sb(s3, [128, 64], F32, "score")
            work, Twork = sb(s3, [128, 64], F32, "work")
            top8, Ttop8 = sb(s3, [128, 16], F32, "top8")
            negm, Tnegm = sb(s3, [128, 64], F32, "negm")
            negT = [sb(s3, [128, 128], BF16, "negT") for _ in range(2)]
            oc, Toc = sb(s3, [128, 4, 128], F32, "oc")
            on, Ton = sb(s3, [128, 4, 128], F32, "on")
            jk, Tjk = sb(s3, [128, 128], F32, "jk")
            pS = [psb(s3, [128, 512], F32, "pS") for _ in range(2)]
            pO = [psb(s3, [128, 512], F32, "pO") for _ in range(4)]
            pX = [psb(s3, [128, 512], F32, "pX") for _ in range(2)]
            Tgtd = T("gtd")
            if dbg:
                obr, Tobr = sb(s3, [128, 4, 128], F32, "obr")
            s3a = ExitStack()
            rb33, Trb33 = sb(s3a, [33, 8], F32, "rb33")
            rb31, Trb31 = sb(s3a, [8, 1], F32, "rb31")
            DMA("sp", rb31[:], rel_bias[31:32, :].rearrange("a h -> h a"), [], [Trb31], **NCD)
            oh33c = [sb(s3a, [33, 512], F32, "oh33c") for _ in range(2)]
            gtsc = [sb(s3a, [8, 512], F32, "gtsc") for _ in range(2)]
            hkc, Thkc = sb(s3a, [128, NQ], F32, "hkc")
            hk, Thk = hkc[:].rearrange("p (a b) -> p a b", a=8), Thkc

            OP("pool", "memset", [], [Teexp], ap=eexp[64:128, :], constant=0.0)
            DMA("sp", eexp[0:64, :], eexp_d, [], [Teexp])
            for (nT_, TnT_) in negT:
                OP("pool", "memset", [], [TnT_], ap=nT_[:], constant=0.0)
            DMA("sp", cbnf[:], cbnf_d, [], [Tcbnf])
            DMA("sp", sadd[:], sadd_d, [], [Tsadd])
            DMA("sp", cbm[:], cb_d, [], [Tcbm])
            OP("dve", "memset", [], [Trb33], ap=rb33[32:33, :], constant=1.0)
            DMA("sp", rb33[0:32, :], rel_bias, [], [Trb33])
            for c0 in range(0, MTOT, 512):
                wd = min(512, MTOT - c0)
                px, Tpx = pX[(c0 // 512) % 2]
                oh33, Toh33 = oh33c[(c0 // 512) % 2]
                gts, Tgts = gtsc[(c0 // 512) % 2]
                DMA("sp", oh33[:, 0:wd], oh33_d[:, c0:c0 + wd], [], [Toh33])
                OP("pe", "matmul", [Trb33, Toh33], [Tpx], out=px[0:8, 0:wd], lhsT=rb33[:, :], rhs=oh33[:, 0:wd], start=True, stop=True)
                OP("dve", "tensor_scalar", [Tpx, Trb31], [Tgts], out=gts[:, 0:wd], in0=px[0:8, 0:wd], scalar1=rb31[:, 0:1], scalar2=None, op0=ALU.subtract)
                DMA("sp", gtd[:, c0:c0 + wd], gts[:, 0:wd], [Tgts], [Tgtd])

            def hankel(off, pstep, ncol, heads=True):
                if heads:
                    return bass.AP(tensor=gtd.tensor, offset=off, ap=[[pstep, 128], [MTOT, 8], [1, ncol]])
                return bass.AP(tensor=gtd.tensor, offset=off, ap=[[pstep, 128], [1, ncol]])

            for (BT, TBT, off) in ((BT0, TBT0, 0), (BT1, TBT1, 128), (BT4, TBT4, MSEG0)):
                DMA("sp", hk, hankel(off, 1, 128), [Tgtd], [Thk])
                hkf = hkc
                BTf = BT[:].rearrange("p a b -> p (a b)")
                for half in range(2):
                    px, Tpx = pX[half]
                    OP("pe", "matmul", [Tjmat, Thk], [Tpx], out=px[:], lhsT=jmat[:], rhs=hkf[:, half * 512:(half + 1) * 512], start=True, stop=True)
                    OP("dve", "tensor_copy", [Tpx], [TBT], out=BTf[:, half * 512:(half + 1) * 512], in_=px[:])
            for h in range(8):
                DMA("sp", hkc[:], hankel(h * MTOT + MSEG0 + MSEG1, 16, NQ, heads=False), [Tgtd], [Thkc])
                for half in range(2):
                    px, Tpx = pX[half]
                    OP("pe", "matmul", [Tjmat, Thkc], [Tpx], out=px[:], lhsT=jmat[:], rhs=hkc[:, half * 512:(half + 1) * 512], start=True, stop=True)
                    OP("dve", "tensor_copy", [Tpx], [TCB], out=CB[:, h, half * 512:(half + 1) * 512], in_=px[:])
            OP("dve", "tensor_copy", [Tchcol], [TBTC], out=BTC[:], in_=chcol[:].unsqueeze(2).to_broadcast([128, 8, 128]))

            P.barrier()
            s3a.close()
            for (tab, c0) in ((peer_u, 0), (peer_v, D)):
                for r0 in range(0, 16384, 1024):
                    P.dma("pool", dict(out=uv16[r0:r0 + 1024, c0:c0 + D], in_=tab[r0:r0 + 1024, :]), reads=[], writes=[Tuv16], nowaw=True)
            srr = [0]
            prr = [0]
            trr = [0]
            nrr = [0]

            def nextS():
                srr[0] += 1
                return pS[srr[0] % 2]

            def nextPT():
                prr[0] += 1
                return PT[prr[0] % 4]

            def nextTmp():
                trr[0] += 1
                return tmpS[trr[0] % 2]

            pending = []

            def emit_tr(qt_, kvh_):
                px, Tpx = pX[1]
                for g in range(4):
                    OP("pe", "transpose", [Ton, Tident], [Tpx], out=px[:, g * 128:(g + 1) * 128], in_=on[:, g, :], identity=ident[:])
                for g in range(4):
                    OP("dve", "tensor_scalar", [Tpx, Tagw], [TmixT], out=mixT[:, 4 * kvh_ + g, qt_ * 128:(qt_ + 1) * 128], in0=px[:, g * 128:(g + 1) * 128],
                       scalar1=agw[:, 4 * kvh_ + g:4 * kvh_ + g + 1], scalar2=None, op0=ALU.mult)

            for qt in range(8):
                qs = slice(qt * 128, (qt + 1) * 128)
                for kvh in range(2):
                    hs = slice(4 * kvh, 4 * kvh + 4)
                    q4 = qT[:, hs, qs]
                    ocmp, Tocmp = ocmp2[(2 * qt + kvh) % 2]
                    for tl in range(2):
                        ps, Tps = nextS()
                        OP("pe", "matmul", [TkcT, TqT], [Tps], out=ps[:], lhsT=kcT[:, kvh, tl * 128:(tl + 1) * 128], rhs=q4, start=True, stop=True)
                        if tl == 0:
                            OP("act", "activation", [Tps, Tcvalid], [Tec], out=ec[:, 0, :], in_=ps[:], func=AF.Exp, scale=SCALE, bias=cvalid[:, 0:1])
                        else:
                            tm, Ttm = nextTmp()
                            OP("dve", "scalar_tensor_tensor", [Tps, TCB], [Ttm], out=tm[:].rearrange("p (a b) -> p a b", a=4), in0=ps[:].rearrange("p (a b) -> p a b", a=4),
                               scalar=SCALE, in1=CB[:, hs, qs], op0=ALU.mult, op1=ALU.add)
                            OP("act", "activation", [Ttm, Tcvalid], [Tec], out=ec[:, 1, :], in_=tm[:], func=AF.Exp, bias=cvalid[:, 1:2])
                    for g in range(4):
                        po, Tpo = pO[g]
                        for tl in range(2):
                            OP("pe", "matmul", [Tec, Tvcaug], [Tpo], out=po[:, 0:194], lhsT=ec[:, tl, g * 128:(g + 1) * 128], rhs=vcaug[:, tl, kvh, :],
                               start=(tl == 0), stop=(tl == 1))
                        evac(ocmp[:, g, :], po[:, 0:194], [Tpo], [Tocmp])
                    OP("dve", "tensor_scalar", [Tocmp], [Tsm], out=sm[:, 0:4], in0=ocmp[:, :, 128], scalar1=1e-30, scalar2=None, op0=ALU.max)
                    OP("dve", "reciprocal", [Tsm], [Tsm], out=sm[:, 0:4], in_=sm[:, 0:4])
                    OP("dve", "tensor_scalar", [Tocmp, Tsm], [Timp], out=imp[:], in0=ocmp[:, 0, 130:194], scalar1=sm[:, 0:1], scalar2=None, op0=ALU.mult)
                    for g in range(1, 4):
                        OP("dve", "scalar_tensor_tensor", [Tocmp, Tsm, Timp], [Timp], out=imp[:], in0=ocmp[:, g, 130:194], scalar=sm[:, g:g + 1],
                           in1=imp[:], op0=ALU.mult, op1=ALU.add)
                    OP("dve", "tensor_tensor", [Timp, Tcbnf], [Tscore], out=score[:], in0=imp[:], in1=cbnf[:, qt, :], op=ALU.mult)
                    OP("dve", "tensor_tensor", [Tscore, Tsadd], [Tscore], out=score[:], in0=score[:], in1=sadd[:, qt, :], op=ALU.add)
                    OP("dve", "max", [Tscore], [Ttop8], out=top8[:, 0:8], in_=score[:])
                    OP("dve", "match_replace", [Tscore, Ttop8], [Twork], out=work[:], in_to_replace=top8[:, 0:8], in_values=score[:], imm_value=-1e30)
                    OP("dve", "max", [Twork], [Ttop8], out=top8[:, 8:16], in_=work[:])
                    OP("dve", "tensor_scalar", [Tscore, Ttop8], [Twork], out=work[:], in0=score[:], scalar1=top8[:, 15:16], scalar2=None, op0=ALU.is_ge)
                    OP("dve", "tensor_tensor", [Twork, Tcbm], [Twork], out=work[:], in0=work[:], in1=cbm[:, qt, :], op=ALU.mult)
                    OP("dve", "tensor_scalar", [Twork], [Tnegm], out=negm[:], in0=work[:], scalar1=-NEG, scalar2=NEG, op0=ALU.mult, op1=ALU.add)
                    if pending:
                        emit_tr(*pending.pop())
                    def win_S(k):
                        wslot = qt + k
                        ps, Tps = nextS()
                        OP("pe", "matmul", [TkwT, TqT], [Tps], out=ps[:], lhsT=kwT[:, kvh, wslot * 128:(wslot + 1) * 128], rhs=q4, start=True, stop=True)
                        return ps, Tps
                    nxt = win_S(0)
                    for k in range(5):
                        BT, TBT = ((BT4, TBT4), (BTC, TBTC), (BTC, TBTC), (BT1, TBT1), (BT0, TBT0))[k]
                        wslot = qt + k
                        ps, Tps = nxt
                        if k < 4:
                            nxt = win_S(k + 1)
                        pt, Tpt = nextPT()
                        if k in (1, 2):
                            OP("act", "activation", [Tps, Tkvalid], [Tpt], out=pt[:], in_=ps[:], func=AF.Exp, scale=SCALE, bias=kvalid[:, 20 + wslot:21 + wslot])
                        else:
                            tm, Ttm = nextTmp()
                            OP("dve", "scalar_tensor_tensor", [Tps, TBT], [Ttm], out=tm[:].rearrange("p (a b) -> p a b", a=4), in0=ps[:].rearrange("p (a b) -> p a b", a=4),
                               scalar=SCALE, in1=BT[:, hs, :], op0=ALU.mult, op1=ALU.add)
                            OP("act", "activation", [Ttm, Tkvalid], [Tpt], out=pt[:], in_=tm[:], func=AF.Exp, bias=kvalid[:, 20 + wslot:21 + wslot])
                        for g in range(4):
                            po, Tpo = pO[g]
                            OP("pe", "matmul", [Tpt, Tvw], [Tpo], out=po[:, 0:130], lhsT=pt[:, g * 128:(g + 1) * 128], rhs=vw[:, wslot, kvh, :],
                               start=(k == 0), stop=(k == 4))
                    for g in range(4):
                        evac(owin[:, g, :], pO[g][0][:, 0:130], [pO[g][1]], [Towin])
                    px, Tpx = pX[0]
                    OP("pe", "transpose", [Tnegm, Tident], [Tpx], out=px[0:64, 0:128], in_=negm[:], identity=ident[:])
                    nrr[0] += 1
                    nT, TnT = negT[nrr[0] % 2]
                    OP("act", "activation", [Tpx], [TnT], out=nT[0:64, :], in_=px[0:64, 0:128], func=AF.Copy)
                    last = 24 + qt
                    nTb = nT[:].unsqueeze(1).to_broadcast([128, 4, 128])

                    def sel_S(slot):
                        ps, Tps = nextS()
                        OP("pe", "matmul", [TksT, TqT], [Tps], out=ps[:], lhsT=ksT[:, kvh, slot * 128:(slot + 1) * 128], rhs=q4, start=True, stop=False)
                        OP("pe", "matmul", [Teexp, TnT], [Tps], out=ps[:], lhsT=eexp[:, slot * 128:(slot + 1) * 128], rhs=nTb, start=False, stop=True)
                        return ps, Tps
                    nxt = sel_S(0)
                    for slot in range(last + 1):
                        ps, Tps = nxt
                        if slot < last:
                            nxt = sel_S(slot + 1)
                        pt, Tpt = nextPT()
                        rel = slot - last
                        if rel >= -1:
                            BT, TBT = (BT0, TBT0) if rel == 0 else (BT1, TBT1)
                            tm, Ttm = nextTmp()
                            OP("dve", "scalar_tensor_tensor", [Tps, TBT], [Ttm], out=tm[:].rearrange("p (a b) -> p a b", a=4),
                               in0=ps[:].rearrange("p (a b) -> p a b", a=4), scalar=SCALE, in1=BT[:, hs, :], op0=ALU.mult, op1=ALU.add)
                            OP("act", "activation", [Ttm], [Tpt], out=pt[:], in_=tm[:], func=AF.Exp)
                        else:
                            OP("act", "activation", [Tps], [Tpt], out=pt[:], in_=ps[:], func=AF.Exp, scale=SCALE)
                        for g in range(4):
                            po, Tpo = pO[g]
                            OP("pe", "matmul", [Tpt, Tvs], [Tpo], out=po[:, 0:130], lhsT=pt[:, g * 128:(g + 1) * 128], rhs=vs[:, slot, kvh, :],
                               start=(slot == 0), stop=(slot == last))
                    for g in range(4):
                        evac(osel[:, g, :], pO[g][0][:, 0:130], [pO[g][1]], [Tosel])
                    OP("dve", "reciprocal", [Tosel], [Tsm], out=sm[:, 4:8], in_=osel[:, :, 128])
                    OP("dve", "reciprocal", [Towin], [Tsm], out=sm[:, 8:12], in_=owin[:, :, 128])
                    gv = gsig[:, qt, 12 * kvh:12 * kvh + 12].rearrange("p (g b) -> p g b", b=3)
                    for br in range(3):
                        OP("dve", "tensor_tensor", [Tsm, Tgsig], [Tsm], out=sm[:, 12 + 4 * br:16 + 4 * br], in0=sm[:, 4 * br:4 * br + 4], in1=gv[:, :, br], op=ALU.mult)
                    if dbg and stop_after == 3 and qt in (0, 3, 7):
                        for br, (ob, Tob) in enumerate(((ocmp, Tocmp), (osel, Tosel), (owin, Towin))):
                            for g in range(4):
                                OP("dve", "tensor_scalar", [Tob, Tsm], [Tobr], out=obr[:, g, :], in0=ob[:, g, 0:128], scalar1=sm[:, 4 * br + g:4 * br + g + 1],
                                   scalar2=None, op0=ALU.mult)
                            dump("obr_%d_%d_%d" % (qt, kvh, br), obr[:], Tobr, [128, 4, 128], F32)
                        dump("score_%d_%d" % (qt, kvh), score[:], Tscore, [128, 64], F32)
                    for g in range(4):
                        OP("dve", "tensor_scalar", [Tocmp, Tsm], [Toc], out=oc[:, g, :], in0=ocmp[:, g, 0:128], scalar1=sm[:, 12 + g:13 + g], scalar2=None, op0=ALU.mult)
                        OP("dve", "scalar_tensor_tensor", [Tosel, Tsm, Toc], [Toc], out=oc[:, g, :], in0=osel[:, g, 0:128], scalar=sm[:, 16 + g:17 + g], in1=oc[:, g, :],
                           op0=ALU.mult, op1=ALU.add)
                        OP("dve", "scalar_tensor_tensor", [Towin, Tsm, Toc], [Toc], out=oc[:, g, :], in0=owin[:, g, 0:128], scalar=sm[:, 20 + g:21 + g], in1=oc[:, g, :],
                           op0=ALU.mult, op1=ALU.add)
                        OP("dve", "scalar_tensor_tensor", [Toc], [Tjk, Tsm], out=jk[:], in0=oc[:, g, :], scalar=1.0, in1=oc[:, g, :],
                           op0=ALU.mult, op1=ALU.mult, accum_out=sm[:, 24 + g:25 + g])
                    OP("dve", "tensor_scalar", [Tsm], [Tsm], out=sm[:, 28:32], in0=sm[:, 24:28], scalar1=1.0 / 128, scalar2=EPS, op0=ALU.mult, op1=ALU.add)
                    OP("act", "activation", [Tsm], [Tsm], out=sm[:, 28:32], in_=sm[:, 28:32], func=AF.Sqrt)
                    OP("dve", "reciprocal", [Tsm], [Tsm], out=sm[:, 24:28], in_=sm[:, 28:32])
                    OP("dve", "tensor_tensor", [Toc, Tsm], [Ton], out=on[:], in0=oc[:], in1=sm[:, 24:28].unsqueeze(2).to_broadcast([128, 4, 128]), op=ALU.mult)
                    pending.append((qt, kvh))
            emit_tr(*pending.pop())
            if stop_after == 3:
                dump("mixa", mixT[:, 0:8, :], TmixT, [128, 8, NQ], BF16)
            P.barrier()
        sA.close()
        if stop_after == 3:
            finish()
            return nc, dbg_outs
        h2, Th2 = sb(st, [128, 8, D], F32, "h2")
        for tile in range(8):
            DMA("sp", h2[:, tile, :], xs[3072 + tile * 128:3072 + (tile + 1) * 128, :], [], [Th2])
        with ExitStack() as s4:
            wos = [sb(s4, [128, 16, 512], F32, "wos") for _ in range(2)]
            wob = [sb(s4, [128, 16, 512], BF16, "wob") for _ in range(2)]
            pM4 = [psb(s4, [128, 512], F32, "pM4") for _ in range(4)]
            m4 = [0]
            for cc in range(4):
                stg, Tstg = wos[cc % 2]
                wb, Twb = wob[cc % 2]
                cs = slice(cc * 512, (cc + 1) * 512)
                DMA("sp", stg[:], w_out[:, cs].rearrange("(k p) c -> p k c", p=128), [], [Tstg])
                for hh in range(2):
                    OP("pool" if hh == 0 else "act", "tensor_copy" if hh == 0 else "activation", [Tstg], [Twb],
                       **(dict(out=wb[:, 8 * hh:8 * hh + 8, :], in_=stg[:, 8 * hh:8 * hh + 8, :]) if hh == 0 else
                          dict(out=wb[:, 8 * hh:8 * hh + 8, :], in_=stg[:, 8 * hh:8 * hh + 8, :], func=AF.Copy)))
                for tile in range(8):
                    m4[0] += 1
                    pm, Tpm = pM4[m4[0] % 4]
                    for kc in range(16):
                        OP("pe", "matmul", [TmixT, Twb], [Tpm], out=pm[:], lhsT=mixT[:, kc, tile * 128:(tile + 1) * 128], rhs=wb[:, kc, :],
                           start=(kc == 0), stop=(kc == 15))
                    OP("dve", "tensor_tensor", [Tpm, Th2], [Th2], out=h2[:, tile, cs], in0=pm[:], in1=h2[:, tile, cs], op=ALU.add)
            if stop_after == 4:
                dump("h2", h2[:], Th2, [128, 8, D], F32)
            P.barrier()
        if stop_after == 4:
            finish()
            return nc, dbg_outs

        xn2, Txn2 = sb(st, [128, 8, D], BF16, "xn2")
        eidx, Teidx = sb(st, [128, 8, 128], U32, "eidx")
        gates, Tgates = sb(st, [128, 8, 128], F32, "gates")
        iota16, Tiota = sb(st, [128, 16], F32, "iota16")
        DMA("sp", iota16[:], iota16_d, [], [Tiota])
        xn2T, Txn2T = mixT, T("xn2T")
        with ExitStack() as s4b:
            eqB, TeqB = sb(s4b, [128, 8, 16, 16], F32, "eqB")
            g2b, Tg2b = eqB[:].rearrange("p t a b -> p (t a b)"), TeqB
            sq4, Tsq4 = sb(s4b, [128, 2], F32, "sq4")
            SKT, TSKT = sb(s4b, [128, 16, 128], BF16, "SKT")
            wqs = [sb(s4b, [128, 16, 128], F32, "wqs")] * 2
            wqb = [sb(s4b, [128, 16, 128], BF16, "wqb") for _ in range(2)]
            skst, Tskst = wqs[0]
            skb, Tskb = wqb[0]
            qpT = [sb(s4b, [128, NQ], BF16, "qpT") for _ in range(2)]
            sw = [sb(s4b, [128, 128], F32, "sw") for _ in range(2)]
            sw2, Tsw2 = sb(s4b, [128, 128], F32, "sw2")
            sv, Tsv = sb(s4b, [128, 8, 16, 16], F32, "sv")
            si, Tsi = sb(s4b, [128, 8, 16, 16], U32, "si")
            pT4 = [psb(s4b, [128, 8, 128], BF16, "pT4") for _ in range(2)]
            pM5 = [psb(s4b, [128, 512], F32, "pM5") for _ in range(4)]
            pS5 = [psb(s4b, [128, 512], F32, "pS5") for _ in range(2)]
            DMA("sp", g2b, ffn_norm_w.partition_broadcast(128), [], [Tg2b], **NCD)
            for tile in range(8):
                OP("act", "activation", [Th2], [Txn2, Tsq4], out=xn2[:, tile, :], in_=h2[:, tile, :], func=AF.Square, accum_out=sq4[:, 0:1])
                rstd_from_sumsq(sq4, Tsq4, D)
                OP("dve", "scalar_tensor_tensor", [Th2, Tsq4, Tg2b], [Txn2], out=xn2[:, tile, :], in0=h2[:, tile, :], scalar=sq4[:, 0:1], in1=g2b,
                   op0=ALU.mult, op1=ALU.mult)
                for half in range(2):
                    pt, Tpt = pT4[half]
                    for k8 in range(8):
                        kc = half * 8 + k8
                        OP("pe", "transpose", [Txn2, Tidentb], [Tpt], out=pt[:, k8, :], in_=xn2[:, tile, kc * 128:(kc + 1) * 128], identity=identb[:])
                    evac(xn2T[:, half * 8:(half + 1) * 8, tile * 128:(tile + 1) * 128], pt[:], [Tpt], [Txn2T])
            DMA("sp", skst[:], peer_sk.rearrange("h n d -> n h d"), [], [Tskst])
            OP("pool", "tensor_copy", [Tskst], [Tskb], out=skb[:], in_=skst[:])
            for half in range(2):
                pt, Tpt = pT4[half]
                for k8 in range(8):
                    OP("pe", "transpose", [Tskb, Tidentb], [Tpt], out=pt[:, k8, :], in_=skb[:, half * 8 + k8, :], identity=identb[:])
                evac(SKT[:, half * 8:(half + 1) * 8, :], pt[:], [Tpt], [TSKT])
            candB, TcandB = sb(s4b, [128, 8, 256], F32, "candB")
            cand2B, Tcand2B = sb(s4b, [128, 256], F32, "cand2B")
            cvB, TcvB = sb(s4b, [128, 8, 16], F32, "cvB")
            ciB, TciB = sb(s4b, [128, 8, 16], U32, "ciB")
            pabB, TpabB = sb(s4b, [128, 2, 8, 16], U32, "pabB")
            rf, Trf = sb(s4b, [128, 8, 8, 16], F32, "rf")
            rsm, Trsm = sb(s4b, [128, 2, 8], F32, "rsm")
            B4 = [128, 8, 16, 16]

            def route_h(h):
                c1, c2 = 2 * h, 2 * h + 1
                OP("dve", "tensor_tensor", [Tsv], [TcandB], out=candB[:].rearrange("p t (a b) -> p t a b", a=16),
                   in0=sv[:, :, c1, :].unsqueeze(3).to_broadcast(B4), in1=sv[:, :, c2, :].unsqueeze(2).to_broadcast(B4), op=ALU.add)
                for t in range(8):
                    OP("dve", "max", [TcandB], [TcvB], out=cvB[:, t, 0:8], in_=candB[:, t, :])
                    OP("dve", "max_index", [TcandB, TcvB], [TciB], out=ciB[:, t, 0:8], in_max=cvB[:, t, 0:8], in_values=candB[:, t, :])
                    OP("dve", "match_replace", [TcandB, TcvB], [Tcand2B], out=cand2B[:], in_to_replace=cvB[:, t, 0:8], in_values=candB[:, t, :], imm_value=-1e30)
                    OP("dve", "max", [Tcand2B], [TcvB], out=cvB[:, t, 8:16], in_=cand2B[:])
                    OP("dve", "max_index", [Tcand2B, TcvB], [TciB], out=ciB[:, t, 8:16], in_max=cvB[:, t, 8:16], in_values=cand2B[:])
                OP("dve", "tensor_single_scalar", [TciB], [TpabB], out=pabB[:, 0], in_=ciB[:], scalar=4, op=ALU.logical_shift_right)
                OP("dve", "tensor_single_scalar", [TciB], [TpabB], out=pabB[:, 1], in_=ciB[:], scalar=15, op=ALU.bitwise_and)
                OP("dve", "tensor_copy", [TpabB], [Trf], out=rf[:, 0:2], in_=pabB[:])
                OP("dve", "tensor_copy", [Tsi], [Trf], out=rf[:, 2], in_=si[:, :, c1, :])
                OP("dve", "tensor_copy", [Tsi], [Trf], out=rf[:, 3], in_=si[:, :, c2, :])
                for (src, sif, dst) in ((0, 2, 4), (1, 3, 5)):
                    OP("dve", "tensor_tensor", [Trf, Tiota], [TeqB], out=eqB[:], in0=rf[:, src].unsqueeze(3).to_broadcast(B4),
                       in1=iota16[:].unsqueeze(1).unsqueeze(1).to_broadcast(B4), op=ALU.is_equal)
                    OP("dve", "tensor_tensor", [TeqB, Trf], [TeqB], out=eqB[:], in0=eqB[:], in1=rf[:, sif].unsqueeze(2).to_broadcast(B4), op=ALU.mult)
                    OP("dve", "tensor_reduce", [TeqB], [Trf], out=rf[:, dst], in_=eqB[:], axis=AX.X, op=ALU.add)
                OP("dve", "scalar_tensor_tensor", [Trf], [Trf], out=rf[:, 6], in0=rf[:, 4], scalar=128.0, in1=rf[:, 5], op0=ALU.mult, op1=ALU.add)
                OP("dve", "tensor_copy", [Trf], [Teidx], out=eidx[:, :, 16 * h:16 * h + 16], in_=rf[:, 6])
                OP("dve", "tensor_tensor", [TcvB], [Trf], out=rf[:, 7], in0=cvB[:], in1=cvB[:, :, 0:1].to_broadcast([128, 8, 16]), op=ALU.subtract)
                OP("act", "activation", [Trf], [Trf], out=rf[:, 7], in_=rf[:, 7], func=AF.Exp)
                OP("dve", "tensor_reduce", [Trf], [Trsm], out=rsm[:, 0], in_=rf[:, 7], axis=AX.X, op=ALU.add)
                OP("dve", "reciprocal", [Trsm], [Trsm], out=rsm[:, 1], in_=rsm[:, 0])
                OP("dve", "tensor_tensor", [Trf, Trsm], [Tgates], out=gates[:, :, 16 * h:16 * h + 16], in0=rf[:, 7],
                   in1=rsm[:, 1].unsqueeze(2).to_broadcast([128, 8, 16]), op=ALU.mult)

            m5 = [0]
            for hc in range(16):
                stg, Tstg = wqs[hc % 2]
                wb, Twb = wqb[hc % 2]
                qp, Tqp = qpT[hc % 2]
                DMA("sp", stg[:], peer_wq[:, hc * 128:(hc + 1) * 128].rearrange("(k p) c -> p k c", p=128), [], [Tstg])
                OP("pool", "tensor_copy", [Tstg], [Twb], out=wb[:], in_=stg[:])
                for half in range(2):
                    m5[0] += 1
                    pm, Tpm = pM5[m5[0] % 4]
                    for kc in range(16):
                        OP("pe", "matmul", [Twb, Txn2T], [Tpm], out=pm[:], lhsT=wb[:, kc, :], rhs=xn2T[:, kc, half * 512:(half + 1) * 512],
                           start=(kc == 0), stop=(kc == 15))
                    evac(qp[:, half * 512:(half + 1) * 512], pm[:], [Tpm], [Tqp])
                for tile in range(8):
                    ps, Tps = pS5[tile % 2]
                    swt, Tswt = sw[tile % 2]
                    OP("pe", "matmul", [Tqp, TSKT], [Tps], out=ps[:, 0:128], lhsT=qp[:, tile * 128:(tile + 1) * 128], rhs=SKT[:, hc, :], start=True, stop=True)
                    OP("act", "activation", [Tps], [Tswt], out=swt[:], in_=ps[:, 0:128], func=AF.Copy)
                    OP("dve", "max", [Tswt], [Tsv], out=sv[:, tile, hc, 0:8], in_=swt[:])
                    OP("dve", "max_index", [Tswt, Tsv], [Tsi], out=si[:, tile, hc, 0:8], in_max=sv[:, tile, hc, 0:8], in_values=swt[:])
                    OP("dve", "match_replace", [Tswt, Tsv], [Tsw2], out=sw2[:], in_to_replace=sv[:, tile, hc, 0:8], in_values=swt[:], imm_value=-1e30)
                    OP("dve", "max", [Tsw2], [Tsv], out=sv[:, tile, hc, 8:16], in_=sw2[:])
                    OP("dve", "max_index", [Tsw2, Tsv], [Tsi], out=si[:, tile, hc, 8:16], in_max=sv[:, tile, hc, 8:16], in_values=sw2[:])
                if hc % 2 == 1:
                    route_h(hc // 2)
            if stop_after == 5:
                dump("eidx", eidx[:], Teidx, [128, 8, 128], U32)
                dump("gates", gates[:], Tgates, [128, 8, 128], F32)
            P.barrier()
        if stop_after == 5:
            finish()
            return nc, dbg_outs

        with ExitStack() as s5:
            gfin, Tgfin = sb(s5, [128, D], F32, "gfin")
            mixG = mixT[:].rearrange("p a b -> p (a b)").rearrange("p (k c) -> p k c", k=4)
            gb = [(mixG[:, i, :], T("gbm%d" % i)) for i in range(4)]
            gb += [(lambda t: (t[0][:], t[1]))(sb(s5, [128, 2 * D], BF16, "gb")) for _ in range(4)]
            acc, Tacc = sb(s5, [128, D], F32, "acc")
            junk5, Tjunk5 = sb(s5, [128, D], BF16, "junk5")
            aw2 = [sb(s5, [128, 128], F32, "aw") for _ in range(2)]
            ww2 = [sb(s5, [128, 128], F32, "ww") for _ in range(2)]
            t12 = [sb(s5, [128, 2], F32, "t1") for _ in range(2)]
            sq5, Tsq5 = sb(s5, [128, 2], F32, "sq5")
            dg = [sb(s5, [128, 128], BF16, "dg") for _ in range(8)]
            pV, TpV = psb(s5, [128, D], F32, "pV")
            DMA("sp", gfin[:], final_norm_w.partition_broadcast(128), [], [Tgfin], **NCD)
            GS = 2
            NG = 128 // GS
            gi = [0]
            bufs = {}

            def dots(tile, grp):
                a_, Ta_ = aw2[tile % 2]
                lst = []
                for j in range(GS):
                    slot = grp * GS + j
                    gi[0] += 1
                    g_, Tg_ = gb[gi[0] % 8]
                    P.dma("pool", dict(out=g_, out_offset=None, in_=uv16,
                                       in_offset=bass.IndirectOffsetOnAxis(ap=eidx[:, tile, slot:slot + 1], axis=0)),
                          reads=[Teidx, Tuv16], writes=[Tg_], name="indirect_dma_start")
                    OP("dve", "scalar_tensor_tensor", [Tg_, Txn2], [Tjunk5, Ta_], out=junk5[:], in0=g_[:, 0:D], scalar=1.0, in1=xn2[:, tile, :],
                       op0=ALU.mult, op1=ALU.mult, accum_out=a_[:, slot:slot + 1])
                    lst.append((g_, Tg_))
                bufs[(tile, grp)] = lst

            def gelu_pre(tile, grp):
                a_, Ta_ = aw2[tile % 2]
                t1, Tt1 = t12[grp % 2]
                av = a_[:, grp * GS:(grp + 1) * GS]
                OP("dve", "tensor_tensor", [Ta_], [Tt1], out=t1[:], in0=av, in1=av, op=ALU.mult)
                OP("dve", "tensor_tensor", [Tt1, Ta_], [Tt1], out=t1[:], in0=t1[:], in1=av, op=ALU.mult)
                OP("dve", "scalar_tensor_tensor", [Tt1, Ta_], [Tt1], out=t1[:], in0=t1[:], scalar=0.044715, in1=av, op0=ALU.mult, op1=ALU.add)
                OP("act", "activation", [Tt1], [Tt1], out=t1[:], in_=t1[:], func=AF.Tanh, scale=0.7978845608028654)

            def post_v(tile, grp):
                a_, Ta_ = aw2[tile % 2]
                w_, Tw_ = ww2[tile % 2]
                t1, Tt1 = t12[grp % 2]
                sl = slice(grp * GS, (grp + 1) * GS)
                OP("dve", "tensor_scalar", [Tt1], [Tt1], out=t1[:], in0=t1[:], scalar1=0.5, scalar2=0.5, op0=ALU.mult, op1=ALU.add)
                OP("dve", "tensor_tensor", [Tt1, Ta_], [Tt1], out=t1[:], in0=t1[:], in1=a_[:, sl], op=ALU.mult)
                OP("dve", "tensor_tensor", [Tt1, Tgates], [Tw_], out=w_[:, sl], in0=t1[:], in1=gates[:, tile, sl], op=ALU.mult)
                for j, (g_, Tg_) in enumerate(bufs.pop((tile, grp))):
                    slot = grp * GS + j
                    dk, Tdk = dg[slot % 8]
                    OP("act", "activation", [Tident, Tw_], [Tdk], out=dk[:], in_=ident[:], func=AF.Copy, scale=w_[:, slot:slot + 1])
                    for bq in range(4):
                        OP("pe", "matmul", [Tdk, Tg_], [TpV], out=pV[:, bq * 512:(bq + 1) * 512], lhsT=dk[:], rhs=g_[:, D + bq * 512:D + (bq + 1) * 512],
                           start=(slot == 0), stop=(slot == 127))

            def final(tile):
                OP("dve", "tensor_tensor", [TpV, Th2], [Tacc], out=acc[:], in0=pV[:], in1=h2[:, tile, :], op=ALU.add)
                OP("act", "activation", [Tacc], [Tjunk5, Tsq5], out=junk5[:], in_=acc[:], func=AF.Square, accum_out=sq5[:, 0:1])
                rstd_from_sumsq(sq5, Tsq5, D)
                OP("dve", "scalar_tensor_tensor", [Tacc, Tsq5, Tgfin], [Tacc], out=acc[:], in0=acc[:], scalar=sq5[:, 0:1], in1=gfin[:], op0=ALU.mult, op1=ALU.mult)
                DMA("sp", y_out[tile * 128:(tile + 1) * 128, :], acc[:], [Tacc], [Tout])

            prev = None
            for tile in range(8):
                for grp in range(NG):
                    dots(tile, grp)
                    gelu_pre(tile, grp)
                    if prev is not None:
                        post_v(*prev)
                        if prev[1] == NG - 1:
                            final(prev[0])
                    prev = (tile, grp)
            post_v(*prev)
            final(prev[0])
        finish()
        return nc, dbg_outs
        raise NotImplementedError
    return nc, dbg_outs


def _prep_inputs(inputs):
    x = np.asarray(inputs["x"], np.float32)
    cst = _consts()
    shared = {
        "attn_norm_w": np.ascontiguousarray(inputs["attn_norm_w"][0]),
        "w_in": np.ascontiguousarray(inputs["w_in"][0]),
        "w_cmp_k": np.ascontiguousarray(inputs["w_cmp_k"][0]),
        "w_cmp_v": np.ascontiguousarray(inputs["w_cmp_v"][0]),
        "cmp_pos": np.ascontiguousarray(inputs["cmp_pos"][0]),
        "conv_w": np.ascontiguousarray(inputs["conv_w"][0]),
        "conv_b": np.ascontiguousarray(inputs["conv_b"][0]),
        "attn_group_norm_w": np.ascontiguousarray(inputs["attn_group_norm_w"][0]),
        "conv_group_norm_w": np.ascontiguousarray(inputs["conv_group_norm_w"][0]),
        "w_out": np.ascontiguousarray(inputs["w_out"][0]),
        "rel_bias": np.ascontiguousarray(inputs["rel_bias"]),
        "ffn_norm_w": np.ascontiguousarray(inputs["ffn_norm_w"][0]),
        "peer_wq": np.ascontiguousarray(inputs["peer_wq"][0]),
        "peer_subkeys": np.ascontiguousarray(inputs["peer_subkeys"][0]).reshape(16, 128, 128),
        "peer_u": np.ascontiguousarray(inputs["peer_u"][0]),
        "peer_v": np.ascontiguousarray(inputs["peer_v"][0]),
        "final_norm_w": np.ascontiguousarray(inputs["final_norm_w"]),
    }
    shared = {k: np.asarray(v, np.float32) for k, v in shared.items()}
    shared.update(cst)
    in_maps = []
    for core in range(8):
        b, c = core // 4, core % 4
        q0 = 1024 * c
        ws = q0 - 3072
        xw = np.zeros((S, D), np.float32)
        lo = max(ws, 0)
        xw[lo - ws:, :] = x[b, lo:q0 + 1024, :]
        m = dict(shared)
        m["xs"] = xw
        m.update(_core_consts(c))
        in_maps.append(m)
    return in_maps


def kernel(**inputs):
    in_maps = _prep_inputs(inputs)
    nc, _ = build()
    res = run_bass_kernel_spmd(nc, in_maps, core_ids=list(range(8)))
    out = np.zeros((2, S, D), np.float32)
    for core in range(8):
        b, c = core // 4, core % 4
        out[b, 1024 * c:1024 * (c + 1), :] = res.results[core]["y"]
    return out
```

```python
import math
from contextlib import ExitStack
import numpy as np
import ml_dtypes
import concourse.bass as bass
import concourse.mybir as mybir
from concourse.bass_utils import run_bass_kernel_spmd

F32 = mybir.dt.float32
BF16 = mybir.dt.bfloat16
U32 = mybir.dt.uint32
ALU = mybir.AluOpType
AF = mybir.ActivationFunctionType
AX = mybir.AxisListType

D = 2048
S = 4096
NQ = 1024
NEG = -30000.0
EPS = 1e-6
SCALE = 128 ** -0.5
DIN = 5656
MSEG0, MSEG1, MSEG2 = 384, 256, 3072
MTOT = MSEG0 + MSEG1 + MSEG2
NDBG = []


class T:
    __slots__ = ("name", "w", "r", "dsem")

    def __init__(self, name):
        self.name = name
        self.w = {}
        self.r = {}
        self.dsem = None


class Prog:
    ENG = ("pe", "dve", "act", "pool", "sp")

    def __init__(self, nc, stack):
        self.nc = nc
        self.stack = stack
        self.stream = {k: [] for k in self.ENG}
        self.cnt = {}
        self.sems = {}
        self.known = {k: {} for k in self.ENG}
        for k in self.ENG:
            self.sems[k] = stack.enter_context(nc.semaphore("s_" + k))
            self.cnt[k] = 0
        self.ndsem = 0

    def _dsem(self, t):
        if t.dsem is None:
            key = "d%d" % self.ndsem
            self.ndsem += 1
            self.sems[key] = self.stack.enter_context(self.nc.semaphore(key))
            self.cnt[key] = 0
            t.dsem = key
        return t.dsem

    def _waits(self, eng, reads, writes, nowaw=False):
        need = {}

        def add(k, v):
            if v > need.get(k, 0):
                need[k] = v
        for t in reads:
            for k, v in t.w.items():
                add(k, v)
        for t in writes:
            if not nowaw:
                for k, v in t.w.items():
                    add(k, v)
            for k, v in t.r.items():
                add(k, v)
        kn = self.known[eng]
        out = []
        for k, v in need.items():
            if k == eng and eng == "pe":
                continue
            if kn.get(k, 0) < v:
                kn[k] = v
                out.append((k, v))
        return out

    def op(self, eng, name, kw, reads=(), writes=()):
        waits = self._waits(eng, reads, writes)
        self.cnt[eng] += 1
        v = self.cnt[eng]
        self.stream[eng].append((waits, (name, kw), eng, 1))
        for t in reads:
            t.r[eng] = v
        for t in writes:
            t.w[eng] = v

    def dma(self, eng, kw, reads=(), writes=(), nowaw=False, name="dma_start"):
        assert len(writes) == 1
        t = writes[0]
        key = self._dsem(t)
        waits = self._waits(eng, reads, writes, nowaw=nowaw)
        self.cnt[key] += 16
        v = self.cnt[key]
        self.stream[eng].append((waits, (name, kw), key, 16))
        for s in reads:
            s.r[key] = v
        t.w[key] = v

    def barrier(self):
        snap = dict(self.cnt)
        for e in self.ENG:
            kn = self.known[e]
            waits = []
            for k, v in snap.items():
                if v > 0 and kn.get(k, 0) < v:
                    kn[k] = v
                    waits.append((k, v))
            self.stream[e].append((waits, None, None, 0))

    def final_wait(self, eng, tiles):
        waits = self._waits(eng, tiles, ())
        self.stream[eng].append((waits, None, None, 0))

    def emit(self):
        nc = self.nc
        with nc.Block() as block:
            def mk(name):
                def body(e):
                    for waits, fn, key, inc in self.stream[name]:
                        for k, v in waits:
                            e.wait_ge(self.sems[k], v)
                        if fn is not None:
                            getattr(e, fn[0])(**fn[1]).then_inc(self.sems[key], inc)
                return body
            block.tensor(mk("pe"))
            block.vector(mk("dve"))
            block.scalar(mk("act"))
            block.gpsimd(mk("pool"))
            block.sync(mk("sp"))


def _t5_bucket_np(n):
    n = np.maximum(n, 0)
    nf = np.maximum(n, 1).astype(np.float32)
    large = 16 + (np.log(nf / np.float32(16)) / np.float32(math.log(8.0)) * np.float32(16)).astype(np.int32)
    large = np.minimum(large, 31)
    return np.where(n < 16, n, large)


def _consts():
    oh = np.zeros((33, MTOT), np.float32)
    m = np.arange(MSEG0)
    d = m - 127
    bk = _t5_bucket_np(d)
    for i in range(MSEG0):
        if d[i] < 0:
            oh[32, i] = NEG
        else:
            oh[bk[i], i] = 1.0
    for i in range(MSEG1):
        if i - 127 < 0:
            oh[31, MSEG0 + i] = 1.0
        else:
            oh[32, MSEG0 + i] = NEG
    m = np.arange(MSEG2)
    d = m - 1023
    bk = _t5_bucket_np(d)
    base = MSEG0 + MSEG1
    for i in range(MSEG2):
        if d[i] < 0:
            oh[32, base + i] = NEG
        else:
            oh[bk[i], base + i] = 1.0
    ident = np.eye(128, dtype=np.float32)
    jmat = ident[::-1].copy()
    eexp = np.zeros((64, 4096), np.float32)
    eexp[np.arange(4096) // 64, np.arange(4096)] = 1.0
    ov = np.zeros((256, 64), np.float32)
    for n in range(255):
        pos = n * 16 + np.arange(32)
        for p in pos:
            ov[n + 1, p // 64] += 1.0 / 32
    ovs = ov.reshape(2, 128, 64).transpose(1, 0, 2).copy()
    iota16 = np.tile(np.arange(16, dtype=np.float32)[None, :], (128, 1))
    return dict(iota16=iota16, oh33=oh, ident=ident, jmat=jmat, identb=ident.astype(ml_dtypes.bfloat16),
                eexp=eexp.astype(ml_dtypes.bfloat16), ovs=ovs)


def _core_consts(c):
    q0 = 1024 * c
    ws = q0 - 3072
    kvalid = np.zeros((128, 32), np.float32)
    for s in range(32):
        if ws + 128 * s < 0:
            kvalid[:, s] = NEG
    cvalid = np.zeros((128, 2), np.float32)
    for tl in range(2):
        for p in range(128):
            n_rel = tl * 128 + p - 1
            n_abs = n_rel + ws // 16
            if n_rel < 0 or n_abs < 0:
                cvalid[p, tl] = NEG
    t_abs = q0 + np.arange(1024)
    j_abs = np.arange(64) + ws // 64
    exists = j_abs >= 0
    causal = exists[None, :] & (j_abs[None, :] * 64 <= t_abs[:, None])
    blk_t = t_abs // 64
    forced = exists[None, :] & ((j_abs[None, :] == 0) | (j_abs[None, :] == blk_t[:, None]) | (j_abs[None, :] == blk_t[:, None] - 1))
    cbnf = (causal & ~forced).astype(np.float32)
    sadd = np.where(forced, 5.0, np.where(causal, 0.0, -1.0)).astype(np.float32)
    cb = causal.astype(np.float32)
    f = lambda a: a.reshape(8, 128, 64).transpose(1, 0, 2).copy()
    return dict(kvalid=kvalid, cvalid=cvalid, cbnf=f(cbnf), sadd=f(sadd), cb=f(cb))


def build(stop_after=None, dbg=False):
    nc = bass.Bass("TRN2", target_bir_lowering=False)
    din = lambda n, shp, dt=F32: nc.dram_tensor(n, list(shp), dt, kind="ExternalInput").ap()
    xs = din("xs", [S, D])
    attn_norm_w = din("attn_norm_w", [D])
    w_in = din("w_in", [D, DIN])
    w_cmp_k = din("w_cmp_k", [32, 128, 128])
    w_cmp_v = din("w_cmp_v", [32, 128, 128])
    cmp_pos = din("cmp_pos", [32, 128])
    conv_w = din("conv_w", [3, 1024])
    conv_b = din("conv_b", [1024])
    agnw = din("attn_group_norm_w", [1024])
    cgnw = din("conv_group_norm_w", [1024])
    w_out = din("w_out", [D, D])
    rel_bias = din("rel_bias", [32, 8])
    ffn_norm_w = din("ffn_norm_w", [D])
    peer_wq = din("peer_wq", [D, D])
    peer_sk = din("peer_subkeys", [16, 128, 128])
    peer_u = din("peer_u", [16384, D])
    peer_v = din("peer_v", [16384, D])
    final_norm_w = din("final_norm_w", [D])
    oh33_d = din("oh33", [33, MTOT])
    ident_d = din("ident", [128, 128])
    jmat_d = din("jmat", [128, 128])
    identb_d = din("identb", [128, 128], BF16)
    eexp_d = din("eexp", [64, 4096], BF16)
    ovs_d = din("ovs", [128, 2, 64])
    kvalid_d = din("kvalid", [128, 32])
    cvalid_d = din("cvalid", [128, 2])
    cbnf_d = din("cbnf", [128, 8, 64])
    sadd_d = din("sadd", [128, 8, 64])
    cb_d = din("cb", [128, 8, 64])
    iota16_d = din("iota16", [128, 16])
    y_out = nc.dram_tensor("y", [NQ, D], F32, kind="ExternalOutput").ap()
    gtd = nc.dram_tensor("gtd", [8, MTOT], F32, kind="Internal").ap()
    uv16 = nc.dram_tensor("uv16", [16384, 2 * D], BF16, kind="Internal").ap()
    Tuv16 = T("uv16")
    dbg_outs = {}

    with ExitStack() as st:
        P = Prog(nc, st)
        ntile = [0]

        def sb(stk, shape, dt=F32, name=None):
            ntile[0] += 1
            nm = (name or "t") + "_%d" % ntile[0]
            return stk.enter_context(nc.sbuf_tensor(nm, list(shape), dt)), T(nm)

        def psb(stk, shape, dt=F32, name=None):
            ntile[0] += 1
            nm = (name or "p") + "_%d" % ntile[0]
            return stk.enter_context(nc.psum_tensor(nm, list(shape), dt)), T(nm)

        Tout = T("y_out")

        def dump(name, ap, t, shape, dt=F32):
            if not dbg:
                return
            o = nc.dram_tensor("dbg_" + name, list(shape), dt, kind="ExternalOutput").ap()
            to = T("dbg_" + name)
            dbg_outs[name] = to
            P.dma("sp", dict(out=o, in_=ap), reads=[t], writes=[to])

        def finish():
            P.final_wait("sp", [Tout] + list(dbg_outs.values()))
            P.emit()

        def OP(eng, name, reads, writes, **kw):
            P.op(eng, name, kw, reads, writes)

        def DMA(eng, out, in_, reads, writes, **kw):
            P.dma(eng, dict(out=out, in_=in_, **kw), reads, writes)

        evac_rr = [0]

        def evac(out_ap, in_ap, reads, writes):
            evac_rr[0] += 1
            if evac_rr[0] % 2:
                OP("act", "activation", reads, writes, out=out_ap, in_=in_ap, func=AF.Copy)
            else:
                OP("dve", "tensor_copy", reads, writes, out=out_ap, in_=in_ap)

        ident, Tident = sb(st, [128, 128], F32, "ident")
        identb, Tidentb = sb(st, [128, 128], BF16, "identb")
        jmat, Tjmat = sb(st, [128, 128], F32, "jmat")
        onesf, Tonesf = sb(st, [128, 128], F32, "onesf")
        gcol, Tgcol = sb(st, [128, 16], F32, "gcol")
        chcol, Tchcol = sb(st, [128, 8], F32, "chcol")
        kvalid, Tkvalid = sb(st, [128, 32], F32, "kvalid")
        cvalid, Tcvalid = sb(st, [128, 2], F32, "cvalid")
        ccol0, Tccol0 = sb(st, [128, 8], F32, "ccol0")
        cw, Tcw = sb(st, [128, 3, 8], F32, "cw")
        cbias, Tcbias = sb(st, [128, 8], F32, "cbias")
        agw, Tagw = sb(st, [128, 8], F32, "agw")
        cgw, Tcgw = sb(st, [128, 8], F32, "cgw")
        DMA("sp", ident[:], ident_d, [], [Tident])
        DMA("sp", identb[:], identb_d, [], [Tidentb])
        DMA("sp", jmat[:], jmat_d, [], [Tjmat])
        DMA("sp", kvalid[:], kvalid_d, [], [Tkvalid])
        DMA("sp", cvalid[:], cvalid_d, [], [Tcvalid])
        OP("dve", "memset", [], [Tonesf], ap=onesf[:], constant=1.0)
        NCD = dict(allow_slow_non_contiguous=True)
        DMA("sp", gcol[:], attn_norm_w.rearrange("(k p) -> p k", p=128), [], [Tgcol], **NCD)
        for wi in range(3):
            DMA("sp", cw[:, wi, :], conv_w[wi].rearrange("(c p) -> p c", p=128), [], [Tcw], **NCD)
        DMA("sp", cbias[:], conv_b.rearrange("(c p) -> p c", p=128), [], [Tcbias], **NCD)
        DMA("sp", agw[:], agnw.rearrange("(c p) -> p c", p=128), [], [Tagw], **NCD)
        DMA("sp", cgw[:], cgnw.rearrange("(c p) -> p c", p=128), [], [Tcgw], **NCD)
        DMA("sp", chcol[:], rel_bias[31:32, :].partition_broadcast(128), [], [Tchcol], **NCD)
        OP("dve", "tensor_scalar", [Tchcol, Tcvalid], [Tccol0], out=ccol0[:], in0=chcol[:], scalar1=cvalid[:, 0:1],
           scalar2=None, op0=ALU.add)

        mixT, TmixT = sb(st, [128, 16, NQ], BF16, "mixT")
        gsig, Tgsig = sb(st, [128, 8, 24], F32, "gsig")
        mixF = mixT[:].rearrange("p a b -> p (a b)").bitcast(F32)
        sA = ExitStack()
        ksT, TksT = sb(sA, [128, 2, S], BF16, "ksT")
        vs, Tvs = sb(sA, [128, 32, 2, 130], BF16, "vs")
        kwT, TkwT = sb(sA, [128, 2, 1536], BF16, "kwT")
        vw, Tvw = sb(sA, [128, 12, 2, 130], BF16, "vw")
        kcT, TkcT = sb(sA, [128, 2, 256], BF16, "kcT")
        vcaug, Tvcaug = sb(sA, [128, 2, 2, 194], F32, "vcaug")
        xnT, _ = sb(sA, [128, 2, 16, 512], BF16, "xnT")
        xnTA, TxnTA = xnT[:, 0], T("xnTA")
        xnTB, TxnTB = xnT[:, 1], T("xnTB")
        xnTh, TxnTh = sb(sA, [128, 16, 2], BF16, "xnTh")
        OP("pool", "memset", [], [Tvs], ap=vs[:], constant=1.0)
        OP("pool", "memset", [], [Tvw], ap=vw[:], constant=1.0)
        OP("pool", "memset", [], [Tvcaug], ap=vcaug[:], constant=0.0)
        OP("pool", "memset", [], [Tvcaug], ap=vcaug[:, :, :, 128:129], constant=1.0)
        for tl in range(2):
            for kvh in range(2):
                DMA("sp", vcaug[:, tl, kvh, 130:194], ovs_d[:, tl, :], [], [Tvcaug])

        def rstd_from_sumsq(sqt, Tsq, n):
            OP("dve", "tensor_scalar", [Tsq], [Tsq], out=sqt[:, 1:2], in0=sqt[:, 0:1], scalar1=1.0 / n, scalar2=EPS,
               op0=ALU.mult, op1=ALU.add)
            OP("act", "activation", [Tsq], [Tsq], out=sqt[:, 1:2], in_=sqt[:, 1:2], func=AF.Sqrt)
            OP("dve", "reciprocal", [Tsq], [Tsq], out=sqt[:, 0:1], in_=sqt[:, 1:2])

        with ExitStack() as s1:
            wkv, Twkv = sb(s1, [128, 16, 1536], BF16, "wkv")
            wck, Twck = sb(s1, [128, 32, 128], BF16, "wck")
            wcv, Twcv = sb(s1, [128, 32, 128], BF16, "wcv")
            cposT, TcposT = sb(s1, [128, 32], BF16, "cposT")
            constk, Tconstk = sb(s1, [128, 2], F32, "constk")
            kcvbuf, Tkcv = sb(s1, [128, 4, 528], BF16, "kcvbuf")
            vcT, TvcT = sb(s1, [128, 2, 256], F32, "vcT")
            xst = [(mixF[:, 4096 + i * 2048:4096 + (i + 1) * 2048], T("xst%d" % i)) for i in range(2)]
            xnb = [sb(s1, [128, D], BF16, "xnb") for _ in range(2)]
            junk, Tjunk = sb(s1, [128, D], BF16, "junk")
            sq = [sb(s1, [128, 2], F32, "sq") for _ in range(2)]
            wst = [(mixF[:, i * 2048:(i + 1) * 2048].rearrange("p (k c) -> p k c", k=16), T("wst%d" % i)) for i in range(2)]
            pT = [psb(s1, [128, 8, 128], BF16, "pT") for _ in range(2)]
            pM = [psb(s1, [128, 512], F32, "pM") for _ in range(4)]
            pC, TpC = psb(s1, [128, 512], F32, "pC")

            for ci in range(12):
                stg, Tstg = wst[ci % 2]
                c0 = 1024 + ci * 128
                DMA("sp", stg, w_in[:, c0:c0 + 128].rearrange("(k p) c -> p k c", p=128), [], [Tstg])
                OP("pool", "tensor_tensor", [Tstg, Tgcol], [Twkv], out=wkv[:, :, ci * 128:(ci + 1) * 128], in0=stg,
                   in1=gcol[:].unsqueeze(2).to_broadcast([128, 16, 128]), op=ALU.mult)
            DMA("pool", wck[:], w_cmp_k.rearrange("l d e -> d l e"), [], [Twck])
            DMA("pool", wcv[:], w_cmp_v.rearrange("l d e -> d l e"), [], [Twcv])
            DMA("pool", cposT[:], cmp_pos.rearrange("l d -> d l"), [], [TcposT], **NCD)
            OP("dve", "memset", [], [Tkcv], ap=kcvbuf[:], constant=0.0)
            for ti, (wc, Twc) in enumerate(((wck, Twck), (wcv, Twcv))):
                for l in range(32):
                    OP("pe", "matmul", [Twc, TcposT], [TpC], out=pC[:, ti:ti + 1], lhsT=wc[:, l, :], rhs=cposT[:, l:l + 1],
                       start=(l == 0), stop=(l == 31))
            OP("dve", "tensor_copy", [TpC], [Tconstk], out=constk[:], in_=pC[:, 0:2])

            xcount = [0]

            def norm_transpose(row0, dst, Tdst, col0):
                i = xcount[0] % 2
                xcount[0] += 1
                (xt, Txt), (xb, Txb), (sqt, Tsq) = xst[i], xnb[i], sq[i]
                DMA("sp", xt, xs[row0:row0 + 128, :], [], [Txt])
                OP("act", "activation", [Txt], [Tjunk, Tsq], out=junk[:], in_=xt, func=AF.Square, accum_out=sqt[:, 0:1])
                rstd_from_sumsq(sqt, Tsq, D)
                OP("dve", "tensor_scalar", [Txt, Tsq], [Txb], out=xb[:], in0=xt, scalar1=sqt[:, 0:1], scalar2=None, op0=ALU.mult)
                for half in range(2):
                    pt, Tpt = pT[half]
                    for k8 in range(8):
                        kc = half * 8 + k8
                        OP("pe", "transpose", [Txb, Tidentb], [Tpt], out=pt[:, k8, :], in_=xb[:, kc * 128:(kc + 1) * 128],
                           identity=identb[:])
                    evac(dst[:, half * 8:(half + 1) * 8, col0:col0 + 128], pt[:], [Tpt], [Tdst])

            mrr = [0]

            def nextpm():
                mrr[0] += 1
                return pM[mrr[0] % 4]

            def NT(g):
                xg, Txg = (xnTA, TxnTA) if g % 2 == 0 else (xnTB, TxnTB)
                for j in range(4):
                    norm_transpose((4 * g + j) * 128, xg, Txg, j * 128)
                if g == 5:
                    OP("dve", "tensor_copy", [TxnTB], [TxnTh], out=xnTh[:], in_=xnTB[:, :, 510:512])

            NT(0)
            for g in range(8):
                xg, Txg = (xnTA, TxnTA) if g % 2 == 0 else (xnTB, TxnTB)
                if g < 7:
                    NT(g + 1)
                fm = [(0, kcvbuf[:, 0, 16:528], Tkcv), (128, kcvbuf[:, 1, 16:528], Tkcv),
                      (256, kcvbuf[:, 2, 16:528], Tkcv), (384, kcvbuf[:, 3, 16:528], Tkcv),
                      (512, ksT[:, 0, g * 512:(g + 1) * 512], TksT), (640, ksT[:, 1, g * 512:(g + 1) * 512], TksT)]
                if g >= 5:
                    fm += [(1024, kwT[:, 0, (g - 5) * 512:(g - 4) * 512], TkwT),
                           (1152, kwT[:, 1, (g - 5) * 512:(g - 4) * 512], TkwT)]
                for (co, dst, Td) in fm:
                    pm, Tpm = nextpm()
                    for kc in range(16):
                        OP("pe", "matmul", [Twkv, Txg], [Tpm], out=pm[:], lhsT=wkv[:, kc, co:co + 128], rhs=xg[:, kc, :],
                           start=(kc == 0), stop=(kc == 15))
                    evac(dst, pm[:], [Tpm], [Td])
                for j in range(4):
                    slot = 4 * g + j
                    pm, Tpm = nextpm()
                    for kc in range(16):
                        OP("pe", "matmul", [Twkv, Txg], [Tpm], out=pm[:, 0:256], lhsT=xg[:, kc, j * 128:(j + 1) * 128],
                           rhs=wkv[:, kc, 768:1024], start=(kc == 0), stop=(kc == 15))
                    evac(vs[:, slot, :, 0:128], pm[:, 0:256].rearrange("p (a b) -> p a b", a=2), [Tpm], [Tvs])
                    if g >= 5:
                        pm, Tpm = nextpm()
                        for kc in range(16):
                            OP("pe", "matmul", [Twkv, Txg], [Tpm], out=pm[:, 0:256], lhsT=xg[:, kc, j * 128:(j + 1) * 128],
                               rhs=wkv[:, kc, 1280:1536], start=(kc == 0), stop=(kc == 15))
                        evac(vw[:, slot - 20, :, 0:128], pm[:, 0:256].rearrange("p (a b) -> p a b", a=2), [Tpm], [Tvw])
                for ti in range(2):
                    wc, Twc = (wck, Twck) if ti == 0 else (wcv, Twcv)
                    for l in range(32):
                        OP("pe", "matmul", [Twc, Tkcv], [TpC], out=pC[:, 64 * ti:64 * ti + 64].rearrange("p (a b) -> p a b", a=2), lhsT=wc[:, l, :],
                           rhs=kcvbuf[:, 2 * ti:2 * ti + 2, l:l + 497:16], start=(l == 0), stop=(l == 31))
                for kvh in range(2):
                    OP("dve", "tensor_scalar", [TpC, Tconstk], [TkcT], out=kcT[:, kvh, 32 * g:32 * g + 32],
                       in0=pC[:, 32 * kvh:32 * kvh + 32], scalar1=constk[:, 0:1], scalar2=None, op0=ALU.add)
                    OP("dve", "tensor_scalar", [TpC, Tconstk], [TvcT], out=vcT[:, kvh, 32 * g:32 * g + 32],
                       in0=pC[:, 64 + 32 * kvh:96 + 32 * kvh], scalar1=constk[:, 1:2], scalar2=None, op0=ALU.add)
                OP("dve", "tensor_copy", [Tkcv], [Tkcv], out=kcvbuf[:, :, 0:16], in_=kcvbuf[:, :, 512:528])
            for tl in range(2):
                for kvh in range(2):
                    pm, Tpm = nextpm()
                    OP("pe", "transpose", [TvcT, Tident], [Tpm], out=pm[:, 0:128], in_=vcT[:, kvh, tl * 128:(tl + 1) * 128],
                       identity=ident[:])
                    OP("dve", "tensor_copy", [Tpm], [Tvcaug], out=vcaug[:, tl, kvh, 0:128], in_=pm[:, 0:128])
            if stop_after == 1:
                dump("ksT", ksT[:], TksT, [128, 2, S], BF16)
                dump("vs", vs[:], Tvs, [128, 32, 2, 130], BF16)
                dump("kwT", kwT[:], TkwT, [128, 2, 1536], BF16)
                dump("kcT", kcT[:], TkcT, [128, 2, 256], BF16)
                dump("vcaug", vcaug[:], Tvcaug, [128, 2, 2, 194], F32)
            P.barrier()
        if stop_after == 1:
            finish()
            return nc, dbg_outs

        qT, TqT = sb(sA, [128, 8, NQ], BF16, "qT")
        xh = [(xnTA, TxnTA), (xnTB, TxnTB)]
        with ExitStack() as s2:
            wst2 = [sb(s2, [128, 16, 128], F32, "wst2") for _ in range(2)]
            wb2 = [sb(s2, [128, 16, 128], BF16, "wb2") for _ in range(4)]
            bT, TbT = sb(s2, [128, 1026], F32, "bT")
            cT, TcT = sb(s2, [128, 1026], F32, "cT")
            hT, ThT = sb(s2, [128, 1026], F32, "hT")
            uu, Tuu = sb(s2, [128, 1026], F32, "uu")
            yy, Tyy = sb(s2, [128, 1024], F32, "yy")
            oo, Too = sb(s2, [128, 1024], F32, "oo")
            osq, Tosq = sb(s2, [128, 1024], F32, "osq")
            rs, Trs = sb(s2, [128, 1024], F32, "rs")
            pM2 = [psb(s2, [128, 512], F32, "pM2") for _ in range(4)]
            pH, TpH = psb(s2, [128, 512], F32, "pH")
            wrr = [0]
            mr2 = [0]

            def nextpm2():
                mr2[0] += 1
                return pM2[mr2[0] % 4]

            def load_w(c0, width=128):
                i = wrr[0]
                wrr[0] += 1
                stg, Tstg = wst2[i % 2]
                wb, Twb = wb2[i % 4]
                DMA("sp", stg[:, :, 0:width], w_in[:, c0:c0 + width].rearrange("(k p) c -> p k c", p=128), [], [Tstg])
                OP("pool", "tensor_tensor", [Tstg, Tgcol], [Twb], out=wb[:, :, 0:width], in0=stg[:, :, 0:width],
                   in1=gcol[:].unsqueeze(2).to_broadcast([128, 16, width]), op=ALU.mult)
                return wb, Twb

            def proj_fm(wb, Twb, dst_fn, Tdst):
                for half in range(2):
                    xg, Txg = xh[half]
                    pm, Tpm = nextpm2()
                    for kc in range(16):
                        OP("pe", "matmul", [Twb, Txg], [Tpm], out=pm[:], lhsT=wb[:, kc, :], rhs=xg[:, kc, :],
                           start=(kc == 0), stop=(kc == 15))
                    evac(dst_fn(half), pm[:], [Tpm], [Tdst])

            for h in range(8):
                wb, Twb = load_w(h * 128)
                proj_fm(wb, Twb, lambda half, h=h: qT[:, h, half * 512:(half + 1) * 512], TqT)
            wg, Twg = load_w(2560, 24)
            for tile in range(8):
                xg, Txg = xh[tile // 4]
                cs = (tile % 4) * 128
                pm, Tpm = nextpm2()
                for kc in range(16):
                    OP("pe", "matmul", [Twg, Txg], [Tpm], out=pm[:, 0:24], lhsT=xg[:, kc, cs:cs + 128], rhs=wg[:, kc, 0:24],
                       start=(kc == 0), stop=(kc == 15))
                OP("act", "activation", [Tpm], [Tgsig], out=gsig[:, tile, :], in_=pm[:, 0:24], func=AF.Sigmoid)
            for ch in range(8):
                wb_b, Twb_b = load_w(2584 + ch * 128)
                wb_c, Twb_c = load_w(3608 + ch * 128)
                wb_h, Twb_h = load_w(4632 + ch * 128)
                for (wb, Twb, dT, TdT) in ((wb_b, Twb_b, bT, TbT), (wb_c, Twb_c, cT, TcT), (wb_h, Twb_h, hT, ThT)):
                    proj_fm(wb, Twb, lambda half, dT=dT: dT[:, 2 + half * 512:2 + (half + 1) * 512], TdT)
                for hi, (wb, Twb, dT, TdT) in enumerate(((wb_c, Twb_c, cT, TcT), (wb_h, Twb_h, hT, ThT))):
                    for kc in range(16):
                        OP("pe", "matmul", [Twb, TxnTh], [TpH], out=pH[:, 2 * hi:2 * hi + 2], lhsT=wb[:, kc, :], rhs=xnTh[:, kc, :],
                           start=(kc == 0), stop=(kc == 15))
                    OP("dve", "tensor_copy", [TpH], [TdT], out=dT[:, 0:2], in_=pH[:, 2 * hi:2 * hi + 2])
                OP("dve", "tensor_tensor", [TcT, ThT], [Tuu], out=uu[:], in0=cT[:], in1=hT[:], op=ALU.mult)
                OP("dve", "tensor_scalar", [Tuu, Tcw, Tcbias], [Tyy], out=yy[:], in0=uu[:, 2:1026], scalar1=cw[:, 2, ch:ch + 1],
                   scalar2=cbias[:, ch:ch + 1], op0=ALU.mult, op1=ALU.add)
                OP("dve", "scalar_tensor_tensor", [Tuu, Tcw, Tyy], [Tyy], out=yy[:], in0=uu[:, 1:1025], scalar=cw[:, 1, ch:ch + 1],
                   in1=yy[:], op0=ALU.mult, op1=ALU.add)
                OP("dve", "scalar_tensor_tensor", [Tuu, Tcw, Tyy], [Tyy], out=yy[:], in0=uu[:, 0:1024], scalar=cw[:, 0, ch:ch + 1],
                   in1=yy[:], op0=ALU.mult, op1=ALU.add)
                OP("dve", "tensor_tensor", [TbT, Tyy], [Too], out=oo[:], in0=bT[:, 2:1026], in1=yy[:], op=ALU.mult)
                OP("pool", "tensor_tensor", [Too], [Tosq], out=osq[:], in0=oo[:], in1=oo[:], op=ALU.mult)
                for half in range(2):
                    pm, Tpm = nextpm2()
                    OP("pe", "matmul", [Tonesf, Tosq], [Tpm], out=pm[:], lhsT=onesf[:], rhs=osq[:, half * 512:(half + 1) * 512],
                       start=True, stop=True)
                    OP("dve", "tensor_scalar", [Tpm], [Trs], out=rs[:, half * 512:(half + 1) * 512], in0=pm[:], scalar1=1.0 / 128,
                       scalar2=EPS, op0=ALU.mult, op1=ALU.add)
                OP("act", "activation", [Trs], [Trs], out=rs[:], in_=rs[:], func=AF.Sqrt)
                OP("dve", "reciprocal", [Trs], [Trs], out=rs[:], in_=rs[:])
                OP("dve", "scalar_tensor_tensor", [Too, Tcgw, Trs], [TmixT], out=mixT[:, 8 + ch, :], in0=oo[:], scalar=cgw[:, ch:ch + 1],
                   in1=rs[:], op0=ALU.mult, op1=ALU.mult)
            if stop_after == 2:
                dump("qT", qT[:], TqT, [128, 8, NQ], BF16)
                dump("gsig", gsig[:], Tgsig, [128, 8, 24], F32)
                dump("mixc", mixT[:, 8:16, :], TmixT, [128, 8, NQ], BF16)
            P.barrier()
        if stop_after == 2:
            finish()
            return nc, dbg_outs
        with ExitStack() as s3:
            BT0, TBT0 = sb(s3, [128, 8, 128], F32, "BT0")
            BT1, TBT1 = sb(s3, [128, 8, 128], F32, "BT1")
            BT4, TBT4 = sb(s3, [128, 8, 128], F32, "BT4")
            BTC, TBTC = sb(s3, [128, 8, 128], F32, "BTC")
            CB, TCB = xnT[:].rearrange("p a b c -> p (a b c)").bitcast(F32).rearrange("p (h t) -> p h t", h=8), T("CB")
            eexp, Teexp = sb(s3, [128, 4096], BF16, "eexp")
            cbnf, Tcbnf = sb(s3, [128, 8, 64], F32, "cbnf")
            sadd, Tsadd = sb(s3, [128, 8, 64], F32, "sadd")
            cbm, Tcbm = sb(s3, [128, 8, 64], F32, "cbm")
            PT = [sb(s3, [128, 512], BF16, "PT") for _ in range(4)]
            ec, Tec = sb(s3, [128, 2, 512], F32, "ec")
            tmpS = [sb(s3, [128, 512], F32, "tmpS") for _ in range(2)]
            ocmp2 = [sb(s3, [128, 4, 194], F32, "ocmp") for _ in range(2)]
            osel, Tosel = sb(s3, [128, 4, 130], F32, "osel")
            owin, Towin = sb(s3, [128, 4, 130], F32, "owin")
            sm, Tsm = sb(s3, [128, 64], F32, "sm")
            imp, Timp = sb(s3, [128, 64], F32, "imp")
            score, Tscore = sb(s3, [128, 64], F32, "score")
            work, Twork = sb(s3, [128, 64], F32, "work")
            top8, Ttop8 = sb(s3, [128, 16], F32, "top8")
            negm, Tnegm = sb(s3, [128, 64], F32, "negm")
            negT = [sb(s3, [128, 128], BF16, "negT") for _ in range(2)]
            oc, Toc = sb(s3, [128, 4, 128], F32, "oc")
            on, Ton = sb(s3, [128, 4, 128], F32, "on")
            jk, Tjk = sb(s3, [128, 128], F32, "jk")
            pS = [psb(s3, [128, 512], F32, "pS") for _ in range(2)]
            pO = [psb(s3, [128, 512], F32, "pO") for _ in range(4)]
            pX = [psb(s3, [128, 512], F32, "pX") for _ in range(2)]
            Tgtd = T("gtd")
            if dbg:
                obr, Tobr = sb(s3, [128, 4, 128], F32, "obr")
            s3a = ExitStack()
            rb33, Trb33 = sb(s3a, [33, 8], F32, "rb33")
            rb31, Trb31 = sb(s3a, [8, 1], F32, "rb31")
            DMA("sp", rb31[:], rel_bias[31:32, :].rearrange("a h -> h a"), [], [Trb31], **NCD)
            oh33c = [sb(s3a, [33, 512], F32, "oh33c") for _ in range(2)]
            gtsc = [sb(s3a, [8, 512], F32, "gtsc") for _ in range(2)]
            hkc, Thkc = sb(s3a, [128, NQ], F32, "hkc")
            hk, Thk = hkc[:].rearrange("p (a b) -> p a b", a=8), Thkc

            OP("pool", "memset", [], [Teexp], ap=eexp[64:128, :], constant=0.0)
            DMA("sp", eexp[0:64, :], eexp_d, [], [Teexp])
            for (nT_, TnT_) in negT:
                OP("pool", "memset", [], [TnT_], ap=nT_[:], constant=0.0)
            DMA("sp", cbnf[:], cbnf_d, [], [Tcbnf])
            DMA("sp", sadd[:], sadd_d, [], [Tsadd])
            DMA("sp", cbm[:], cb_d, [], [Tcbm])
            OP("dve", "memset", [], [Trb33], ap=rb33[32:33, :], constant=1.0)
            DMA("sp", rb33[0:32, :], rel_bias, [], [Trb33])
            for c0 in range(0, MTOT, 512):
                wd = min(512, MTOT - c0)
                px, Tpx = pX[(c0 // 512) % 2]
                oh33, Toh33 = oh33c[(c0 // 512) % 2]
                gts, Tgts = gtsc[(c0 // 512) % 2]
                DMA("sp", oh33[:, 0:wd], oh33_d[:, c0:c0 + wd], [], [Toh33])
                OP("pe", "matmul", [Trb33, Toh33], [Tpx], out=px[0:8, 0:wd], lhsT=rb33[:, :], rhs=oh33[:, 0:wd], start=True, stop=True)
                OP("dve", "tensor_scalar", [Tpx, Trb31], [Tgts], out=gts[:, 0:wd], in0=px[0:8, 0:wd], scalar1=rb31[:, 0:1], scalar2=None, op0=ALU.subtract)
                DMA("sp", gtd[:, c0:c0 + wd], gts[:, 0:wd], [Tgts], [Tgtd])

            def hankel(off, pstep, ncol, heads=True):
                if heads:
                    return bass.AP(tensor=gtd.tensor, offset=off, ap=[[pstep, 128], [MTOT, 8], [1, ncol]])
                return bass.AP(tensor=gtd.tensor, offset=off, ap=[[pstep, 128], [1, ncol]])

            for (BT, TBT, off) in ((BT0, TBT0, 0), (BT1, TBT1, 128), (BT4, TBT4, MSEG0)):
                DMA("sp", hk, hankel(off, 1, 128), [Tgtd], [Thk])
                hkf = hkc
                BTf = BT[:].rearrange("p a b -> p (a b)")
                for half in range(2):
                    px, Tpx = pX[half]
                    OP("pe", "matmul", [Tjmat, Thk], [Tpx], out=px[:], lhsT=jmat[:], rhs=hkf[:, half * 512:(half + 1) * 512], start=True, stop=True)
                    OP("dve", "tensor_copy", [Tpx], [TBT], out=BTf[:, half * 512:(half + 1) * 512], in_=px[:])
            for h in range(8):
                DMA("sp", hkc[:], hankel(h * MTOT + MSEG0 + MSEG1, 16, NQ, heads=False), [Tgtd], [Thkc])
                for half in range(2):
                    px, Tpx = pX[half]
                    OP("pe", "matmul", [Tjmat, Thkc], [Tpx], out=px[:], lhsT=jmat[:], rhs=hkc[:, half * 512:(half + 1) * 512], start=True, stop=True)
                    OP("dve", "tensor_copy", [Tpx], [TCB], out=CB[:, h, half * 512:(half + 1) * 512], in_=px[:])
            OP("dve", "tensor_copy", [Tchcol], [TBTC], out=BTC[:], in_=chcol[:].unsqueeze(2).to_broadcast([128, 8, 128]))

            P.barrier()
            s3a.close()
            for (tab, c0) in ((peer_u, 0), (peer_v, D)):
                for r0 in range(0, 16384, 1024):
                    P.dma("pool", dict(out=uv16[r0:r0 + 1024, c0:c0 + D], in_=tab[r0:r0 + 1024, :]), reads=[], writes=[Tuv16], nowaw=True)
            srr = [0]
            prr = [0]
            trr = [0]
            nrr = [0]

            def nextS():
                srr[0] += 1
                return pS[srr[0] % 2]

            def nextPT():
                prr[0] += 1
                return PT[prr[0] % 4]

            def nextTmp():
                trr[0] += 1
                return tmpS[trr[0] % 2]

            pending = []

            def emit_tr(qt_, kvh_):
                px, Tpx = pX[1]
                for g in range(4):
                    OP("pe", "transpose", [Ton, Tident], [Tpx], out=px[:, g * 128:(g + 1) * 128], in_=on[:, g, :], identity=ident[:])
                for g in range(4):
                    OP("dve", "tensor_scalar", [Tpx, Tagw], [TmixT], out=mixT[:, 4 * kvh_ + g, qt_ * 128:(qt_ + 1) * 128], in0=px[:, g * 128:(g + 1) * 128],
                       scalar1=agw[:, 4 * kvh_ + g:4 * kvh_ + g + 1], scalar2=None, op0=ALU.mult)

            for qt in range(8):
                qs = slice(qt * 128, (qt + 1) * 128)
                for kvh in range(2):
                    hs = slice(4 * kvh, 4 * kvh + 4)
                    q4 = qT[:, hs, qs]
                    ocmp, Tocmp = ocmp2[(2 * qt + kvh) % 2]
                    for tl in range(2):
                        ps, Tps = nextS()
                        OP("pe", "matmul", [TkcT, TqT], [Tps], out=ps[:], lhsT=kcT[:, kvh, tl * 128:(tl + 1) * 128], rhs=q4, start=True, stop=True)
                        if tl == 0:
                            OP("act", "activation", [Tps, Tcvalid], [Tec], out=ec[:, 0, :], in_=ps[:], func=AF.Exp, scale=SCALE, bias=cvalid[:, 0:1])
                        else:
                            tm, Ttm = nextTmp()
                            OP("dve", "scalar_tensor_tensor", [Tps, TCB], [Ttm], out=tm[:].rearrange("p (a b) -> p a b", a=4), in0=ps[:].rearrange("p (a b) -> p a b", a=4),
                               scalar=SCALE, in1=CB[:, hs, qs], op0=ALU.mult, op1=ALU.add)
                            OP("act", "activation", [Ttm, Tcvalid], [Tec], out=ec[:, 1, :], in_=tm[:], func=AF.Exp, bias=cvalid[:, 1:2])
                    for g in range(4):
                        po, Tpo = pO[g]
                        for tl in range(2):
                            OP("pe", "matmul", [Tec, Tvcaug], [Tpo], out=po[:, 0:194], lhsT=ec[:, tl, g * 128:(g + 1) * 128], rhs=vcaug[:, tl, kvh, :],
                               start=(tl == 0), stop=(tl == 1))
                        evac(ocmp[:, g, :], po[:, 0:194], [Tpo], [Tocmp])
                    OP("dve", "tensor_scalar", [Tocmp], [Tsm], out=sm[:, 0:4], in0=ocmp[:, :, 128], scalar1=1e-30, scalar2=None, op0=ALU.max)
                    OP("dve", "reciprocal", [Tsm], [Tsm], out=sm[:, 0:4], in_=sm[:, 0:4])
                    OP("dve", "tensor_scalar", [Tocmp, Tsm], [Timp], out=imp[:], in0=ocmp[:, 0, 130:194], scalar1=sm[:, 0:1], scalar2=None, op0=ALU.mult)
                    for g in range(1, 4):
                        OP("dve", "scalar_tensor_tensor", [Tocmp, Tsm, Timp], [Timp], out=imp[:], in0=ocmp[:, g, 130:194], scalar=sm[:, g:g + 1],
                           in1=imp[:], op0=ALU.mult, op1=ALU.add)
                    OP("dve", "tensor_tensor", [Timp, Tcbnf], [Tscore], out=score[:], in0=imp[:], in1=cbnf[:, qt, :], op=ALU.mult)
                    OP("dve", "tensor_tensor", [Tscore, Tsadd], [Tscore], out=score[:], in0=score[:], in1=sadd[:, qt, :], op=ALU.add)
                    OP("dve", "max", [Tscore], [Ttop8], out=top8[:, 0:8], in_=score[:])
                    OP("dve", "match_replace", [Tscore, Ttop8], [Twork], out=work[:], in_to_replace=top8[:, 0:8], in_values=score[:], imm_value=-1e30)
                    OP("dve", "max", [Twork], [Ttop8], out=top8[:, 8:16], in_=work[:])
                    OP("dve", "tensor_scalar", [Tscore, Ttop8], [Twork], out=work[:], in0=score[:], scalar1=top8[:, 15:16], scalar2=None, op0=ALU.is_ge)
                    OP("dve", "tensor_tensor", [Twork, Tcbm], [Twork], out=work[:], in0=work[:], in1=cbm[:, qt, :], op=ALU.mult)
                    OP("dve", "tensor_scalar", [Twork], [Tnegm], out=negm[:], in0=work[:], scalar1=-NEG, scalar2=NEG, op0=ALU.mult, op1=ALU.add)
                    if pending:
                        emit_tr(*pending.pop())
                    def win_S(k):
                        wslot = qt + k
                        ps, Tps = nextS()
                        OP("pe", "matmul", [TkwT, TqT], [Tps], out=ps[:], lhsT=kwT[:, kvh, wslot * 128:(wslot + 1) * 128], rhs=q4, start=True, stop=True)
                        return ps, Tps
                    nxt = win_S(0)
                    for k in range(5):
                        BT, TBT = ((BT4, TBT4), (BTC, TBTC), (BTC, TBTC), (BT1, TBT1), (BT0, TBT0))[k]
                        wslot = qt + k
                        ps, Tps = nxt
                        if k < 4:
                            nxt = win_S(k + 1)
                        pt, Tpt = nextPT()
                        if k in (1, 2):
                            OP("act", "activation", [Tps, Tkvalid], [Tpt], out=pt[:], in_=ps[:], func=AF.Exp, scale=SCALE, bias=kvalid[:, 20 + wslot:21 + wslot])
                        else:
                            tm, Ttm = nextTmp()
                            OP("dve", "scalar_tensor_tensor", [Tps, TBT], [Ttm], out=tm[:].rearrange("p (a b) -> p a b", a=4), in0=ps[:].rearrange("p (a b) -> p a b", a=4),
                               scalar=SCALE, in1=BT[:, hs, :], op0=ALU.mult, op1=ALU.add)
                            OP("act", "activation", [Ttm, Tkvalid], [Tpt], out=pt[:], in_=tm[:], func=AF.Exp, bias=kvalid[:, 20 + wslot:21 + wslot])
                        for g in range(4):
                            po, Tpo = pO[g]
                            OP("pe", "matmul", [Tpt, Tvw], [Tpo], out=po[:, 0:130], lhsT=pt[:, g * 128:(g + 1) * 128], rhs=vw[:, wslot, kvh, :],
                               start=(k == 0), stop=(k == 4))
                    for g in range(4):
                        evac(owin[:, g, :], pO[g][0][:, 0:130], [pO[g][1]], [Towin])
                    px, Tpx = pX[0]
                    OP("pe", "transpose", [Tnegm, Tident], [Tpx], out=px[0:64, 0:128], in_=negm[:], identity=ident[:])
                    nrr[0] += 1
                    nT, TnT = negT[nrr[0] % 2]
                    OP("act", "activation", [Tpx], [TnT], out=nT[0:64, :], in_=px[0:64, 0:128], func=AF.Copy)
                    last = 24 + qt
                    nTb = nT[:].unsqueeze(1).to_broadcast([128, 4, 128])

                    def sel_S(slot):
                        ps, Tps = nextS()
                        OP("pe", "matmul", [TksT, TqT], [Tps], out=ps[:], lhsT=ksT[:, kvh, slot * 128:(slot + 1) * 128], rhs=q4, start=True, stop=False)
                        OP("pe", "matmul", [Teexp, TnT], [Tps], out=ps[:], lhsT=eexp[:, slot * 128:(slot + 1) * 128], rhs=nTb, start=False, stop=True)
                        return ps, Tps
                    nxt = sel_S(0)
                    for slot in range(last + 1):
                        ps, Tps = nxt
                        if slot < last:
                            nxt = sel_S(slot + 1)
                        pt, Tpt = nextPT()
                        rel = slot - last
                        if rel >= -1:
                            BT, TBT = (BT0, TBT0) if rel == 0 else (BT1, TBT1)
                            tm, Ttm = nextTmp()
                            OP("dve", "scalar_tensor_tensor", [Tps, TBT], [Ttm], out=tm[:].rearrange("p (a b) -> p a b", a=4),
                               in0=ps[:].rearrange("p (a b) -> p a b", a=4), scalar=SCALE, in1=BT[:, hs, :], op0=ALU.mult, op1=ALU.add)
                            OP("act", "activation", [Ttm], [Tpt], out=pt[:], in_=tm[:], func=AF.Exp)
                        else:
                            OP("act", "activation", [Tps], [Tpt], out=pt[:], in_=ps[:], func=AF.Exp, scale=SCALE)
                        for g in range(4):
                            po, Tpo = pO[g]
                            OP("pe", "matmul", [Tpt, Tvs], [Tpo], out=po[:, 0:130], lhsT=pt[:, g * 128:(g + 1) * 128], rhs=vs[:, slot, kvh, :],
                               start=(slot == 0), stop=(slot == last))
                    for g in range(4):
                        evac(osel[:, g, :], pO[g][0][:, 0:130], [pO[g][1]], [Tosel])
                    OP("dve", "reciprocal", [Tosel], [Tsm], out=sm[:, 4:8], in_=osel[:, :, 128])
                    OP("dve", "reciprocal", [Towin], [Tsm], out=sm[:, 8:12], in_=owin[:, :, 128])
                    gv = gsig[:, qt, 12 * kvh:12 * kvh + 12].rearrange("p (g b) -> p g b", b=3)
                    for br in range(3):
                        OP("dve", "tensor_tensor", [Tsm, Tgsig], [Tsm], out=sm[:, 12 + 4 * br:16 + 4 * br], in0=sm[:, 4 * br:4 * br + 4], in1=gv[:, :, br], op=ALU.mult)
                    if dbg and stop_after == 3 and qt in (0, 3, 7):
                        for br, (ob, Tob) in enumerate(((ocmp, Tocmp), (osel, Tosel), (owin, Towin))):
                            for g in range(4):
                                OP("dve", "tensor_scalar", [Tob, Tsm], [Tobr], out=obr[:, g, :], in0=ob[:, g, 0:128], scalar1=sm[:, 4 * br + g:4 * br + g + 1],
                                   scalar2=None, op0=ALU.mult)
                            dump("obr_%d_%d_%d" % (qt, kvh, br), obr[:], Tobr, [128, 4, 128], F32)
                        dump("score_%d_%d" % (qt, kvh), score[:], Tscore, [128, 64], F32)
                    for g in range(4):
                        OP("dve", "tensor_scalar", [Tocmp, Tsm], [Toc], out=oc[:, g, :], in0=ocmp[:, g, 0:128], scalar1=sm[:, 12 + g:13 + g], scalar2=None, op0=ALU.mult)
                        OP("dve", "scalar_tensor_tensor", [Tosel, Tsm, Toc], [Toc], out=oc[:, g, :], in0=osel[:, g, 0:128], scalar=sm[:, 16 + g:17 + g], in1=oc[:, g, :],
                           op0=ALU.mult, op1=ALU.add)
                        OP("dve", "scalar_tensor_tensor", [Towin, Tsm, Toc], [Toc], out=oc[:, g, :], in0=owin[:, g, 0:128], scalar=sm[:, 20 + g:21 + g], in1=oc[:, g, :],
                           op0=ALU.mult, op1=ALU.add)
                        OP("dve", "scalar_tensor_tensor", [Toc], [Tjk, Tsm], out=jk[:], in0=oc[:, g, :], scalar=1.0, in1=oc[:, g, :],
                           op0=ALU.mult, op1=ALU.mult, accum_out=sm[:, 24 + g:25 + g])
                    OP("dve", "tensor_scalar", [Tsm], [Tsm], out=sm[:, 28:32], in0=sm[:, 24:28], scalar1=1.0 / 128, scalar2=EPS, op0=ALU.mult, op1=ALU.add)
                    OP("act", "activation", [Tsm], [Tsm], out=sm[:, 28:32], in_=sm[:, 28:32], func=AF.Sqrt)
                    OP("dve", "reciprocal", [Tsm], [Tsm], out=sm[:, 24:28], in_=sm[:, 28:32])
                    OP("dve", "tensor_tensor", [Toc, Tsm], [Ton], out=on[:], in0=oc[:], in1=sm[:, 24:28].unsqueeze(2).to_broadcast([128, 4, 128]), op=ALU.mult)
                    pending.append((qt, kvh))
            emit_tr(*pending.pop())
            if stop_after == 3:
                dump("mixa", mixT[:, 0:8, :], TmixT, [128, 8, NQ], BF16)
            P.barrier()
        sA.close()
        if stop_after == 3:
            finish()
            return nc, dbg_outs
        h2, Th2 = sb(st, [128, 8, D], F32, "h2")
        for tile in range(8):
            DMA("sp", h2[:, tile, :], xs[3072 + tile * 128:3072 + (tile + 1) * 128, :], [], [Th2])
        with ExitStack() as s4:
            wos = [sb(s4, [128, 16, 512], F32, "wos") for _ in range(2)]
            wob = [sb(s4, [128, 16, 512], BF16, "wob") for _ in range(2)]
            pM4 = [psb(s4, [128, 512], F32, "pM4") for _ in range(4)]
            m4 = [0]
            for cc in range(4):
                stg, Tstg = wos[cc % 2]
                wb, Twb = wob[cc % 2]
                cs = slice(cc * 512, (cc + 1) * 512)
                DMA("sp", stg[:], w_out[:, cs].rearrange("(k p) c -> p k c", p=128), [], [Tstg])
                for hh in range(2):
                    OP("pool" if hh == 0 else "act", "tensor_copy" if hh == 0 else "activation", [Tstg], [Twb],
                       **(dict(out=wb[:, 8 * hh:8 * hh + 8, :], in_=stg[:, 8 * hh:8 * hh + 8, :]) if hh == 0 else
                          dict(out=wb[:, 8 * hh:8 * hh + 8, :], in_=stg[:, 8 * hh:8 * hh + 8, :], func=AF.Copy)))
                for tile in range(8):
                    m4[0] += 1
                    pm, Tpm = pM4[m4[0] % 4]
                    for kc in range(16):
                        OP("pe", "matmul", [TmixT, Twb], [Tpm], out=pm[:], lhsT=mixT[:, kc, tile * 128:(tile + 1) * 128], rhs=wb[:, kc, :],
                           start=(kc == 0), stop=(kc == 15))
                    OP("dve", "tensor_tensor", [Tpm, Th2], [Th2], out=h2[:, tile, cs], in0=pm[:], in1=h2[:, tile, cs], op=ALU.add)
            if stop_after == 4:
                dump("h2", h2[:], Th2, [128, 8, D], F32)
            P.barrier()
        if stop_after == 4:
            finish()
            return nc, dbg_outs

        xn2, Txn2 = sb(st, [128, 8, D], BF16, "xn2")
        eidx, Teidx = sb(st, [128, 8, 128], U32, "eidx")
        gates, Tgates = sb(st, [128, 8, 128], F32, "gates")
        iota16, Tiota = sb(st, [128, 16], F32, "iota16")
        DMA("sp", iota16[:], iota16_d, [], [Tiota])
        xn2T, Txn2T = mixT, T("xn2T")
        with ExitStack() as s4b:
            eqB, TeqB = sb(s4b, [128, 8, 16, 16], F32, "eqB")
            g2b, Tg2b = eqB[:].rearrange("p t a b -> p (t a b)"), TeqB
            sq4, Tsq4 = sb(s4b, [128, 2], F32, "sq4")
            SKT, TSKT = sb(s4b, [128, 16, 128], BF16, "SKT")
            wqs = [sb(s4b, [128, 16, 128], F32, "wqs")] * 2
            wqb = [sb(s4b, [128, 16, 128], BF16, "wqb") for _ in range(2)]
            skst, Tskst = wqs[0]
            skb, Tskb = wqb[0]
            qpT = [sb(s4b, [128, NQ], BF16, "qpT") for _ in range(2)]
            sw = [sb(s4b, [128, 128], F32, "sw") for _ in range(2)]
            sw2, Tsw2 = sb(s4b, [128, 128], F32, "sw2")
            sv, Tsv = sb(s4b, [128, 8, 16, 16], F32, "sv")
            si, Tsi = sb(s4b, [128, 8, 16, 16], U32, "si")
            pT4 = [psb(s4b, [128, 8, 128], BF16, "pT4") for _ in range(2)]
            pM5 = [psb(s4b, [128, 512], F32, "pM5") for _ in range(4)]
            pS5 = [psb(s4b, [128, 512], F32, "pS5") for _ in range(2)]
            DMA("sp", g2b, ffn_norm_w.partition_broadcast(128), [], [Tg2b], **NCD)
            for tile in range(8):
                OP("act", "activation", [Th2], [Txn2, Tsq4], out=xn2[:, tile, :], in_=h2[:, tile, :], func=AF.Square, accum_out=sq4[:, 0:1])
                rstd_from_sumsq(sq4, Tsq4, D)
                OP("dve", "scalar_tensor_tensor", [Th2, Tsq4, Tg2b], [Txn2], out=xn2[:, tile, :], in0=h2[:, tile, :], scalar=sq4[:, 0:1], in1=g2b,
                   op0=ALU.mult, op1=ALU.mult)
                for half in range(2):
                    pt, Tpt = pT4[half]
                    for k8 in range(8):
                        kc = half * 8 + k8
                        OP("pe", "transpose", [Txn2, Tidentb], [Tpt], out=pt[:, k8, :], in_=xn2[:, tile, kc * 128:(kc + 1) * 128], identity=identb[:])
                    evac(xn2T[:, half * 8:(half + 1) * 8, tile * 128:(tile + 1) * 128], pt[:], [Tpt], [Txn2T])
            DMA("sp", skst[:], peer_sk.rearrange("h n d -> n h d"), [], [Tskst])
            OP("pool", "tensor_copy", [Tskst], [Tskb], out=skb[:], in_=skst[:])
            for half in range(2):
                pt, Tpt = pT4[half]
                for k8 in range(8):
                    OP("pe", "transpose", [Tskb, Tidentb], [Tpt], out=pt[:, k8, :], in_=skb[:, half * 8 + k8, :], identity=identb[:])
                evac(SKT[:, half * 8:(half + 1) * 8, :], pt[:], [Tpt], [TSKT])
            candB, TcandB = sb(s4b, [128, 8, 256], F32, "candB")
            cand2B, Tcand2B = sb(s4b, [128, 256], F32, "cand2B")
            cvB, TcvB = sb(s4b, [128, 8, 16], F32, "cvB")
            ciB, TciB = sb(s4b, [128, 8, 16], U32, "ciB")
            pabB, TpabB = sb(s4b, [128, 2, 8, 16], U32, "pabB")
            rf, Trf = sb(s4b, [128, 8, 8, 16], F32, "rf")
            rsm, Trsm = sb(s4b, [128, 2, 8], F32, "rsm")
            B4 = [128, 8, 16, 16]

            def route_h(h):
                c1, c2 = 2 * h, 2 * h + 1
                OP("dve", "tensor_tensor", [Tsv], [TcandB], out=candB[:].rearrange("p t (a b) -> p t a b", a=16),
                   in0=sv[:, :, c1, :].unsqueeze(3).to_broadcast(B4), in1=sv[:, :, c2, :].unsqueeze(2).to_broadcast(B4), op=ALU.add)
                for t in range(8):
                    OP("dve", "max", [TcandB], [TcvB], out=cvB[:, t, 0:8], in_=candB[:, t, :])
                    OP("dve", "max_index", [TcandB, TcvB], [TciB], out=ciB[:, t, 0:8], in_max=cvB[:, t, 0:8], in_values=candB[:, t, :])
                    OP("dve", "match_replace", [TcandB, TcvB], [Tcand2B], out=cand2B[:], in_to_replace=cvB[:, t, 0:8], in_values=candB[:, t, :], imm_value=-1e30)
                    OP("dve", "max", [Tcand2B], [TcvB], out=cvB[:, t, 8:16], in_=cand2B[:])
                    OP("dve", "max_index", [Tcand2B, TcvB], [TciB], out=ciB[:, t, 8:16], in_max=cvB[:, t, 8:16], in_values=cand2B[:])
                OP("dve", "tensor_single_scalar", [TciB], [TpabB], out=pabB[:, 0], in_=ciB[:], scalar=4, op=ALU.logical_shift_right)
                OP("dve", "tensor_single_scalar", [TciB], [TpabB], out=pabB[:, 1], in_=ciB[:], scalar=15, op=ALU.bitwise_and)
                OP("dve", "tensor_copy", [TpabB], [Trf], out=rf[:, 0:2], in_=pabB[:])
                OP("dve", "tensor_copy", [Tsi], [Trf], out=rf[:, 2], in_=si[:, :, c1, :])
                OP("dve", "tensor_copy", [Tsi], [Trf], out=rf[:, 3], in_=si[:, :, c2, :])
                for (src, sif, dst) in ((0, 2, 4), (1, 3, 5)):
                    OP("dve", "tensor_tensor", [Trf, Tiota], [TeqB], out=eqB[:], in0=rf[:, src].unsqueeze(3).to_broadcast(B4),
                       in1=iota16[:].unsqueeze(1).unsqueeze(1).to_broadcast(B4), op=ALU.is_equal)
                    OP("dve", "tensor_tensor", [TeqB, Trf], [TeqB], out=eqB[:], in0=eqB[:], in1=rf[:, sif].unsqueeze(2).to_broadcast(B4), op=ALU.mult)
                    OP("dve", "tensor_reduce", [TeqB], [Trf], out=rf[:, dst], in_=eqB[:], axis=AX.X, op=ALU.add)
                OP("dve", "scalar_tensor_tensor", [Trf], [Trf], out=rf[:, 6], in0=rf[:, 4], scalar=128.0, in1=rf[:, 5], op0=ALU.mult, op1=ALU.add)
                OP("dve", "tensor_copy", [Trf], [Teidx], out=eidx[:, :, 16 * h:16 * h + 16], in_=rf[:, 6])
                OP("dve", "tensor_tensor", [TcvB], [Trf], out=rf[:, 7], in0=cvB[:], in1=cvB[:, :, 0:1].to_broadcast([128, 8, 16]), op=ALU.subtract)
                OP("act", "activation", [Trf], [Trf], out=rf[:, 7], in_=rf[:, 7], func=AF.Exp)
                OP("dve", "tensor_reduce", [Trf], [Trsm], out=rsm[:, 0], in_=rf[:, 7], axis=AX.X, op=ALU.add)
                OP("dve", "reciprocal", [Trsm], [Trsm], out=rsm[:, 1], in_=rsm[:, 0])
                OP("dve", "tensor_tensor", [Trf, Trsm], [Tgates], out=gates[:, :, 16 * h:16 * h + 16], in0=rf[:, 7],
                   in1=rsm[:, 1].unsqueeze(2).to_broadcast([128, 8, 16]), op=ALU.mult)

            m5 = [0]
            for hc in range(16):
                stg, Tstg = wqs[hc % 2]
                wb, Twb = wqb[hc % 2]
                qp, Tqp = qpT[hc % 2]
                DMA("sp", stg[:], peer_wq[:, hc * 128:(hc + 1) * 128].rearrange("(k p) c -> p k c", p=128), [], [Tstg])
                OP("pool", "tensor_copy", [Tstg], [Twb], out=wb[:], in_=stg[:])
                for half in range(2):
                    m5[0] += 1
                    pm, Tpm = pM5[m5[0] % 4]
                    for kc in range(16):
                        OP("pe", "matmul", [Twb, Txn2T], [Tpm], out=pm[:], lhsT=wb[:, kc, :], rhs=xn2T[:, kc, half * 512:(half + 1) * 512],
                           start=(kc == 0), stop=(kc == 15))
                    evac(qp[:, half * 512:(half + 1) * 512], pm[:], [Tpm], [Tqp])
                for tile in range(8):
                    ps, Tps = pS5[tile % 2]
                    swt, Tswt = sw[tile % 2]
                    OP("pe", "matmul", [Tqp, TSKT], [Tps], out=ps[:, 0:128], lhsT=qp[:, tile * 128:(tile + 1) * 128], rhs=SKT[:, hc, :], start=True, stop=True)
                    OP("act", "activation", [Tps], [Tswt], out=swt[:], in_=ps[:, 0:128], func=AF.Copy)
                    OP("dve", "max", [Tswt], [Tsv], out=sv[:, tile, hc, 0:8], in_=swt[:])
                    OP("dve", "max_index", [Tswt, Tsv], [Tsi], out=si[:, tile, hc, 0:8], in_max=sv[:, tile, hc, 0:8], in_values=swt[:])
                    OP("dve", "match_replace", [Tswt, Tsv], [Tsw2], out=sw2[:], in_to_replace=sv[:, tile, hc, 0:8], in_values=swt[:], imm_value=-1e30)
                    OP("dve", "max", [Tsw2], [Tsv], out=sv[:, tile, hc, 8:16], in_=sw2[:])
                    OP("dve", "max_index", [Tsw2, Tsv], [Tsi], out=si[:, tile, hc, 8:16], in_max=sv[:, tile, hc, 8:16], in_values=sw2[:])
                if hc % 2 == 1:
                    route_h(hc // 2)
            if stop_after == 5:
                dump("eidx", eidx[:], Teidx, [128, 8, 128], U32)
                dump("gates", gates[:], Tgates, [128, 8, 128], F32)
            P.barrier()
        if stop_after == 5:
            finish()
            return nc, dbg_outs

        with ExitStack() as s5:
            gfin, Tgfin = sb(s5, [128, D], F32, "gfin")
            mixG = mixT[:].rearrange("p a b -> p (a b)").rearrange("p (k c) -> p k c", k=4)
            gb = [(mixG[:, i, :], T("gbm%d" % i)) for i in range(4)]
            gb += [(lambda t: (t[0][:], t[1]))(sb(s5, [128, 2 * D], BF16, "gb")) for _ in range(4)]
            acc, Tacc = sb(s5, [128, D], F32, "acc")
            junk5, Tjunk5 = sb(s5, [128, D], BF16, "junk5")
            aw2 = [sb(s5, [128, 128], F32, "aw") for _ in range(2)]
            ww2 = [sb(s5, [128, 128], F32, "ww") for _ in range(2)]
            t12 = [sb(s5, [128, 2], F32, "t1") for _ in range(2)]
            sq5, Tsq5 = sb(s5, [128, 2], F32, "sq5")
            dg = [sb(s5, [128, 128], BF16, "dg") for _ in range(8)]
            pV, TpV = psb(s5, [128, D], F32, "pV")
            DMA("sp", gfin[:], final_norm_w.partition_broadcast(128), [], [Tgfin], **NCD)
            GS = 2
            NG = 128 // GS
            gi = [0]
            bufs = {}

            def dots(tile, grp):
                a_, Ta_ = aw2[tile % 2]
                lst = []
                for j in range(GS):
                    slot = grp * GS + j
                    gi[0] += 1
                    g_, Tg_ = gb[gi[0] % 8]
                    P.dma("pool", dict(out=g_, out_offset=None, in_=uv16,
                                       in_offset=bass.IndirectOffsetOnAxis(ap=eidx[:, tile, slot:slot + 1], axis=0)),
                          reads=[Teidx, Tuv16], writes=[Tg_], name="indirect_dma_start")
                    OP("dve", "scalar_tensor_tensor", [Tg_, Txn2], [Tjunk5, Ta_], out=junk5[:], in0=g_[:, 0:D], scalar=1.0, in1=xn2[:, tile, :],
                       op0=ALU.mult, op1=ALU.mult, accum_out=a_[:, slot:slot + 1])
                    lst.append((g_, Tg_))
                bufs[(tile, grp)] = lst

            def gelu_pre(tile, grp):
                a_, Ta_ = aw2[tile % 2]
                t1, Tt1 = t12[grp % 2]
                av = a_[:, grp * GS:(grp + 1) * GS]
                OP("dve", "tensor_tensor", [Ta_], [Tt1], out=t1[:], in0=av, in1=av, op=ALU.mult)
                OP("dve", "tensor_tensor", [Tt1, Ta_], [Tt1], out=t1[:], in0=t1[:], in1=av, op=ALU.mult)
                OP("dve", "scalar_tensor_tensor", [Tt1, Ta_], [Tt1], out=t1[:], in0=t1[:], scalar=0.044715, in1=av, op0=ALU.mult, op1=ALU.add)
                OP("act", "activation", [Tt1], [Tt1], out=t1[:], in_=t1[:], func=AF.Tanh, scale=0.7978845608028654)

            def post_v(tile, grp):
                a_, Ta_ = aw2[tile % 2]
                w_, Tw_ = ww2[tile % 2]
                t1, Tt1 = t12[grp % 2]
                sl = slice(grp * GS, (grp + 1) * GS)
                OP("dve", "tensor_scalar", [Tt1], [Tt1], out=t1[:], in0=t1[:], scalar1=0.5, scalar2=0.5, op0=ALU.mult, op1=ALU.add)
                OP("dve", "tensor_tensor", [Tt1, Ta_], [Tt1], out=t1[:], in0=t1[:], in1=a_[:, sl], op=ALU.mult)
                OP("dve", "tensor_tensor", [Tt1, Tgates], [Tw_], out=w_[:, sl], in0=t1[:], in1=gates[:, tile, sl], op=ALU.mult)
                for j, (g_, Tg_) in enumerate(bufs.pop((tile, grp))):
                    slot = grp * GS + j
                    dk, Tdk = dg[slot % 8]
                    OP("act", "activation", [Tident, Tw_], [Tdk], out=dk[:], in_=ident[:], func=AF.Copy, scale=w_[:, slot:slot + 1])
                    for bq in range(4):
                        OP("pe", "matmul", [Tdk, Tg_], [TpV], out=pV[:, bq * 512:(bq + 1) * 512], lhsT=dk[:], rhs=g_[:, D + bq * 512:D + (bq + 1) * 512],
                           start=(slot == 0), stop=(slot == 127))

            def final(tile):
                OP("dve", "tensor_tensor", [TpV, Th2], [Tacc], out=acc[:], in0=pV[:], in1=h2[:, tile, :], op=ALU.add)
                OP("act", "activation", [Tacc], [Tjunk5, Tsq5], out=junk5[:], in_=acc[:], func=AF.Square, accum_out=sq5[:, 0:1])
                rstd_from_sumsq(sq5, Tsq5, D)
                OP("dve", "scalar_tensor_tensor", [Tacc, Tsq5, Tgfin], [Tacc], out=acc[:], in0=acc[:], scalar=sq5[:, 0:1], in1=gfin[:], op0=ALU.mult, op1=ALU.mult)
                DMA("sp", y_out[tile * 128:(tile + 1) * 128, :], acc[:], [Tacc], [Tout])

            prev = None
            for tile in range(8):
                for grp in range(NG):
                    dots(tile, grp)
                    gelu_pre(tile, grp)
                    if prev is not None:
                        post_v(*prev)
                        if prev[1] == NG - 1:
                            final(prev[0])
                    prev = (tile, grp)
            post_v(*prev)
            final(prev[0])
        finish()
        return nc, dbg_outs
        raise NotImplementedError
    return nc, dbg_outs


def _prep_inputs(inputs):
    x = np.asarray(inputs["x"], np.float32)
    cst = _consts()
    shared = {
        "attn_norm_w": np.ascontiguousarray(inputs["attn_norm_w"][0]),
        "w_in": np.ascontiguousarray(inputs["w_in"][0]),
        "w_cmp_k": np.ascontiguousarray(inputs["w_cmp_k"][0]),
        "w_cmp_v": np.ascontiguousarray(inputs["w_cmp_v"][0]),
        "cmp_pos": np.ascontiguousarray(inputs["cmp_pos"][0]),
        "conv_w": np.ascontiguousarray(inputs["conv_w"][0]),
        "conv_b": np.ascontiguousarray(inputs["conv_b"][0]),
        "attn_group_norm_w": np.ascontiguousarray(inputs["attn_group_norm_w"][0]),
        "conv_group_norm_w": np.ascontiguousarray(inputs["conv_group_norm_w"][0]),
        "w_out": np.ascontiguousarray(inputs["w_out"][0]),
        "rel_bias": np.ascontiguousarray(inputs["rel_bias"]),
        "ffn_norm_w": np.ascontiguousarray(inputs["ffn_norm_w"][0]),
        "peer_wq": np.ascontiguousarray(inputs["peer_wq"][0]),
        "peer_subkeys": np.ascontiguousarray(inputs["peer_subkeys"][0]).reshape(16, 128, 128),
        "peer_u": np.ascontiguousarray(inputs["peer_u"][0]),
        "peer_v": np.ascontiguousarray(inputs["peer_v"][0]),
        "final_norm_w": np.ascontiguousarray(inputs["final_norm_w"]),
    }
    shared = {k: np.asarray(v, np.float32) for k, v in shared.items()}
    shared.update(cst)
    in_maps = []
    for core in range(8):
        b, c = core // 4, core % 4
        q0 = 1024 * c
        ws = q0 - 3072
        xw = np.zeros((S, D), np.float32)
        lo = max(ws, 0)
        xw[lo - ws:, :] = x[b, lo:q0 + 1024, :]
        m = dict(shared)
        m["xs"] = xw
        m.update(_core_consts(c))
        in_maps.append(m)
    return in_maps


def kernel(**inputs):
    in_maps = _prep_inputs(inputs)
    nc, _ = build()
    res = run_bass_kernel_spmd(nc, in_maps, core_ids=list(range(8)))
    out = np.zeros((2, S, D), np.float32)
    for core in range(8):
        b, c = core // 4, core % 4
        out[b, 1024 * c:1024 * (c + 1), :] = res.results[core]["y"]
    return out
```

```python
import math
from contextlib import ExitStack
import numpy as np
import ml_dtypes
import concourse.bass as bass
import concourse.mybir as mybir
from concourse.bass_utils import run_bass_kernel_spmd

F32 = mybir.dt.float32
BF16 = mybir.dt.bfloat16
U32 = mybir.dt.uint32
ALU = mybir.AluOpType
AF = mybir.ActivationFunctionType
AX = mybir.AxisListType

D = 2048
S = 4096
NQ = 1024
NEG = -30000.0
EPS = 1e-6
SCALE = 128 ** -0.5
DIN = 5656
MSEG0, MSEG1, MSEG2 = 384, 256, 3072
MTOT = MSEG0 + MSEG1 + MSEG2
NDBG = []


class T:
    __slots__ = ("name", "w", "r", "dsem")

    def __init__(self, name):
        self.name = name
        self.w = {}
        self.r = {}
        self.dsem = None


class Prog:
    ENG = ("pe", "dve", "act", "pool", "sp")

    def __init__(self, nc, stack):
        self.nc = nc
        self.stack = stack
        self.stream = {k: [] for k in self.ENG}
        self.cnt = {}
        self.sems = {}
        self.known = {k: {} for k in self.ENG}
        for k in self.ENG:
            self.sems[k] = stack.enter_context(nc.semaphore("s_" + k))
            self.cnt[k] = 0
        self.ndsem = 0

    def _dsem(self, t):
        if t.dsem is None:
            key = "d%d" % self.ndsem
            self.ndsem += 1
            self.sems[key] = self.stack.enter_context(self.nc.semaphore(key))
            self.cnt[key] = 0
            t.dsem = key
        return t.dsem

    def _waits(self, eng, reads, writes, nowaw=False):
        need = {}

        def add(k, v):
            if v > need.get(k, 0):
                need[k] = v
        for t in reads:
            for k, v in t.w.items():
                add(k, v)
        for t in writes:
            if not nowaw:
                for k, v in t.w.items():
                    add(k, v)
            for k, v in t.r.items():
                add(k, v)
        kn = self.known[eng]
        out = []
        for k, v in need.items():
            if k == eng and eng == "pe":
                continue
            if kn.get(k, 0) < v:
                kn[k] = v
                out.append((k, v))
        return out

    def op(self, eng, name, kw, reads=(), writes=()):
        waits = self._waits(eng, reads, writes)
        self.cnt[eng] += 1
        v = self.cnt[eng]
        self.stream[eng].append((waits, (name, kw), eng, 1))
        for t in reads:
            t.r[eng] = v
        for t in writes:
            t.w[eng] = v

    def dma(self, eng, kw, reads=(), writes=(), nowaw=False, name="dma_start"):
        assert len(writes) == 1
        t = writes[0]
        key = self._dsem(t)
        waits = self._waits(eng, reads, writes, nowaw=nowaw)
        self.cnt[key] += 16
        v = self.cnt[key]
        self.stream[eng].append((waits, (name, kw), key, 16))
        for s in reads:
            s.r[key] = v
        t.w[key] = v

    def barrier(self):
        snap = dict(self.cnt)
        for e in self.ENG:
            kn = self.known[e]
            waits = []
            for k, v in snap.items():
                if v > 0 and kn.get(k, 0) < v:
                    kn[k] = v
                    waits.append((k, v))
            self.stream[e].append((waits, None, None, 0))

    def final_wait(self, eng, tiles):
        waits = self._waits(eng, tiles, ())
        self.stream[eng].append((waits, None, None, 0))

    def emit(self):
        nc = self.nc
        with nc.Block() as block:
            def mk(name):
                def body(e):
                    for waits, fn, key, inc in self.stream[name]:
                        for k, v in waits:
                            e.wait_ge(self.sems[k], v)
                        if fn is not None:
                            getattr(e, fn[0])(**fn[1]).then_inc(self.sems[key], inc)
                return body
            block.tensor(mk("pe"))
            block.vector(mk("dve"))
            block.scalar(mk("act"))
            block.gpsimd(mk("pool"))
            block.sync(mk("sp"))


def _t5_bucket_np(n):
    n = np.maximum(n, 0)
    nf = np.maximum(n, 1).astype(np.float32)
    large = 16 + (np.log(nf / np.float32(16)) / np.float32(math.log(8.0)) * np.float32(16)).astype(np.int32)
    large = np.minimum(large, 31)
    return np.where(n < 16, n, large)


def _consts():
    oh = np.zeros((33, MTOT), np.float32)
    m = np.arange(MSEG0)
    d = m - 127
    bk = _t5_bucket_np(d)
    for i in range(MSEG0):
        if d[i] < 0:
            oh[32, i] = NEG
        else:
            oh[bk[i], i] = 1.0
    for i in range(MSEG1):
        if i - 127 < 0:
            oh[31, MSEG0 + i] = 1.0
        else:
            oh[32, MSEG0 + i] = NEG
    m = np.arange(MSEG2)
    d = m - 1023
    bk = _t5_bucket_np(d)
    base = MSEG0 + MSEG1
    for i in range(MSEG2):
        if d[i] < 0:
            oh[32, base + i] = NEG
        else:
            oh[bk[i], base + i] = 1.0
    ident = np.eye(128, dtype=np.float32)
    jmat = ident[::-1].copy()
    eexp = np.zeros((64, 4096), np.float32)
    eexp[np.arange(4096) // 64, np.arange(4096)] = 1.0
    ov = np.zeros((256, 64), np.float32)
    for n in range(255):
        pos = n * 16 + np.arange(32)
        for p in pos:
            ov[n + 1, p // 64] += 1.0 / 32
    ovs = ov.reshape(2, 128, 64).transpose(1, 0, 2).copy()
    iota16 = np.tile(np.arange(16, dtype=np.float32)[None, :], (128, 1))
    return dict(iota16=iota16, oh33=oh, ident=ident, jmat=jmat, identb=ident.astype(ml_dtypes.bfloat16),
                eexp=eexp.astype(ml_dtypes.bfloat16), ovs=ovs)


def _core_consts(c):
    q0 = 1024 * c
    ws = q0 - 3072
    kvalid = np.zeros((128, 32), np.float32)
    for s in range(32):
        if ws + 128 * s < 0:
            kvalid[:, s] = NEG
    cvalid = np.zeros((128, 2), np.float32)
    for tl in range(2):
        for p in range(128):
            n_rel = tl * 128 + p - 1
            n_abs = n_rel + ws // 16
            if n_rel < 0 or n_abs < 0:
                cvalid[p, tl] = NEG
    t_abs = q0 + np.arange(1024)
    j_abs = np.arange(64) + ws // 64
    exists = j_abs >= 0
    causal = exists[None, :] & (j_abs[None, :] * 64 <= t_abs[:, None])
    blk_t = t_abs // 64
    forced = exists[None, :] & ((j_abs[None, :] == 0) | (j_abs[None, :] == blk_t[:, None]) | (j_abs[None, :] == blk_t[:, None] - 1))
    cbnf = (causal & ~forced).astype(np.float32)
    sadd = np.where(forced, 5.0, np.where(causal, 0.0, -1.0)).astype(np.float32)
    cb = causal.astype(np.float32)
    f = lambda a: a.reshape(8, 128, 64).transpose(1, 0, 2).copy()
    return dict(kvalid=kvalid, cvalid=cvalid, cbnf=f(cbnf), sadd=f(sadd), cb=f(cb))


def build(stop_after=None, dbg=False):
    nc = bass.Bass("TRN2", target_bir_lowering=False)
    din = lambda n, shp, dt=F32: nc.dram_tensor(n, list(shp), dt, kind="ExternalInput").ap()
    xs = din("xs", [S, D])
    attn_norm_w = din("attn_norm_w", [D])
    w_in = din("w_in", [D, DIN])
    w_cmp_k = din("w_cmp_k", [32, 128, 128])
    w_cmp_v = din("w_cmp_v", [32, 128, 128])
    cmp_pos = din("cmp_pos", [32, 128])
    conv_w = din("conv_w", [3, 1024])
    conv_b = din("conv_b", [1024])
    agnw = din("attn_group_norm_w", [1024])
    cgnw = din("conv_group_norm_w", [1024])
    w_out = din("w_out", [D, D])
    rel_bias = din("rel_bias", [32, 8])
    ffn_norm_w = din("ffn_norm_w", [D])
    peer_wq = din("peer_wq", [D, D])
    peer_sk = din("peer_subkeys", [16, 128, 128])
    peer_u = din("peer_u", [16384, D])
    peer_v = din("peer_v", [16384, D])
    final_norm_w = din("final_norm_w", [D])
    oh33_d = din("oh33", [33, MTOT])
    ident_d = din("ident", [128, 128])
    jmat_d = din("jmat", [128, 128])
    identb_d = din("identb", [128, 128], BF16)
    eexp_d = din("eexp", [64, 4096], BF16)
    ovs_d = din("ovs", [128, 2, 64])
    kvalid_d = din("kvalid", [128, 32])
    cvalid_d = din("cvalid", [128, 2])
    cbnf_d = din("cbnf", [128, 8, 64])
    sadd_d = din("sadd", [128, 8, 64])
    cb_d = din("cb", [128, 8, 64])
    iota16_d = din("iota16", [128, 16])
    y_out = nc.dram_tensor("y", [NQ, D], F32, kind="ExternalOutput").ap()
    gtd = nc.dram_tensor("gtd", [8, MTOT], F32, kind="Internal").ap()
    uv16 = nc.dram_tensor("uv16", [16384, 2 * D], BF16, kind="Internal").ap()
    Tuv16 = T("uv16")
    dbg_outs = {}

    with ExitStack() as st:
        P = Prog(nc, st)
        ntile = [0]

        def sb(stk, shape, dt=F32, name=None):
            ntile[0] += 1
            nm = (name or "t") + "_%d" % ntile[0]
            return stk.enter_context(nc.sbuf_tensor(nm, list(shape), dt)), T(nm)

        def psb(stk, shape, dt=F32, name=None):
            ntile[0] += 1
            nm = (name or "p") + "_%d" % ntile[0]
            return stk.enter_context(nc.psum_tensor(nm, list(shape), dt)), T(nm)

        Tout = T("y_out")

        def dump(name, ap, t, shape, dt=F32):
            if not dbg:
                return
            o = nc.dram_tensor("dbg_" + name, list(shape), dt, kind="ExternalOutput").ap()
            to = T("dbg_" + name)
            dbg_outs[name] = to
            P.dma("sp", dict(out=o, in_=ap), reads=[t], writes=[to])

        def finish():
            P.final_wait("sp", [Tout] + list(dbg_outs.values()))
            P.emit()

        def OP(eng, name, reads, writes, **kw):
            P.op(eng, name, kw, reads, writes)

        def DMA(eng, out, in_, reads, writes, **kw):
            P.dma(eng, dict(out=out, in_=in_, **kw), reads, writes)

        evac_rr = [0]

        def evac(out_ap, in_ap, reads, writes):
            evac_rr[0] += 1
            if evac_rr[0] % 2:
                OP("act", "activation", reads, writes, out=out_ap, in_=in_ap, func=AF.Copy)
            else:
                OP("dve", "tensor_copy", reads, writes, out=out_ap, in_=in_ap)

        ident, Tident = sb(st, [128, 128], F32, "ident")
        identb, Tidentb = sb(st, [128, 128], BF16, "identb")
        jmat, Tjmat = sb(st, [128, 128], F32, "jmat")
        onesf, Tonesf = sb(st, [128, 128], F32, "onesf")
        gcol, Tgcol = sb(st, [128, 16], F32, "gcol")
        chcol, Tchcol = sb(st, [128, 8], F32, "chcol")
        kvalid, Tkvalid = sb(st, [128, 32], F32, "kvalid")
        cvalid, Tcvalid = sb(st, [128, 2], F32, "cvalid")
        ccol0, Tccol0 = sb(st, [128, 8], F32, "ccol0")
        cw, Tcw = sb(st, [128, 3, 8], F32, "cw")
        cbias, Tcbias = sb(st, [128, 8], F32, "cbias")
        agw, Tagw = sb(st, [128, 8], F32, "agw")
        cgw, Tcgw = sb(st, [128, 8], F32, "cgw")
        DMA("sp", ident[:], ident_d, [], [Tident])
        DMA("sp", identb[:], identb_d, [], [Tidentb])
        DMA("sp", jmat[:], jmat_d, [], [Tjmat])
        DMA("sp", kvalid[:], kvalid_d, [], [Tkvalid])
        DMA("sp", cvalid[:], cvalid_d, [], [Tcvalid])
        OP("dve", "memset", [], [Tonesf], ap=onesf[:], constant=1.0)
        NCD = dict(allow_slow_non_contiguous=True)
        DMA("sp", gcol[:], attn_norm_w.rearrange("(k p) -> p k", p=128), [], [Tgcol], **NCD)
        for wi in range(3):
            DMA("sp", cw[:, wi, :], conv_w[wi].rearrange("(c p) -> p c", p=128), [], [Tcw], **NCD)
        DMA("sp", cbias[:], conv_b.rearrange("(c p) -> p c", p=128), [], [Tcbias], **NCD)
        DMA("sp", agw[:], agnw.rearrange("(c p) -> p c", p=128), [], [Tagw], **NCD)
        DMA("sp", cgw[:], cgnw.rearrange("(c p) -> p c", p=128), [], [Tcgw], **NCD)
        DMA("sp", chcol[:], rel_bias[31:32, :].partition_broadcast(128), [], [Tchcol], **NCD)
        OP("dve", "tensor_scalar", [Tchcol, Tcvalid], [Tccol0], out=ccol0[:], in0=chcol[:], scalar1=cvalid[:, 0:1],
           scalar2=None, op0=ALU.add)

        mixT, TmixT = sb(st, [128, 16, NQ], BF16, "mixT")
        gsig, Tgsig = sb(st, [128, 8, 24], F32, "gsig")
        mixF = mixT[:].rearrange("p a b -> p (a b)").bitcast(F32)
        sA = ExitStack()
        ksT, TksT = sb(sA, [128, 2, S], BF16, "ksT")
        vs, Tvs = sb(sA, [128, 32, 2, 130], BF16, "vs")
        kwT, TkwT = sb(sA, [128, 2, 1536], BF16, "kwT")
        vw, Tvw = sb(sA, [128, 12, 2, 130], BF16, "vw")
        kcT, TkcT = sb(sA, [128, 2, 256], BF16, "kcT")
        vcaug, Tvcaug = sb(sA, [128, 2, 2, 194], F32, "vcaug")
        xnT, _ = sb(sA, [128, 2, 16, 512], BF16, "xnT")
        xnTA, TxnTA = xnT[:, 0], T("xnTA")
        xnTB, TxnTB = xnT[:, 1], T("xnTB")
        xnTh, TxnTh = sb(sA, [128, 16, 2], BF16, "xnTh")
        OP("pool", "memset", [], [Tvs], ap=vs[:], constant=1.0)
        OP("pool", "memset", [], [Tvw], ap=vw[:], constant=1.0)
        OP("pool", "memset", [], [Tvcaug], ap=vcaug[:], constant=0.0)
        OP("pool", "memset", [], [Tvcaug], ap=vcaug[:, :, :, 128:129], constant=1.0)
        for tl in range(2):
            for kvh in range(2):
                DMA("sp", vcaug[:, tl, kvh, 130:194], ovs_d[:, tl, :], [], [Tvcaug])

        def rstd_from_sumsq(sqt, Tsq, n):
            OP("dve", "tensor_scalar", [Tsq], [Tsq], out=sqt[:, 1:2], in0=sqt[:, 0:1], scalar1=1.0 / n, scalar2=EPS,
               op0=ALU.mult, op1=ALU.add)
            OP("act", "activation", [Tsq], [Tsq], out=sqt[:, 1:2], in_=sqt[:, 1:2], func=AF.Sqrt)
            OP("dve", "reciprocal", [Tsq], [Tsq], out=sqt[:, 0:1], in_=sqt[:, 1:2])

        with ExitStack() as s1:
            wkv, Twkv = sb(s1, [128, 16, 1536], BF16, "wkv")
            wck, Twck = sb(s1, [128, 32, 128], BF16, "wck")
            wcv, Twcv = sb(s1, [128, 32, 128], BF16, "wcv")
            cposT, TcposT = sb(s1, [128, 32], BF16, "cposT")
            constk, Tconstk = sb(s1, [128, 2], F32, "constk")
            kcvbuf, Tkcv = sb(s1, [128, 4, 528], BF16, "kcvbuf")
            vcT, TvcT = sb(s1, [128, 2, 256], F32, "vcT")
            xst = [(mixF[:, 4096 + i * 2048:4096 + (i + 1) * 2048], T("xst%d" % i)) for i in range(2)]
            xnb = [sb(s1, [128, D], BF16, "xnb") for _ in range(2)]
            junk, Tjunk = sb(s1, [128, D], BF16, "junk")
            sq = [sb(s1, [128, 2], F32, "sq") for _ in range(2)]
            wst = [(mixF[:, i * 2048:(i + 1) * 2048].rearrange("p (k c) -> p k c", k=16), T("wst%d" % i)) for i in range(2)]
            pT = [psb(s1, [128, 8, 128], BF16, "pT") for _ in range(2)]
            pM = [psb(s1, [128, 512], F32, "pM") for _ in range(4)]
            pC, TpC = psb(s1, [128, 512], F32, "pC")

            for ci in range(12):
                stg, Tstg = wst[ci % 2]
                c0 = 1024 + ci * 128
                DMA("sp", stg, w_in[:, c0:c0 + 128].rearrange("(k p) c -> p k c", p=128), [], [Tstg])
                OP("pool", "tensor_tensor", [Tstg, Tgcol], [Twkv], out=wkv[:, :, ci * 128:(ci + 1) * 128], in0=stg,
                   in1=gcol[:].unsqueeze(2).to_broadcast([128, 16, 128]), op=ALU.mult)
            DMA("pool", wck[:], w_cmp_k.rearrange("l d e -> d l e"), [], [Twck])
            DMA("pool", wcv[:], w_cmp_v.rearrange("l d e -> d l e"), [], [Twcv])
            DMA("pool", cposT[:], cmp_pos.rearrange("l d -> d l"), [], [TcposT], **NCD)
            OP("dve", "memset", [], [Tkcv], ap=kcvbuf[:], constant=0.0)
            for ti, (wc, Twc) in enumerate(((wck, Twck), (wcv, Twcv))):
                for l in range(32):
                    OP("pe", "matmul", [Twc, TcposT], [TpC], out=pC[:, ti:ti + 1], lhsT=wc[:, l, :], rhs=cposT[:, l:l + 1],
                       start=(l == 0), stop=(l == 31))
            OP("dve", "tensor_copy", [TpC], [Tconstk], out=constk[:], in_=pC[:, 0:2])

            xcount = [0]

            def norm_transpose(row0, dst, Tdst, col0):
                i = xcount[0] % 2
                xcount[0] += 1
                (xt, Txt), (xb, Txb), (sqt, Tsq) = xst[i], xnb[i], sq[i]
                DMA("sp", xt, xs[row0:row0 + 128, :], [], [Txt])
                OP("act", "activation", [Txt], [Tjunk, Tsq], out=junk[:], in_=xt, func=AF.Square, accum_out=sqt[:, 0:1])
                rstd_from_sumsq(sqt, Tsq, D)
                OP("dve", "tensor_scalar", [Txt, Tsq], [Txb], out=xb[:], in0=xt, scalar1=sqt[:, 0:1], scalar2=None, op0=ALU.mult)
                for half in range(2):
                    pt, Tpt = pT[half]
                    for k8 in range(8):
                        kc = half * 8 + k8
                        OP("pe", "transpose", [Txb, Tidentb], [Tpt], out=pt[:, k8, :], in_=xb[:, kc * 128:(kc + 1) * 128],
                           identity=identb[:])
                    evac(dst[:, half * 8:(half + 1) * 8, col0:col0 + 128], pt[:], [Tpt], [Tdst])

            mrr = [0]

            def nextpm():
                mrr[0] += 1
                return pM[mrr[0] % 4]

            def NT(g):
                xg, Txg = (xnTA, TxnTA) if g % 2 == 0 else (xnTB, TxnTB)
                for j in range(4):
                    norm_transpose((4 * g + j) * 128, xg, Txg, j * 128)
                if g == 5:
                    OP("dve", "tensor_copy", [TxnTB], [TxnTh], out=xnTh[:], in_=xnTB[:, :, 510:512])

            NT(0)
            for g in range(8):
                xg, Txg = (xnTA, TxnTA) if g % 2 == 0 else (xnTB, TxnTB)
                if g < 7:
                    NT(g + 1)
                fm = [(0, kcvbuf[:, 0, 16:528], Tkcv), (128, kcvbuf[:, 1, 16:528], Tkcv),
                      (256, kcvbuf[:, 2, 16:528], Tkcv), (384, kcvbuf[:, 3, 16:528], Tkcv),
                      (512, ksT[:, 0, g * 512:(g + 1) * 512], TksT), (640, ksT[:, 1, g * 512:(g + 1) * 512], TksT)]
                if g >= 5:
                    fm += [(1024, kwT[:, 0, (g - 5) * 512:(g - 4) * 512], TkwT),
                           (1152, kwT[:, 1, (g - 5) * 512:(g - 4) * 512], TkwT)]
                for (co, dst, Td) in fm:
                    pm, Tpm = nextpm()
                    for kc in range(16):
                        OP("pe", "matmul", [Twkv, Txg], [Tpm], out=pm[:], lhsT=wkv[:, kc, co:co + 128], rhs=xg[:, kc, :],
                           start=(kc == 0), stop=(kc == 15))
                    evac(dst, pm[:], [Tpm], [Td])
                for j in range(4):
                    slot = 4 * g + j
                    pm, Tpm = nextpm()
                    for kc in range(16):
                        OP("pe", "matmul", [Twkv, Txg], [Tpm], out=pm[:, 0:256], lhsT=xg[:, kc, j * 128:(j + 1) * 128],
                           rhs=wkv[:, kc, 768:1024], start=(kc == 0), stop=(kc == 15))
                    evac(vs[:, slot, :, 0:128], pm[:, 0:256].rearrange("p (a b) -> p a b", a=2), [Tpm], [Tvs])
                    if g >= 5:
                        pm, Tpm = nextpm()
                        for kc in range(16):
                            OP("pe", "matmul", [Twkv, Txg], [Tpm], out=pm[:, 0:256], lhsT=xg[:, kc, j * 128:(j + 1) * 128],
                               rhs=wkv[:, kc, 1280:1536], start=(kc == 0), stop=(kc == 15))
                        evac(vw[:, slot - 20, :, 0:128], pm[:, 0:256].rearrange("p (a b) -> p a b", a=2), [Tpm], [Tvw])
                for ti in range(2):
                    wc, Twc = (wck, Twck) if ti == 0 else (wcv, Twcv)
                    for l in range(32):
                        OP("pe", "matmul", [Twc, Tkcv], [TpC], out=pC[:, 64 * ti:64 * ti + 64].rearrange("p (a b) -> p a b", a=2), lhsT=wc[:, l, :],
                           rhs=kcvbuf[:, 2 * ti:2 * ti + 2, l:l + 497:16], start=(l == 0), stop=(l == 31))
                for kvh in range(2):
                    OP("dve", "tensor_scalar", [TpC, Tconstk], [TkcT], out=kcT[:, kvh, 32 * g:32 * g + 32],
                       in0=pC[:, 32 * kvh:32 * kvh + 32], scalar1=constk[:, 0:1], scalar2=None, op0=ALU.add)
                    OP("dve", "tensor_scalar", [TpC, Tconstk], [TvcT], out=vcT[:, kvh, 32 * g:32 * g + 32],
                       in0=pC[:, 64 + 32 * kvh:96 + 32 * kvh], scalar1=constk[:, 1:2], scalar2=None, op0=ALU.add)
                OP("dve", "tensor_copy", [Tkcv], [Tkcv], out=kcvbuf[:, :, 0:16], in_=kcvbuf[:, :, 512:528])
            for tl in range(2):
                for kvh in range(2):
                    pm, Tpm = nextpm()
                    OP("pe", "transpose", [TvcT, Tident], [Tpm], out=pm[:, 0:128], in_=vcT[:, kvh, tl * 128:(tl + 1) * 128],
                       identity=ident[:])
                    OP("dve", "tensor_copy", [Tpm], [Tvcaug], out=vcaug[:, tl, kvh, 0:128], in_=pm[:, 0:128])
            if stop_after == 1:
                dump("ksT", ksT[:], TksT, [128, 2, S], BF16)
                dump("vs", vs[:], Tvs, [128, 32, 2, 130], BF16)
                dump("kwT", kwT[:], TkwT, [128, 2, 1536], BF16)
                dump("kcT", kcT[:], TkcT, [128, 2, 256], BF16)
                dump("vcaug", vcaug[:], Tvcaug, [128, 2, 2, 194], F32)
            P.barrier()
        if stop_after == 1:
            finish()
            return nc, dbg_outs

        qT, TqT = sb(sA, [128, 8, NQ], BF16, "qT")
        xh = [(xnTA, TxnTA), (xnTB, TxnTB)]
        with ExitStack() as s2:
            wst2 = [sb(s2, [128, 16, 128], F32, "wst2") for _ in range(2)]
            wb2 = [sb(s2, [128, 16, 128], BF16, "wb2") for _ in range(4)]
            bT, TbT = sb(s2, [128, 1026], F32, "bT")
            cT, TcT = sb(s2, [128, 1026], F32, "cT")
            hT, ThT = sb(s2, [128, 1026], F32, "hT")
            uu, Tuu = sb(s2, [128, 1026], F32, "uu")
            yy, Tyy = sb(s2, [128, 1024], F32, "yy")
            oo, Too = sb(s2, [128, 1024], F32, "oo")
            osq, Tosq = sb(s2, [128, 1024], F32, "osq")
            rs, Trs = sb(s2, [128, 1024], F32, "rs")
            pM2 = [psb(s2, [128, 512], F32, "pM2") for _ in range(4)]
            pH, TpH = psb(s2, [128, 512], F32, "pH")
            wrr = [0]
            mr2 = [0]

            def nextpm2():
                mr2[0] += 1
                return pM2[mr2[0] % 4]

            def load_w(c0, width=128):
                i = wrr[0]
                wrr[0] += 1
                stg, Tstg = wst2[i % 2]
                wb, Twb = wb2[i % 4]
                DMA("sp", stg[:, :, 0:width], w_in[:, c0:c0 + width].rearrange("(k p) c -> p k c", p=128), [], [Tstg])
                OP("pool", "tensor_tensor", [Tstg, Tgcol], [Twb], out=wb[:, :, 0:width], in0=stg[:, :, 0:width],
                   in1=gcol[:].unsqueeze(2).to_broadcast([128, 16, width]), op=ALU.mult)
                return wb, Twb

            def proj_fm(wb, Twb, dst_fn, Tdst):
                for half in range(2):
                    xg, Txg = xh[half]
                    pm, Tpm = nextpm2()
                    for kc in range(16):
                        OP("pe", "matmul", [Twb, Txg], [Tpm], out=pm[:], lhsT=wb[:, kc, :], rhs=xg[:, kc, :],
                           start=(kc == 0), stop=(kc == 15))
                    evac(dst_fn(half), pm[:], [Tpm], [Tdst])

            for h in range(8):
                wb, Twb = load_w(h * 128)
                proj_fm(wb, Twb, lambda half, h=h: qT[:, h, half * 512:(half + 1) * 512], TqT)
            wg, Twg = load_w(2560, 24)
            for tile in range(8):
                xg, Txg = xh[tile // 4]
                cs = (tile % 4) * 128
                pm, Tpm = nextpm2()
                for kc in range(16):
                    OP("pe", "matmul", [Twg, Txg], [Tpm], out=pm[:, 0:24], lhsT=xg[:, kc, cs:cs + 128], rhs=wg[:, kc, 0:24],
                       start=(kc == 0), stop=(kc == 15))
                OP("act", "activation", [Tpm], [Tgsig], out=gsig[:, tile, :], in_=pm[:, 0:24], func=AF.Sigmoid)
            for ch in range(8):
                wb_b, Twb_b = load_w(2584 + ch * 128)
                wb_c, Twb_c = load_w(3608 + ch * 128)
                wb_h, Twb_h = load_w(4632 + ch * 128)
                for (wb, Twb, dT, TdT) in ((wb_b, Twb_b, bT, TbT), (wb_c, Twb_c, cT, TcT), (wb_h, Twb_h, hT, ThT)):
                    proj_fm(wb, Twb, lambda half, dT=dT: dT[:, 2 + half * 512:2 + (half + 1) * 512], TdT)
                for hi, (wb, Twb, dT, TdT) in enumerate(((wb_c, Twb_c, cT, TcT), (wb_h, Twb_h, hT, ThT))):
                    for kc in range(16):
                        OP("pe", "matmul", [Twb, TxnTh], [TpH], out=pH[:, 2 * hi:2 * hi + 2], lhsT=wb[:, kc, :], rhs=xnTh[:, kc, :],
                           start=(kc == 0), stop=(kc == 15))
                    OP("dve", "tensor_copy", [TpH], [TdT], out=dT[:, 0:2], in_=pH[:, 2 * hi:2 * hi + 2])
                OP("dve", "tensor_tensor", [TcT, ThT], [Tuu], out=uu[:], in0=cT[:], in1=hT[:], op=ALU.mult)
                OP("dve", "tensor_scalar", [Tuu, Tcw, Tcbias], [Tyy], out=yy[:], in0=uu[:, 2:1026], scalar1=cw[:, 2, ch:ch + 1],
                   scalar2=cbias[:, ch:ch + 1], op0=ALU.mult, op1=ALU.add)
                OP("dve", "scalar_tensor_tensor", [Tuu, Tcw, Tyy], [Tyy], out=yy[:], in0=uu[:, 1:1025], scalar=cw[:, 1, ch:ch + 1],
                   in1=yy[:], op0=ALU.mult, op1=ALU.add)
                OP("dve", "scalar_tensor_tensor", [Tuu, Tcw, Tyy], [Tyy], out=yy[:], in0=uu[:, 0:1024], scalar=cw[:, 0, ch:ch + 1],
                   in1=yy[:], op0=ALU.mult, op1=ALU.add)
                OP("dve", "tensor_tensor", [TbT, Tyy], [Too], out=oo[:], in0=bT[:, 2:1026], in1=yy[:], op=ALU.mult)
                OP("pool", "tensor_tensor", [Too], [Tosq], out=osq[:], in0=oo[:], in1=oo[:], op=ALU.mult)
                for half in range(2):
                    pm, Tpm = nextpm2()
                    OP("pe", "matmul", [Tonesf, Tosq], [Tpm], out=pm[:], lhsT=onesf[:], rhs=osq[:, half * 512:(half + 1) * 512],
                       start=True, stop=True)
                    OP("dve", "tensor_scalar", [Tpm], [Trs], out=rs[:, half * 512:(half + 1) * 512], in0=pm[:], scalar1=1.0 / 128,
                       scalar2=EPS, op0=ALU.mult, op1=ALU.add)
                OP("act", "activation", [Trs], [Trs], out=rs[:], in_=rs[:], func=AF.Sqrt)
                OP("dve", "reciprocal", [Trs], [Trs], out=rs[:], in_=rs[:])
                OP("dve", "scalar_tensor_tensor", [Too, Tcgw, Trs], [TmixT], out=mixT[:, 8 + ch, :], in0=oo[:], scalar=cgw[:, ch:ch + 1],
                   in1=rs[:], op0=ALU.mult, op1=ALU.mult)
            if stop_after == 2:
                dump("qT", qT[:], TqT, [128, 8, NQ], BF16)
                dump("gsig", gsig[:], Tgsig, [128, 8, 24], F32)
                dump("mixc", mixT[:, 8:16, :], TmixT, [128, 8, NQ], BF16)
            P.barrier()
        if stop_after == 2:
            finish()
            return nc, dbg_outs
        with ExitStack() as s3:
            BT0, TBT0 = sb(s3, [128, 8, 128], F32, "BT0")
            BT1, TBT1 = sb(s3, [128, 8, 128], F32, "BT1")
            BT4, TBT4 = sb(s3, [128, 8, 128], F32, "BT4")
            BTC, TBTC = sb(s3, [128, 8, 128], F32, "BTC")
            CB, TCB = xnT[:].rearrange("p a b c -> p (a b c)").bitcast(F32).rearrange("p (h t) -> p h t", h=8), T("CB")
            eexp, Teexp = sb(s3, [128, 4096], BF16, "eexp")
            cbnf, Tcbnf = sb(s3, [128, 8, 64], F32, "cbnf")
            sadd, Tsadd = sb(s3, [128, 8, 64], F32, "sadd")
            cbm, Tcbm = sb(s3, [128, 8, 64], F32, "cbm")
            PT = [sb(s3, [128, 512], BF16, "PT") for _ in range(4)]
            ec, Tec = sb(s3, [128, 2, 512], F32, "ec")
            tmpS = [sb(s3, [128, 512], F32, "tmpS") for _ in range(2)]
            ocmp2 = [sb(s3, [128, 4, 194], F32, "ocmp") for _ in range(2)]
            osel, Tosel = sb(s3, [128, 4, 130], F32, "osel")
            owin, Towin = sb(s3, [128, 4, 130], F32, "owin")
            sm, Tsm = sb(s3, [128, 64], F32, "sm")
            imp, Timp = sb(s3, [128, 64], F32, "imp")
            score, Tscore = sb(s3, [128, 64], F32, "score")
            work, Twork = sb(s3, [128, 64], F32, "work")
            top8, Ttop8 = sb(s3, [128, 16], F32, "top8")
            negm, Tnegm = sb(s3, [128, 64], F32, "negm")
            negT = [sb(s3, [128, 128], BF16, "negT") for _ in range(2)]
            oc, Toc = sb(s3, [128, 4, 128], F32, "oc")
            on, Ton = sb(s3, [128, 4, 128], F32, "on")
            jk, Tjk = sb(s3, [128, 128], F32, "jk")
            pS = [psb(s3, [128, 512], F32, "pS") for _ in range(2)]
            pO = [psb(s3, [128, 512], F32, "pO") for _ in range(4)]
            pX = [psb(s3, [128, 512], F32, "pX") for _ in range(2)]
            Tgtd = T("gtd")
            if dbg:
                obr, Tobr = sb(s3, [128, 4, 128], F32, "obr")
            s3a = ExitStack()
            rb33, Trb33 = sb(s3a, [33, 8], F32, "rb33")
            rb31, Trb31 = sb(s3a, [8, 1], F32, "rb31")
            DMA("sp", rb31[:], rel_bias[31:32, :].rearrange("a h -> h a"), [], [Trb31], **NCD)
            oh33c = [sb(s3a, [33, 512], F32, "oh33c") for _ in range(2)]
            gtsc = [sb(s3a, [8, 512], F32, "gtsc") for _ in range(2)]
            hkc, Thkc = sb(s3a, [128, NQ], F32, "hkc")
            hk, Thk = hkc[:].rearrange("p (a b) -> p a b", a=8), Thkc

            OP("pool", "memset", [], [Teexp], ap=eexp[64:128, :], constant=0.0)
            DMA("sp", eexp[0:64, :], eexp_d, [], [Teexp])
            for (nT_, TnT_) in negT:
                OP("pool", "memset", [], [TnT_], ap=nT_[:], constant=0.0)
            DMA("sp", cbnf[:], cbnf_d, [], [Tcbnf])
            DMA("sp", sadd[:], sadd_d, [], [Tsadd])
            DMA("sp", cbm[:], cb_d, [], [Tcbm])
            OP("dve", "memset", [], [Trb33], ap=rb33[32:33, :], constant=1.0)
            DMA("sp", rb33[0:32, :], rel_bias, [], [Trb33])
            for c0 in range(0, MTOT, 512):
                wd = min(512, MTOT - c0)
                px, Tpx = pX[(c0 // 512) % 2]
                oh33, Toh33 = oh33c[(c0 // 512) % 2]
                gts, Tgts = gtsc[(c0 // 512) % 2]
                DMA("sp", oh33[:, 0:wd], oh33_d[:, c0:c0 + wd], [], [Toh33])
                OP("pe", "matmul", [Trb33, Toh33], [Tpx], out=px[0:8, 0:wd], lhsT=rb33[:, :], rhs=oh33[:, 0:wd], start=True, stop=True)
                OP("dve", "tensor_scalar", [Tpx, Trb31], [Tgts], out=gts[:, 0:wd], in0=px[0:8, 0:wd], scalar1=rb31[:, 0:1], scalar2=None, op0=ALU.subtract)
                DMA("sp", gtd[:, c0:c0 + wd], gts[:, 0:wd], [Tgts], [Tgtd])

            def hankel(off, pstep, ncol, heads=True):
                if heads:
                    return bass.AP(tensor=gtd.tensor, offset=off, ap=[[pstep, 128], [MTOT, 8], [1, ncol]])
                return bass.AP(tensor=gtd.tensor, offset=off, ap=[[pstep, 128], [1, ncol]])

            for (BT, TBT, off) in ((BT0, TBT0, 0), (BT1, TBT1, 128), (BT4, TBT4, MSEG0)):
                DMA("sp", hk, hankel(off, 1, 128), [Tgtd], [Thk])
                hkf = hkc
                BTf = BT[:].rearrange("p a b -> p (a b)")
                for half in range(2):
                    px, Tpx = pX[half]
                    OP("pe", "matmul", [Tjmat, Thk], [Tpx], out=px[:], lhsT=jmat[:], rhs=hkf[:, half * 512:(half + 1) * 512], start=True, stop=True)
                    OP("dve", "tensor_copy", [Tpx], [TBT], out=BTf[:, half * 512:(half + 1) * 512], in_=px[:])
            for h in range(8):
                DMA("sp", hkc[:], hankel(h * MTOT + MSEG0 + MSEG1, 16, NQ, heads=False), [Tgtd], [Thkc])
                for half in range(2):
                    px, Tpx = pX[half]
                    OP("pe", "matmul", [Tjmat, Thkc], [Tpx], out=px[:], lhsT=jmat[:], rhs=hkc[:, half * 512:(half + 1) * 512], start=True, stop=True)
                    OP("dve", "tensor_copy", [Tpx], [TCB], out=CB[:, h, half * 512:(half + 1) * 512], in_=px[:])
            OP("dve", "tensor_copy", [Tchcol], [TBTC], out=BTC[:], in_=chcol[:].unsqueeze(2).to_broadcast([128, 8, 128]))

            P.barrier()
            s3a.close()
            for (tab, c0) in ((peer_u, 0), (peer_v, D)):
                for r0 in range(0, 16384, 1024):
                    P.dma("pool", dict(out=uv16[r0:r0 + 1024, c0:c0 + D], in_=tab[r0:r0 + 1024, :]), reads=[], writes=[Tuv16], nowaw=True)
            srr = [0]
            prr = [0]
            trr = [0]
            nrr = [0]

            def nextS():
                srr[0] += 1
                return pS[srr[0] % 2]

            def nextPT():
                prr[0] += 1
                return PT[prr[0] % 4]

            def nextTmp():
                trr[0] += 1
                return tmpS[trr[0] % 2]

            pending = []
            sm2 = [(sm, Tsm), sb(s3, [128, 64], F32, "sm1")]
            combos = [(qt, kvh) for qt in range(8) for kvh in range(2)]

            def emit_tr(qt_, kvh_):
                px, Tpx = pX[1]
                for g in range(4):
                    OP("pe", "transpose", [Ton, Tident], [Tpx], out=px[:, g * 128:(g + 1) * 128], in_=on[:, g, :], identity=ident[:])
                for g in range(4):
                    OP("dve", "tensor_scalar", [Tpx, Tagw], [TmixT], out=mixT[:, 4 * kvh_ + g, qt_ * 128:(qt_ + 1) * 128], in0=px[:, g * 128:(g + 1) * 128],
                       scalar1=agw[:, 4 * kvh_ + g:4 * kvh_ + g + 1], scalar2=None, op0=ALU.mult)

            def cmp_and_topk(ci):
                qt, kvh = combos[ci]
                qs = slice(qt * 128, (qt + 1) * 128)
                hs = slice(4 * kvh, 4 * kvh + 4)
                q4 = qT[:, hs, qs]
                ocmp, Tocmp = ocmp2[ci % 2]
                smc, Tsmc = sm2[ci % 2]
                for tl in range(2):
                    ps, Tps = pX[tl]
                    OP("pe", "matmul", [TkcT, TqT], [Tps], out=ps[:], lhsT=kcT[:, kvh, tl * 128:(tl + 1) * 128], rhs=q4, start=True, stop=True)
                    if tl == 0:
                        OP("act", "activation", [Tps, Tcvalid], [Tec], out=ec[:, 0, :], in_=ps[:], func=AF.Exp, scale=SCALE, bias=cvalid[:, 0:1])
                    else:
                        tm, Ttm = nextTmp()
                        OP("dve", "scalar_tensor_tensor", [Tps, TCB], [Ttm], out=tm[:].rearrange("p (a b) -> p a b", a=4), in0=ps[:].rearrange("p (a b) -> p a b", a=4),
                           scalar=SCALE, in1=CB[:, hs, qs], op0=ALU.mult, op1=ALU.add)
                        OP("act", "activation", [Ttm, Tcvalid], [Tec], out=ec[:, 1, :], in_=tm[:], func=AF.Exp, bias=cvalid[:, 1:2])
                for g in range(4):
                    px, Tpx = pX[g // 2]
                    c0 = 256 * (g % 2)
                    for tl in range(2):
                        OP("pe", "matmul", [Tec, Tvcaug], [Tpx], out=px[:, c0:c0 + 194], lhsT=ec[:, tl, g * 128:(g + 1) * 128], rhs=vcaug[:, tl, kvh, :],
                           start=(tl == 0), stop=(tl == 1))
                    evac(ocmp[:, g, :], px[:, c0:c0 + 194], [Tpx], [Tocmp])
                OP("dve", "tensor_scalar", [Tocmp], [Tsmc], out=smc[:, 0:4], in0=ocmp[:, :, 128], scalar1=1e-30, scalar2=None, op0=ALU.max)
                OP("dve", "reciprocal", [Tsmc], [Tsmc], out=smc[:, 0:4], in_=smc[:, 0:4])
                OP("dve", "tensor_scalar", [Tocmp, Tsmc], [Timp], out=imp[:], in0=ocmp[:, 0, 130:194], scalar1=smc[:, 0:1], scalar2=None, op0=ALU.mult)
                for g in range(1, 4):
                    OP("dve", "scalar_tensor_tensor", [Tocmp, Tsmc, Timp], [Timp], out=imp[:], in0=ocmp[:, g, 130:194], scalar=smc[:, g:g + 1],
                       in1=imp[:], op0=ALU.mult, op1=ALU.add)
                OP("dve", "tensor_tensor", [Timp, Tcbnf], [Tscore], out=score[:], in0=imp[:], in1=cbnf[:, qt, :], op=ALU.mult)
                OP("dve", "tensor_tensor", [Tscore, Tsadd], [Tscore], out=score[:], in0=score[:], in1=sadd[:, qt, :], op=ALU.add)
                OP("dve", "max", [Tscore], [Ttop8], out=top8[:, 0:8], in_=score[:])
                OP("dve", "match_replace", [Tscore, Ttop8], [Twork], out=work[:], in_to_replace=top8[:, 0:8], in_values=score[:], imm_value=-1e30)
                OP("dve", "max", [Twork], [Ttop8], out=top8[:, 8:16], in_=work[:])
                OP("dve", "tensor_scalar", [Tscore, Ttop8], [Twork], out=work[:], in0=score[:], scalar1=top8[:, 15:16], scalar2=None, op0=ALU.is_ge)
                OP("dve", "tensor_tensor", [Twork, Tcbm], [Twork], out=work[:], in0=work[:], in1=cbm[:, qt, :], op=ALU.mult)
                OP("dve", "tensor_scalar", [Twork], [Tnegm], out=negm[:], in0=work[:], scalar1=-NEG, scalar2=NEG, op0=ALU.mult, op1=ALU.add)

            def neg_transpose(ci):
                px, Tpx = pX[0]
                OP("pe", "transpose", [Tnegm, Tident], [Tpx], out=px[0:64, 0:128], in_=negm[:], identity=ident[:])
                nT, TnT = negT[ci % 2]
                OP("act", "activation", [Tpx], [TnT], out=nT[0:64, :], in_=px[0:64, 0:128], func=AF.Copy)

            cmp_and_topk(0)
            neg_transpose(0)
            for ci, (qt, kvh) in enumerate(combos):
                    qs = slice(qt * 128, (qt + 1) * 128)
                    hs = slice(4 * kvh, 4 * kvh + 4)
                    q4 = qT[:, hs, qs]
                    ocmp, Tocmp = ocmp2[ci % 2]
                    sm, Tsm = sm2[ci % 2]
                    nT, TnT = negT[ci % 2]
                    if pending:
                        emit_tr(*pending.pop())
                    def win_S(k):
                        wslot = qt + k
                        ps, Tps = nextS()
                        OP("pe", "matmul", [TkwT, TqT], [Tps], out=ps[:], lhsT=kwT[:, kvh, wslot * 128:(wslot + 1) * 128], rhs=q4, start=True, stop=True)
                        return ps, Tps
                    nxt = win_S(0)
                    for k in range(5):
                        BT, TBT = ((BT4, TBT4), (BTC, TBTC), (BTC, TBTC), (BT1, TBT1), (BT0, TBT0))[k]
                        wslot = qt + k
                        ps, Tps = nxt
                        if k < 4:
                            nxt = win_S(k + 1)
                        pt, Tpt = nextPT()
                        if k in (1, 2):
                            OP("act", "activation", [Tps, Tkvalid], [Tpt], out=pt[:], in_=ps[:], func=AF.Exp, scale=SCALE, bias=kvalid[:, 20 + wslot:21 + wslot])
                        else:
                            tm, Ttm = nextTmp()
                            OP("dve", "scalar_tensor_tensor", [Tps, TBT], [Ttm], out=tm[:].rearrange("p (a b) -> p a b", a=4), in0=ps[:].rearrange("p (a b) -> p a b", a=4),
                               scalar=SCALE, in1=BT[:, hs, :], op0=ALU.mult, op1=ALU.add)
                            OP("act", "activation", [Ttm, Tkvalid], [Tpt], out=pt[:], in_=tm[:], func=AF.Exp, bias=kvalid[:, 20 + wslot:21 + wslot])
                        for g in range(4):
                            po, Tpo = pO[g]
                            OP("pe", "matmul", [Tpt, Tvw], [Tpo], out=po[:, 0:130], lhsT=pt[:, g * 128:(g + 1) * 128], rhs=vw[:, wslot, kvh, :],
                               start=(k == 0), stop=(k == 4))
                    for g in range(4):
                        evac(owin[:, g, :], pO[g][0][:, 0:130], [pO[g][1]], [Towin])
                    last = 24 + qt
                    nTb = nT[:].unsqueeze(1).to_broadcast([128, 4, 128])

                    def sel_S(slot):
                        ps, Tps = nextS()
                        OP("pe", "matmul", [TksT, TqT], [Tps], out=ps[:], lhsT=ksT[:, kvh, slot * 128:(slot + 1) * 128], rhs=q4, start=True, stop=False)
                        OP("pe", "matmul", [Teexp, TnT], [Tps], out=ps[:], lhsT=eexp[:, slot * 128:(slot + 1) * 128], rhs=nTb, start=False, stop=True)
                        return ps, Tps
                    nxt = sel_S(0)
                    for slot in range(last + 1):
                        ps, Tps = nxt
                        if slot < last:
                            nxt = sel_S(slot + 1)
                        pt, Tpt = nextPT()
                        rel = slot - last
                        if rel >= -1:
                            BT, TBT = (BT0, TBT0) if rel == 0 else (BT1, TBT1)
                            tm, Ttm = nextTmp()
                            OP("dve", "scalar_tensor_tensor", [Tps, TBT], [Ttm], out=tm[:].rearrange("p (a b) -> p a b", a=4),
                               in0=ps[:].rearrange("p (a b) -> p a b", a=4), scalar=SCALE, in1=BT[:, hs, :], op0=ALU.mult, op1=ALU.add)
                            OP("act", "activation", [Ttm], [Tpt], out=pt[:], in_=tm[:], func=AF.Exp)
                        else:
                            OP("act", "activation", [Tps], [Tpt], out=pt[:], in_=ps[:], func=AF.Exp, scale=SCALE)
                        for g in range(4):
                            po, Tpo = pO[g]
                            OP("pe", "matmul", [Tpt, Tvs], [Tpo], out=po[:, 0:130], lhsT=pt[:, g * 128:(g + 1) * 128], rhs=vs[:, slot, kvh, :],
                               start=(slot == 0), stop=(slot == last))
                        if slot == 3 and ci + 1 < len(combos):
                            cmp_and_topk(ci + 1)
                    if ci + 1 < len(combos):
                        neg_transpose(ci + 1)
                    for g in range(4):
                        evac(osel[:, g, :], pO[g][0][:, 0:130], [pO[g][1]], [Tosel])
                    OP("dve", "reciprocal", [Tosel], [Tsm], out=sm[:, 4:8], in_=osel[:, :, 128])
                    OP("dve", "reciprocal", [Towin], [Tsm], out=sm[:, 8:12], in_=owin[:, :, 128])
                    gv = gsig[:, qt, 12 * kvh:12 * kvh + 12].rearrange("p (g b) -> p g b", b=3)
                    for br in range(3):
                        OP("dve", "tensor_tensor", [Tsm, Tgsig], [Tsm], out=sm[:, 12 + 4 * br:16 + 4 * br], in0=sm[:, 4 * br:4 * br + 4], in1=gv[:, :, br], op=ALU.mult)
                    if dbg and stop_after == 3 and qt in (0, 3, 7):
                        for br, (ob, Tob) in enumerate(((ocmp, Tocmp), (osel, Tosel), (owin, Towin))):
                            for g in range(4):
                                OP("dve", "tensor_scalar", [Tob, Tsm], [Tobr], out=obr[:, g, :], in0=ob[:, g, 0:128], scalar1=sm[:, 4 * br + g:4 * br + g + 1],
                                   scalar2=None, op0=ALU.mult)
                            dump("obr_%d_%d_%d" % (qt, kvh, br), obr[:], Tobr, [128, 4, 128], F32)
                    for g in range(4):
                        OP("dve", "tensor_scalar", [Tocmp, Tsm], [Toc], out=oc[:, g, :], in0=ocmp[:, g, 0:128], scalar1=sm[:, 12 + g:13 + g], scalar2=None, op0=ALU.mult)
                        OP("dve", "scalar_tensor_tensor", [Tosel, Tsm, Toc], [Toc], out=oc[:, g, :], in0=osel[:, g, 0:128], scalar=sm[:, 16 + g:17 + g], in1=oc[:, g, :],
                           op0=ALU.mult, op1=ALU.add)
                        OP("dve", "scalar_tensor_tensor", [Towin, Tsm, Toc], [Toc], out=oc[:, g, :], in0=owin[:, g, 0:128], scalar=sm[:, 20 + g:21 + g], in1=oc[:, g, :],
                           op0=ALU.mult, op1=ALU.add)
                        OP("dve", "scalar_tensor_tensor", [Toc], [Tjk, Tsm], out=jk[:], in0=oc[:, g, :], scalar=1.0, in1=oc[:, g, :],
                           op0=ALU.mult, op1=ALU.mult, accum_out=sm[:, 24 + g:25 + g])
                    OP("dve", "tensor_scalar", [Tsm], [Tsm], out=sm[:, 28:32], in0=sm[:, 24:28], scalar1=1.0 / 128, scalar2=EPS, op0=ALU.mult, op1=ALU.add)
                    OP("act", "activation", [Tsm], [Tsm], out=sm[:, 28:32], in_=sm[:, 28:32], func=AF.Sqrt)
                    OP("dve", "reciprocal", [Tsm], [Tsm], out=sm[:, 24:28], in_=sm[:, 28:32])
                    OP("dve", "tensor_tensor", [Toc, Tsm], [Ton], out=on[:], in0=oc[:], in1=sm[:, 24:28].unsqueeze(2).to_broadcast([128, 4, 128]), op=ALU.mult)
                    pending.append((qt, kvh))
            emit_tr(*pending.pop())
            if stop_after == 3:
                dump("mixa", mixT[:, 0:8, :], TmixT, [128, 8, NQ], BF16)
            P.barrier()
        sA.close()
        if stop_after == 3:
            finish()
            return nc, dbg_outs
        h2, Th2 = sb(st, [128, 8, D], F32, "h2")
        for tile in range(8):
            DMA("sp", h2[:, tile, :], xs[3072 + tile * 128:3072 + (tile + 1) * 128, :], [], [Th2])
        with ExitStack() as s4:
            wos = [sb(s4, [128, 16, 512], F32, "wos") for _ in range(2)]
            wob = [sb(s4, [128, 16, 512], BF16, "wob") for _ in range(2)]
            pM4 = [psb(s4, [128, 512], F32, "pM4") for _ in range(4)]
            m4 = [0]
            for cc in range(4):
                stg, Tstg = wos[cc % 2]
                wb, Twb = wob[cc % 2]
                cs = slice(cc * 512, (cc + 1) * 512)
                DMA("sp", stg[:], w_out[:, cs].rearrange("(k p) c -> p k c", p=128), [], [Tstg])
                for hh in range(2):
                    OP("pool" if hh == 0 else "act", "tensor_copy" if hh == 0 else "activation", [Tstg], [Twb],
                       **(dict(out=wb[:, 8 * hh:8 * hh + 8, :], in_=stg[:, 8 * hh:8 * hh + 8, :]) if hh == 0 else
                          dict(out=wb[:, 8 * hh:8 * hh + 8, :], in_=stg[:, 8 * hh:8 * hh + 8, :], func=AF.Copy)))
                for tile in range(8):
                    m4[0] += 1
                    pm, Tpm = pM4[m4[0] % 4]
                    for kc in range(16):
                        OP("pe", "matmul", [TmixT, Twb], [Tpm], out=pm[:], lhsT=mixT[:, kc, tile * 128:(tile + 1) * 128], rhs=wb[:, kc, :],
                           start=(kc == 0), stop=(kc == 15))
                    OP("dve", "tensor_tensor", [Tpm, Th2], [Th2], out=h2[:, tile, cs], in0=pm[:], in1=h2[:, tile, cs], op=ALU.add)
            if stop_after == 4:
                dump("h2", h2[:], Th2, [128, 8, D], F32)
            P.barrier()
        if stop_after == 4:
            finish()
            return nc, dbg_outs

        xn2, Txn2 = sb(st, [128, 8, D], BF16, "xn2")
        eidx, Teidx = sb(st, [128, 8, 128], U32, "eidx")
        gates, Tgates = sb(st, [128, 8, 128], F32, "gates")
        iota16, Tiota = sb(st, [128, 16], F32, "iota16")
        DMA("sp", iota16[:], iota16_d, [], [Tiota])
        xn2T, Txn2T = mixT, T("xn2T")
        with ExitStack() as s4b:
            eqB, TeqB = sb(s4b, [128, 8, 16, 16], F32, "eqB")
            g2b, Tg2b = eqB[:].rearrange("p t a b -> p (t a b)"), TeqB
            sq4, Tsq4 = sb(s4b, [128, 2], F32, "sq4")
            SKT, TSKT = sb(s4b, [128, 16, 128], BF16, "SKT")
            wqs = [sb(s4b, [128, 16, 128], F32, "wqs")] * 2
            wqb = [sb(s4b, [128, 16, 128], BF16, "wqb") for _ in range(2)]
            skst, Tskst = wqs[0]
            skb, Tskb = wqb[0]
            qpT = [sb(s4b, [128, NQ], BF16, "qpT") for _ in range(2)]
            sw = [sb(s4b, [128, 128], F32, "sw") for _ in range(2)]
            sw2, Tsw2 = sb(s4b, [128, 128], F32, "sw2")
            sv, Tsv = sb(s4b, [128, 8, 16, 16], F32, "sv")
            si, Tsi = sb(s4b, [128, 8, 16, 16], U32, "si")
            pT4 = [psb(s4b, [128, 8, 128], BF16, "pT4") for _ in range(2)]
            pM5 = [psb(s4b, [128, 512], F32, "pM5") for _ in range(4)]
            pS5 = [psb(s4b, [128, 512], F32, "pS5") for _ in range(2)]
            DMA("sp", g2b, ffn_norm_w.partition_broadcast(128), [], [Tg2b], **NCD)
            for tile in range(8):
                OP("act", "activation", [Th2], [Txn2, Tsq4], out=xn2[:, tile, :], in_=h2[:, tile, :], func=AF.Square, accum_out=sq4[:, 0:1])
                rstd_from_sumsq(sq4, Tsq4, D)
                OP("dve", "scalar_tensor_tensor", [Th2, Tsq4, Tg2b], [Txn2], out=xn2[:, tile, :], in0=h2[:, tile, :], scalar=sq4[:, 0:1], in1=g2b,
                   op0=ALU.mult, op1=ALU.mult)
                for half in range(2):
                    pt, Tpt = pT4[half]
                    for k8 in range(8):
                        kc = half * 8 + k8
                        OP("pe", "transpose", [Txn2, Tidentb], [Tpt], out=pt[:, k8, :], in_=xn2[:, tile, kc * 128:(kc + 1) * 128], identity=identb[:])
                    evac(xn2T[:, half * 8:(half + 1) * 8, tile * 128:(tile + 1) * 128], pt[:], [Tpt], [Txn2T])
            DMA("sp", skst[:], peer_sk.rearrange("h n d -> n h d"), [], [Tskst])
            OP("pool", "tensor_copy", [Tskst], [Tskb], out=skb[:], in_=skst[:])
            for half in range(2):
                pt, Tpt = pT4[half]
                for k8 in range(8):
                    OP("pe", "transpose", [Tskb, Tidentb], [Tpt], out=pt[:, k8, :], in_=skb[:, half * 8 + k8, :], identity=identb[:])
                evac(SKT[:, half * 8:(half + 1) * 8, :], pt[:], [Tpt], [TSKT])
            candB, TcandB = sb(s4b, [128, 8, 256], F32, "candB")
            cand2B, Tcand2B = sb(s4b, [128, 256], F32, "cand2B")
            cvB, TcvB = sb(s4b, [128, 8, 16], F32, "cvB")
            ciB, TciB = sb(s4b, [128, 8, 16], U32, "ciB")
            pabB, TpabB = sb(s4b, [128, 2, 8, 16], U32, "pabB")
            rf, Trf = sb(s4b, [128, 8, 8, 16], F32, "rf")
            rsm, Trsm = sb(s4b, [128, 2, 8], F32, "rsm")
            B4 = [128, 8, 16, 16]

            def route_h(h):
                c1, c2 = 2 * h, 2 * h + 1
                OP("dve", "tensor_tensor", [Tsv], [TcandB], out=candB[:].rearrange("p t (a b) -> p t a b", a=16),
                   in0=sv[:, :, c1, :].unsqueeze(3).to_broadcast(B4), in1=sv[:, :, c2, :].unsqueeze(2).to_broadcast(B4), op=ALU.add)
                for t in range(8):
                    OP("dve", "max", [TcandB], [TcvB], out=cvB[:, t, 0:8], in_=candB[:, t, :])
                    OP("dve", "max_index", [TcandB, TcvB], [TciB], out=ciB[:, t, 0:8], in_max=cvB[:, t, 0:8], in_values=candB[:, t, :])
                    OP("dve", "match_replace", [TcandB, TcvB], [Tcand2B], out=cand2B[:], in_to_replace=cvB[:, t, 0:8], in_values=candB[:, t, :], imm_value=-1e30)
                    OP("dve", "max", [Tcand2B], [TcvB], out=cvB[:, t, 8:16], in_=cand2B[:])
                    OP("dve", "max_index", [Tcand2B, TcvB], [TciB], out=ciB[:, t, 8:16], in_max=cvB[:, t, 8:16], in_values=cand2B[:])
                OP("dve", "tensor_single_scalar", [TciB], [TpabB], out=pabB[:, 0], in_=ciB[:], scalar=4, op=ALU.logical_shift_right)
                OP("dve", "tensor_single_scalar", [TciB], [TpabB], out=pabB[:, 1], in_=ciB[:], scalar=15, op=ALU.bitwise_and)
                OP("dve", "tensor_copy", [TpabB], [Trf], out=rf[:, 0:2], in_=pabB[:])
                OP("dve", "tensor_copy", [Tsi], [Trf], out=rf[:, 2], in_=si[:, :, c1, :])
                OP("dve", "tensor_copy", [Tsi], [Trf], out=rf[:, 3], in_=si[:, :, c2, :])
                for (src, sif, dst) in ((0, 2, 4), (1, 3, 5)):
                    OP("dve", "tensor_tensor", [Trf, Tiota], [TeqB], out=eqB[:], in0=rf[:, src].unsqueeze(3).to_broadcast(B4),
                       in1=iota16[:].unsqueeze(1).unsqueeze(1).to_broadcast(B4), op=ALU.is_equal)
                    OP("dve", "tensor_tensor", [TeqB, Trf], [TeqB], out=eqB[:], in0=eqB[:], in1=rf[:, sif].unsqueeze(2).to_broadcast(B4), op=ALU.mult)
                    OP("dve", "tensor_reduce", [TeqB], [Trf], out=rf[:, dst], in_=eqB[:], axis=AX.X, op=ALU.add)
                OP("dve", "scalar_tensor_tensor", [Trf], [Trf], out=rf[:, 6], in0=rf[:, 4], scalar=128.0, in1=rf[:, 5], op0=ALU.mult, op1=ALU.add)
                OP("dve", "tensor_scalar", [Trf], [Trf], out=rf[:, 6], in0=rf[:, 6], scalar1=0.0, scalar2=16383.0, op0=ALU.max, op1=ALU.min)
                OP("dve", "tensor_copy", [Trf], [Teidx], out=eidx[:, :, 16 * h:16 * h + 16], in_=rf[:, 6])
                OP("dve", "tensor_tensor", [TcvB], [Trf], out=rf[:, 7], in0=cvB[:], in1=cvB[:, :, 0:1].to_broadcast([128, 8, 16]), op=ALU.subtract)
                OP("act", "activation", [Trf], [Trf], out=rf[:, 7], in_=rf[:, 7], func=AF.Exp)
                OP("dve", "tensor_reduce", [Trf], [Trsm], out=rsm[:, 0], in_=rf[:, 7], axis=AX.X, op=ALU.add)
                OP("dve", "reciprocal", [Trsm], [Trsm], out=rsm[:, 1], in_=rsm[:, 0])
                OP("dve", "tensor_tensor", [Trf, Trsm], [Tgates], out=gates[:, :, 16 * h:16 * h + 16], in0=rf[:, 7],
                   in1=rsm[:, 1].unsqueeze(2).to_broadcast([128, 8, 16]), op=ALU.mult)

            m5 = [0]
            for hc in range(16):
                stg, Tstg = wqs[hc % 2]
                wb, Twb = wqb[hc % 2]
                qp, Tqp = qpT[hc % 2]
                DMA("sp", stg[:], peer_wq[:, hc * 128:(hc + 1) * 128].rearrange("(k p) c -> p k c", p=128), [], [Tstg])
                OP("pool", "tensor_copy", [Tstg], [Twb], out=wb[:], in_=stg[:])
                for half in range(2):
                    m5[0] += 1
                    pm, Tpm = pM5[m5[0] % 4]
                    for kc in range(16):
                        OP("pe", "matmul", [Twb, Txn2T], [Tpm], out=pm[:], lhsT=wb[:, kc, :], rhs=xn2T[:, kc, half * 512:(half + 1) * 512],
                           start=(kc == 0), stop=(kc == 15))
                    evac(qp[:, half * 512:(half + 1) * 512], pm[:], [Tpm], [Tqp])
                for tile in range(8):
                    ps, Tps = pS5[tile % 2]
                    swt, Tswt = sw[tile % 2]
                    OP("pe", "matmul", [Tqp, TSKT], [Tps], out=ps[:, 0:128], lhsT=qp[:, tile * 128:(tile + 1) * 128], rhs=SKT[:, hc, :], start=True, stop=True)
                    OP("act", "activation", [Tps], [Tswt], out=swt[:], in_=ps[:, 0:128], func=AF.Copy)
                    OP("dve", "max", [Tswt], [Tsv], out=sv[:, tile, hc, 0:8], in_=swt[:])
                    OP("dve", "max_index", [Tswt, Tsv], [Tsi], out=si[:, tile, hc, 0:8], in_max=sv[:, tile, hc, 0:8], in_values=swt[:])
                    OP("dve", "match_replace", [Tswt, Tsv], [Tsw2], out=sw2[:], in_to_replace=sv[:, tile, hc, 0:8], in_values=swt[:], imm_value=-1e30)
                    OP("dve", "max", [Tsw2], [Tsv], out=sv[:, tile, hc, 8:16], in_=sw2[:])
                    OP("dve", "max_index", [Tsw2, Tsv], [Tsi], out=si[:, tile, hc, 8:16], in_max=sv[:, tile, hc, 8:16], in_values=sw2[:])
                if hc % 2 == 1:
                    route_h(hc // 2)
            if stop_after == 5:
                dump("eidx", eidx[:], Teidx, [128, 8, 128], U32)
                dump("gates", gates[:], Tgates, [128, 8, 128], F32)
            P.barrier()
        if stop_after == 5:
            finish()
            return nc, dbg_outs

        with ExitStack() as s5:
            gfin, Tgfin = sb(s5, [128, D], F32, "gfin")
            mixG = mixT[:].rearrange("p a b -> p (a b)").rearrange("p (k c) -> p k c", k=4)
            gb = [(mixG[:, i, :], T("gbm%d" % i)) for i in range(4)]
            gb += [(lambda t: (t[0][:], t[1]))(sb(s5, [128, 2 * D], BF16, "gb")) for _ in range(4)]
            acc, Tacc = sb(s5, [128, D], F32, "acc")
            junk5, Tjunk5 = sb(s5, [128, D], BF16, "junk5")
            aw2 = [sb(s5, [128, 128], F32, "aw") for _ in range(2)]
            ww2 = [sb(s5, [128, 128], F32, "ww") for _ in range(2)]
            t12 = [sb(s5, [128, 2], F32, "t1") for _ in range(2)]
            sq5, Tsq5 = sb(s5, [128, 2], F32, "sq5")
            dg = [sb(s5, [128, 128], BF16, "dg") for _ in range(8)]
            pV, TpV = psb(s5, [128, D], F32, "pV")
            DMA("sp", gfin[:], final_norm_w.partition_broadcast(128), [], [Tgfin], **NCD)
            GS = 2
            NG = 128 // GS
            gi = [0]
            bufs = {}

            def dots(tile, grp):
                a_, Ta_ = aw2[tile % 2]
                lst = []
                for j in range(GS):
                    slot = grp * GS + j
                    gi[0] += 1
                    g_, Tg_ = gb[gi[0] % 8]
                    P.dma("pool", dict(out=g_, out_offset=None, in_=uv16,
                                       in_offset=bass.IndirectOffsetOnAxis(ap=eidx[:, tile, slot:slot + 1], axis=0)),
                          reads=[Teidx, Tuv16], writes=[Tg_], name="indirect_dma_start")
                    OP("dve", "scalar_tensor_tensor", [Tg_, Txn2], [Tjunk5, Ta_], out=junk5[:], in0=g_[:, 0:D], scalar=1.0, in1=xn2[:, tile, :],
                       op0=ALU.mult, op1=ALU.mult, accum_out=a_[:, slot:slot + 1])
                    lst.append((g_, Tg_))
                bufs[(tile, grp)] = lst

            def gelu_pre(tile, grp):
                a_, Ta_ = aw2[tile % 2]
                t1, Tt1 = t12[grp % 2]
                av = a_[:, grp * GS:(grp + 1) * GS]
                OP("dve", "tensor_tensor", [Ta_], [Tt1], out=t1[:], in0=av, in1=av, op=ALU.mult)
                OP("dve", "tensor_tensor", [Tt1, Ta_], [Tt1], out=t1[:], in0=t1[:], in1=av, op=ALU.mult)
                OP("dve", "scalar_tensor_tensor", [Tt1, Ta_], [Tt1], out=t1[:], in0=t1[:], scalar=0.044715, in1=av, op0=ALU.mult, op1=ALU.add)
                OP("act", "activation", [Tt1], [Tt1], out=t1[:], in_=t1[:], func=AF.Tanh, scale=0.7978845608028654)

            def post_v(tile, grp):
                a_, Ta_ = aw2[tile % 2]
                w_, Tw_ = ww2[tile % 2]
                t1, Tt1 = t12[grp % 2]
                sl = slice(grp * GS, (grp + 1) * GS)
                OP("dve", "tensor_scalar", [Tt1], [Tt1], out=t1[:], in0=t1[:], scalar1=0.5, scalar2=0.5, op0=ALU.mult, op1=ALU.add)
                OP("dve", "tensor_tensor", [Tt1, Ta_], [Tt1], out=t1[:], in0=t1[:], in1=a_[:, sl], op=ALU.mult)
                OP("dve", "tensor_tensor", [Tt1, Tgates], [Tw_], out=w_[:, sl], in0=t1[:], in1=gates[:, tile, sl], op=ALU.mult)
                for j, (g_, Tg_) in enumerate(bufs.pop((tile, grp))):
                    slot = grp * GS + j
                    dk, Tdk = dg[slot % 8]
                    OP("act", "activation", [Tident, Tw_], [Tdk], out=dk[:], in_=ident[:], func=AF.Copy, scale=w_[:, slot:slot + 1])
                    for bq in range(4):
                        OP("pe", "matmul", [Tdk, Tg_], [TpV], out=pV[:, bq * 512:(bq + 1) * 512], lhsT=dk[:], rhs=g_[:, D + bq * 512:D + (bq + 1) * 512],
                           start=(slot == 0), stop=(slot == 127))

            def final(tile):
                OP("dve", "tensor_tensor", [TpV, Th2], [Tacc], out=acc[:], in0=pV[:], in1=h2[:, tile, :], op=ALU.add)
                OP("act", "activation", [Tacc], [Tjunk5, Tsq5], out=junk5[:], in_=acc[:], func=AF.Square, accum_out=sq5[:, 0:1])
                rstd_from_sumsq(sq5, Tsq5, D)
                OP("dve", "scalar_tensor_tensor", [Tacc, Tsq5, Tgfin], [Tacc], out=acc[:], in0=acc[:], scalar=sq5[:, 0:1], in1=gfin[:], op0=ALU.mult, op1=ALU.mult)
                DMA("sp", y_out[tile * 128:(tile + 1) * 128, :], acc[:], [Tacc], [Tout])

            prev = None
            for tile in range(8):
                for grp in range(NG):
                    dots(tile, grp)
                    gelu_pre(tile, grp)
                    if prev is not None:
                        post_v(*prev)
                        if prev[1] == NG - 1:
                            final(prev[0])
                    prev = (tile, grp)
            post_v(*prev)
            final(prev[0])
        finish()
        return nc, dbg_outs
        raise NotImplementedError
    return nc, dbg_outs


def _prep_inputs(inputs):
    x = np.asarray(inputs["x"], np.float32)
    cst = _consts()
    shared = {
        "attn_norm_w": np.ascontiguousarray(inputs["attn_norm_w"][0]),
        "w_in": np.ascontiguousarray(inputs["w_in"][0]),
        "w_cmp_k": np.ascontiguousarray(inputs["w_cmp_k"][0]),
        "w_cmp_v": np.ascontiguousarray(inputs["w_cmp_v"][0]),
        "cmp_pos": np.ascontiguousarray(inputs["cmp_pos"][0]),
        "conv_w": np.ascontiguousarray(inputs["conv_w"][0]),
        "conv_b": np.ascontiguousarray(inputs["conv_b"][0]),
        "attn_group_norm_w": np.ascontiguousarray(inputs["attn_group_norm_w"][0]),
        "conv_group_norm_w": np.ascontiguousarray(inputs["conv_group_norm_w"][0]),
        "w_out": np.ascontiguousarray(inputs["w_out"][0]),
        "rel_bias": np.ascontiguousarray(inputs["rel_bias"]),
        "ffn_norm_w": np.ascontiguousarray(inputs["ffn_norm_w"][0]),
        "peer_wq": np.ascontiguousarray(inputs["peer_wq"][0]),
        "peer_subkeys": np.ascontiguousarray(inputs["peer_subkeys"][0]).reshape(16, 128, 128),
        "peer_u": np.ascontiguousarray(inputs["peer_u"][0]),
        "peer_v": np.ascontiguousarray(inputs["peer_v"][0]),
        "final_norm_w": np.ascontiguousarray(inputs["final_norm_w"]),
    }
    shared = {k: np.asarray(v, np.float32) for k, v in shared.items()}
    shared.update(cst)
    in_maps = []
    for core in range(8):
        b, c = core // 4, core % 4
        q0 = 1024 * c
        ws = q0 - 3072
        xw = np.zeros((S, D), np.float32)
        lo = max(ws, 0)
        xw[lo - ws:, :] = x[b, lo:q0 + 1024, :]
        m = dict(shared)
        m["xs"] = xw
        m.update(_core_consts(c))
        in_maps.append(m)
    return in_maps


def kernel(**inputs):
    in_maps = _prep_inputs(inputs)
    nc, _ = build()
    res = run_bass_kernel_spmd(nc, in_maps, core_ids=list(range(8)))
    out = np.zeros((2, S, D), np.float32)
    for core in range(8):
        b, c = core // 4, core % 4
        out[b, 1024 * c:1024 * (c + 1), :] = res.results[core]["y"]
    return out
```

```python
import math
from contextlib import ExitStack
import numpy as np
import ml_dtypes
import concourse.bass as bass
import concourse.mybir as mybir
from concourse.bass_utils import run_bass_kernel_spmd

F32 = mybir.dt.float32
BF16 = mybir.dt.bfloat16
U32 = mybir.dt.uint32
ALU = mybir.AluOpType
AF = mybir.ActivationFunctionType
AX = mybir.AxisListType

D = 2048
S = 4096
NQ = 1024
NEG = -30000.0
EPS = 1e-6
SCALE = 128 ** -0.5
DIN = 5656
MSEG0, MSEG1, MSEG2 = 384, 256, 3072
MTOT = MSEG0 + MSEG1 + MSEG2
NDBG = []


class T:
    __slots__ = ("name", "w", "r", "dsem")

    def __init__(self, name):
        self.name = name
        self.w = {}
        self.r = {}
        self.dsem = None


class Prog:
    ENG = ("pe", "dve", "act", "pool", "sp")

    def __init__(self, nc, stack):
        self.nc = nc
        self.stack = stack
        self.stream = {k: [] for k in self.ENG}
        self.cnt = {}
        self.sems = {}
        self.known = {k: {} for k in self.ENG}
        for k in self.ENG:
            self.sems[k] = stack.enter_context(nc.semaphore("s_" + k))
            self.cnt[k] = 0
        self.ndsem = 0

    def _dsem(self, t):
        if t.dsem is None:
            key = "d%d" % self.ndsem
            self.ndsem += 1
            self.sems[key] = self.stack.enter_context(self.nc.semaphore(key))
            self.cnt[key] = 0
            t.dsem = key
        return t.dsem

    def _waits(self, eng, reads, writes, nowaw=False):
        need = {}

        def add(k, v):
            if v > need.get(k, 0):
                need[k] = v
        for t in reads:
            for k, v in t.w.items():
                add(k, v)
        for t in writes:
            if not nowaw:
                for k, v in t.w.items():
                    add(k, v)
            for k, v in t.r.items():
                add(k, v)
        kn = self.known[eng]
        out = []
        for k, v in need.items():
            if k == eng and eng == "pe":
                continue
            if kn.get(k, 0) < v:
                kn[k] = v
                out.append((k, v))
        return out

    def op(self, eng, name, kw, reads=(), writes=()):
        waits = self._waits(eng, reads, writes)
        self.cnt[eng] += 1
        v = self.cnt[eng]
        self.stream[eng].append((waits, (name, kw), eng, 1))
        for t in reads:
            t.r[eng] = v
        for t in writes:
            t.w[eng] = v

    def dma(self, eng, kw, reads=(), writes=(), nowaw=False, name="dma_start"):
        assert len(writes) == 1
        t = writes[0]
        key = self._dsem(t)
        waits = self._waits(eng, reads, writes, nowaw=nowaw)
        self.cnt[key] += 16
        v = self.cnt[key]
        self.stream[eng].append((waits, (name, kw), key, 16))
        for s in reads:
            s.r[key] = v
        t.w[key] = v

    def barrier(self):
        snap = dict(self.cnt)
        for e in self.ENG:
            kn = self.known[e]
            waits = []
            for k, v in snap.items():
                if v > 0 and kn.get(k, 0) < v:
                    kn[k] = v
                    waits.append((k, v))
            self.stream[e].append((waits, None, None, 0))

    def final_wait(self, eng, tiles):
        waits = self._waits(eng, tiles, ())
        self.stream[eng].append((waits, None, None, 0))

    def emit(self):
        nc = self.nc
        with nc.Block() as block:
            def mk(name):
                def body(e):
                    for waits, fn, key, inc in self.stream[name]:
                        for k, v in waits:
                            e.wait_ge(self.sems[k], v)
                        if fn is not None:
                            getattr(e, fn[0])(**fn[1]).then_inc(self.sems[key], inc)
                return body
            block.tensor(mk("pe"))
            block.vector(mk("dve"))
            block.scalar(mk("act"))
            block.gpsimd(mk("pool"))
            block.sync(mk("sp"))


def _t5_bucket_np(n):
    n = np.maximum(n, 0)
    nf = np.maximum(n, 1).astype(np.float32)
    large = 16 + (np.log(nf / np.float32(16)) / np.float32(math.log(8.0)) * np.float32(16)).astype(np.int32)
    large = np.minimum(large, 31)
    return np.where(n < 16, n, large)


def _consts():
    oh = np.zeros((33, MTOT), np.float32)
    m = np.arange(MSEG0)
    d = m - 127
    bk = _t5_bucket_np(d)
    for i in range(MSEG0):
        if d[i] < 0:
            oh[32, i] = NEG
        else:
            oh[bk[i], i] = 1.0
    for i in range(MSEG1):
        if i - 127 < 0:
            oh[31, MSEG0 + i] = 1.0
        else:
            oh[32, MSEG0 + i] = NEG
    m = np.arange(MSEG2)
    d = m - 1023
    bk = _t5_bucket_np(d)
    base = MSEG0 + MSEG1
    for i in range(MSEG2):
        if d[i] < 0:
            oh[32, base + i] = NEG
        else:
            oh[bk[i], base + i] = 1.0
    ident = np.eye(128, dtype=np.float32)
    jmat = ident[::-1].copy()
    eexp = np.zeros((64, 4096), np.float32)
    eexp[np.arange(4096) // 64, np.arange(4096)] = 1.0
    ov = np.zeros((256, 64), np.float32)
    for n in range(255):
        pos = n * 16 + np.arange(32)
        for p in pos:
            ov[n + 1, p // 64] += 1.0 / 32
    ovs = ov.reshape(2, 128, 64).transpose(1, 0, 2).copy()
    iota16 = np.tile(np.arange(16, dtype=np.float32)[None, :], (128, 1))
    return dict(iota16=iota16, oh33=oh, ident=ident, jmat=jmat, identb=ident.astype(ml_dtypes.bfloat16),
                eexp=eexp.astype(ml_dtypes.bfloat16), ovs=ovs)


def _core_consts(c):
    q0 = 1024 * c
    ws = q0 - 3072
    kvalid = np.zeros((128, 32), np.float32)
    for s in range(32):
        if ws + 128 * s < 0:
            kvalid[:, s] = NEG
    cvalid = np.zeros((128, 2), np.float32)
    for tl in range(2):
        for p in range(128):
            n_rel = tl * 128 + p - 1
            n_abs = n_rel + ws // 16
            if n_rel < 0 or n_abs < 0:
                cvalid[p, tl] = NEG
    t_abs = q0 + np.arange(1024)
    j_abs = np.arange(64) + ws // 64
    exists = j_abs >= 0
    causal = exists[None, :] & (j_abs[None, :] * 64 <= t_abs[:, None])
    blk_t = t_abs // 64
    forced = exists[None, :] & ((j_abs[None, :] == 0) | (j_abs[None, :] == blk_t[:, None]) | (j_abs[None, :] == blk_t[:, None] - 1))
    cbnf = (causal & ~forced).astype(np.float32)
    sadd = np.where(forced, 5.0, np.where(causal, 0.0, -1.0)).astype(np.float32)
    cb = causal.astype(np.float32)
    f = lambda a: a.reshape(8, 128, 64).transpose(1, 0, 2).copy()
    return dict(kvalid=kvalid, cvalid=cvalid, cbnf=f(cbnf), sadd=f(sadd), cb=f(cb))


def build(stop_after=None, dbg=False):
    nc = bass.Bass("TRN2", target_bir_lowering=False)
    din = lambda n, shp, dt=F32: nc.dram_tensor(n, list(shp), dt, kind="ExternalInput").ap()
    xs = din("xs", [S, D])
    attn_norm_w = din("attn_norm_w", [D])
    w_in = din("w_in", [D, DIN])
    w_cmp_k = din("w_cmp_k", [32, 128, 128])
    w_cmp_v = din("w_cmp_v", [32, 128, 128])
    cmp_pos = din("cmp_pos", [32, 128])
    conv_w = din("conv_w", [3, 1024])
    conv_b = din("conv_b", [1024])
    agnw = din("attn_group_norm_w", [1024])
    cgnw = din("conv_group_norm_w", [1024])
    w_out = din("w_out", [D, D])
    rel_bias = din("rel_bias", [32, 8])
    ffn_norm_w = din("ffn_norm_w", [D])
    peer_wq = din("peer_wq", [D, D])
    peer_sk = din("peer_subkeys", [16, 128, 128])
    peer_u = din("peer_u", [16384, D])
    peer_v = din("peer_v", [16384, D])
    final_norm_w = din("final_norm_w", [D])
    oh33_d = din("oh33", [33, MTOT])
    ident_d = din("ident", [128, 128])
    jmat_d = din("jmat", [128, 128])
    identb_d = din("identb", [128, 128], BF16)
    eexp_d = din("eexp", [64, 4096], BF16)
    ovs_d = din("ovs", [128, 2, 64])
    kvalid_d = din("kvalid", [128, 32])
    cvalid_d = din("cvalid", [128, 2])
    cbnf_d = din("cbnf", [128, 8, 64])
    sadd_d = din("sadd", [128, 8, 64])
    cb_d = din("cb", [128, 8, 64])
    iota16_d = din("iota16", [128, 16])
    y_out = nc.dram_tensor("y", [NQ, D], F32, kind="ExternalOutput").ap()
    gtd = nc.dram_tensor("gtd", [8, MTOT], F32, kind="Internal").ap()
    uv16 = nc.dram_tensor("uv16", [16384, 2 * D], BF16, kind="Internal").ap()
    Tuv16 = T("uv16")
    dbg_outs = {}

    with ExitStack() as st:
        P = Prog(nc, st)
        ntile = [0]

        def sb(stk, shape, dt=F32, name=None):
            ntile[0] += 1
            nm = (name or "t") + "_%d" % ntile[0]
            return stk.enter_context(nc.sbuf_tensor(nm, list(shape), dt)), T(nm)

        def psb(stk, shape, dt=F32, name=None):
            ntile[0] += 1
            nm = (name or "p") + "_%d" % ntile[0]
            return stk.enter_context(nc.psum_tensor(nm, list(shape), dt)), T(nm)

        Tout = T("y_out")

        def dump(name, ap, t, shape, dt=F32):
            if not dbg:
                return
            o = nc.dram_tensor("dbg_" + name, list(shape), dt, kind="ExternalOutput").ap()
            to = T("dbg_" + name)
            dbg_outs[name] = to
            P.dma("sp", dict(out=o, in_=ap), reads=[t], writes=[to])

        def finish():
            P.final_wait("sp", [Tout] + list(dbg_outs.values()))
            P.emit()

        def OP(eng, name, reads, writes, **kw):
            P.op(eng, name, kw, reads, writes)

        def DMA(eng, out, in_, reads, writes, **kw):
            P.dma(eng, dict(out=out, in_=in_, **kw), reads, writes)

        evac_rr = [0]

        def evac(out_ap, in_ap, reads, writes):
            evac_rr[0] += 1
            if evac_rr[0] % 2:
                OP("act", "activation", reads, writes, out=out_ap, in_=in_ap, func=AF.Copy)
            else:
                OP("dve", "tensor_copy", reads, writes, out=out_ap, in_=in_ap)

        ident, Tident = sb(st, [128, 128], F32, "ident")
        identb, Tidentb = sb(st, [128, 128], BF16, "identb")
        jmat, Tjmat = sb(st, [128, 128], F32, "jmat")
        onesf, Tonesf = sb(st, [128, 128], F32, "onesf")
        gcol, Tgcol = sb(st, [128, 16], F32, "gcol")
        chcol, Tchcol = sb(st, [128, 8], F32, "chcol")
        kvalid, Tkvalid = sb(st, [128, 32], F32, "kvalid")
        cvalid, Tcvalid = sb(st, [128, 2], F32, "cvalid")
        ccol0, Tccol0 = sb(st, [128, 8], F32, "ccol0")
        cw, Tcw = sb(st, [128, 3, 8], F32, "cw")
        cbias, Tcbias = sb(st, [128, 8], F32, "cbias")
        agw, Tagw = sb(st, [128, 8], F32, "agw")
        cgw, Tcgw = sb(st, [128, 8], F32, "cgw")
        DMA("sp", ident[:], ident_d, [], [Tident])
        DMA("sp", identb[:], identb_d, [], [Tidentb])
        DMA("sp", jmat[:], jmat_d, [], [Tjmat])
        DMA("sp", kvalid[:], kvalid_d, [], [Tkvalid])
        DMA("sp", cvalid[:], cvalid_d, [], [Tcvalid])
        OP("dve", "memset", [], [Tonesf], ap=onesf[:], constant=1.0)
        NCD = dict(allow_slow_non_contiguous=True)
        DMA("sp", gcol[:], attn_norm_w.rearrange("(k p) -> p k", p=128), [], [Tgcol], **NCD)
        for wi in range(3):
            DMA("sp", cw[:, wi, :], conv_w[wi].rearrange("(c p) -> p c", p=128), [], [Tcw], **NCD)
        DMA("sp", cbias[:], conv_b.rearrange("(c p) -> p c", p=128), [], [Tcbias], **NCD)
        DMA("sp", agw[:], agnw.rearrange("(c p) -> p c", p=128), [], [Tagw], **NCD)
        DMA("sp", cgw[:], cgnw.rearrange("(c p) -> p c", p=128), [], [Tcgw], **NCD)
        DMA("sp", chcol[:], rel_bias[31:32, :].partition_broadcast(128), [], [Tchcol], **NCD)
        OP("dve", "tensor_scalar", [Tchcol, Tcvalid], [Tccol0], out=ccol0[:], in0=chcol[:], scalar1=cvalid[:, 0:1],
           scalar2=None, op0=ALU.add)

        mixT, TmixT = sb(st, [128, 16, NQ], BF16, "mixT")
        gsig, Tgsig = sb(st, [128, 8, 24], F32, "gsig")
        mixF = mixT[:].rearrange("p a b -> p (a b)").bitcast(F32)
        sA = ExitStack()
        ksT, TksT = sb(sA, [128, 2, S], BF16, "ksT")
        vs, Tvs = sb(sA, [128, 32, 2, 130], BF16, "vs")
        kwT, TkwT = sb(sA, [128, 2, 1536], BF16, "kwT")
        vw, Tvw = sb(sA, [128, 12, 2, 130], BF16, "vw")
        kcT, TkcT = sb(sA, [128, 2, 256], BF16, "kcT")
        vcaug, Tvcaug = sb(sA, [128, 2, 2, 194], F32, "vcaug")
        xnT, _ = sb(sA, [128, 2, 16, 512], BF16, "xnT")
        xnTA, TxnTA = xnT[:, 0], T("xnTA")
        xnTB, TxnTB = xnT[:, 1], T("xnTB")
        xnTh, TxnTh = sb(sA, [128, 16, 2], BF16, "xnTh")
        OP("pool", "memset", [], [Tvs], ap=vs[:], constant=1.0)
        OP("pool", "memset", [], [Tvw], ap=vw[:], constant=1.0)
        OP("pool", "memset", [], [Tvcaug], ap=vcaug[:], constant=0.0)
        OP("pool", "memset", [], [Tvcaug], ap=vcaug[:, :, :, 128:129], constant=1.0)
        for tl in range(2):
            for kvh in range(2):
                DMA("sp", vcaug[:, tl, kvh, 130:194], ovs_d[:, tl, :], [], [Tvcaug])

        def rstd_from_sumsq(sqt, Tsq, n):
            OP("dve", "tensor_scalar", [Tsq], [Tsq], out=sqt[:, 1:2], in0=sqt[:, 0:1], scalar1=1.0 / n, scalar2=EPS,
               op0=ALU.mult, op1=ALU.add)
            OP("act", "activation", [Tsq], [Tsq], out=sqt[:, 1:2], in_=sqt[:, 1:2], func=AF.Sqrt)
            OP("dve", "reciprocal", [Tsq], [Tsq], out=sqt[:, 0:1], in_=sqt[:, 1:2])

        with ExitStack() as s1:
            wkv, Twkv = sb(s1, [128, 16, 1536], BF16, "wkv")
            wck, Twck = sb(s1, [128, 32, 128], BF16, "wck")
            wcv, Twcv = sb(s1, [128, 32, 128], BF16, "wcv")
            cposT, TcposT = sb(s1, [128, 32], BF16, "cposT")
            constk, Tconstk = sb(s1, [128, 2], F32, "constk")
            kcvbuf, Tkcv = sb(s1, [128, 4, 528], BF16, "kcvbuf")
            vcT, TvcT = sb(s1, [128, 2, 256], F32, "vcT")
            xst = [(mixF[:, 4096 + i * 2048:4096 + (i + 1) * 2048], T("xst%d" % i)) for i in range(2)]
            xnb = [sb(s1, [128, D], BF16, "xnb") for _ in range(2)]
            junk, Tjunk = sb(s1, [128, D], BF16, "junk")
            sq = [sb(s1, [128, 2], F32, "sq") for _ in range(2)]
            wst = [(mixF[:, i * 2048:(i + 1) * 2048].rearrange("p (k c) -> p k c", k=16), T("wst%d" % i)) for i in range(2)]
            pT = [psb(s1, [128, 8, 128], BF16, "pT") for _ in range(2)]
            pM = [psb(s1, [128, 512], F32, "pM") for _ in range(4)]
            pC, TpC = psb(s1, [128, 512], F32, "pC")

            for ci in range(12):
                stg, Tstg = wst[ci % 2]
                c0 = 1024 + ci * 128
                DMA("sp", stg, w_in[:, c0:c0 + 128].rearrange("(k p) c -> p k c", p=128), [], [Tstg])
                OP("pool", "tensor_tensor", [Tstg, Tgcol], [Twkv], out=wkv[:, :, ci * 128:(ci + 1) * 128], in0=stg,
                   in1=gcol[:].unsqueeze(2).to_broadcast([128, 16, 128]), op=ALU.mult)
            DMA("pool", wck[:], w_cmp_k.rearrange("l d e -> d l e"), [], [Twck])
            DMA("pool", wcv[:], w_cmp_v.rearrange("l d e -> d l e"), [], [Twcv])
            DMA("pool", cposT[:], cmp_pos.rearrange("l d -> d l"), [], [TcposT], **NCD)
            OP("dve", "memset", [], [Tkcv], ap=kcvbuf[:], constant=0.0)
            for ti, (wc, Twc) in enumerate(((wck, Twck), (wcv, Twcv))):
                for l in range(32):
                    OP("pe", "matmul", [Twc, TcposT], [TpC], out=pC[:, ti:ti + 1], lhsT=wc[:, l, :], rhs=cposT[:, l:l + 1],
                       start=(l == 0), stop=(l == 31))
            OP("dve", "tensor_copy", [TpC], [Tconstk], out=constk[:], in_=pC[:, 0:2])

            xcount = [0]

            def norm_transpose(row0, dst, Tdst, col0):
                i = xcount[0] % 2
                xcount[0] += 1
                (xt, Txt), (xb, Txb), (sqt, Tsq) = xst[i], xnb[i], sq[i]
                DMA("sp", xt, xs[row0:row0 + 128, :], [], [Txt])
                OP("act", "activation", [Txt], [Tjunk, Tsq], out=junk[:], in_=xt, func=AF.Square, accum_out=sqt[:, 0:1])
                rstd_from_sumsq(sqt, Tsq, D)
                OP("dve", "tensor_scalar", [Txt, Tsq], [Txb], out=xb[:], in0=xt, scalar1=sqt[:, 0:1], scalar2=None, op0=ALU.mult)
                for half in range(2):
                    pt, Tpt = pT[half]
                    for k8 in range(8):
                        kc = half * 8 + k8
                        OP("pe", "transpose", [Txb, Tidentb], [Tpt], out=pt[:, k8, :], in_=xb[:, kc * 128:(kc + 1) * 128],
                           identity=identb[:])
                    evac(dst[:, half * 8:(half + 1) * 8, col0:col0 + 128], pt[:], [Tpt], [Tdst])

            mrr = [0]

            def nextpm():
                mrr[0] += 1
                return pM[mrr[0] % 4]

            def nt_tile(g, j):
                xg, Txg = (xnTA, TxnTA) if g % 2 == 0 else (xnTB, TxnTB)
                norm_transpose((4 * g + j) * 128, xg, Txg, j * 128)
                if g == 5 and j == 3:
                    OP("dve", "tensor_copy", [TxnTB], [TxnTh], out=xnTh[:], in_=xnTB[:, :, 510:512])

            def mm_units(g):
                xg, Txg = (xnTA, TxnTA) if g % 2 == 0 else (xnTB, TxnTB)
                units = []
                fm = [(0, kcvbuf[:, 0, 16:528], Tkcv), (128, kcvbuf[:, 1, 16:528], Tkcv),
                      (256, kcvbuf[:, 2, 16:528], Tkcv), (384, kcvbuf[:, 3, 16:528], Tkcv),
                      (512, ksT[:, 0, g * 512:(g + 1) * 512], TksT), (640, ksT[:, 1, g * 512:(g + 1) * 512], TksT)]
                if g >= 5:
                    fm += [(1024, kwT[:, 0, (g - 5) * 512:(g - 4) * 512], TkwT),
                           (1152, kwT[:, 1, (g - 5) * 512:(g - 4) * 512], TkwT)]

                def u_fm(co, dst, Td):
                    pm, Tpm = nextpm()
                    for kc in range(16):
                        OP("pe", "matmul", [Twkv, Txg], [Tpm], out=pm[:], lhsT=wkv[:, kc, co:co + 128], rhs=xg[:, kc, :],
                           start=(kc == 0), stop=(kc == 15))
                    evac(dst, pm[:], [Tpm], [Td])

                def u_tm(j, c0, dst, Td):
                    pm, Tpm = nextpm()
                    for kc in range(16):
                        OP("pe", "matmul", [Twkv, Txg], [Tpm], out=pm[:, 0:256], lhsT=xg[:, kc, j * 128:(j + 1) * 128],
                           rhs=wkv[:, kc, c0:c0 + 256], start=(kc == 0), stop=(kc == 15))
                    evac(dst, pm[:, 0:256].rearrange("p (a b) -> p a b", a=2), [Tpm], [Td])

                def u_cmp():
                    for ti in range(2):
                        wc, Twc = (wck, Twck) if ti == 0 else (wcv, Twcv)
                        for l in range(32):
                            OP("pe", "matmul", [Twc, Tkcv], [TpC], out=pC[:, 64 * ti:64 * ti + 64].rearrange("p (a b) -> p a b", a=2), lhsT=wc[:, l, :],
                               rhs=kcvbuf[:, 2 * ti:2 * ti + 2, l:l + 497:16], start=(l == 0), stop=(l == 31))
                    for kvh in range(2):
                        OP("dve", "tensor_scalar", [TpC, Tconstk], [TkcT], out=kcT[:, kvh, 32 * g:32 * g + 32],
                           in0=pC[:, 32 * kvh:32 * kvh + 32], scalar1=constk[:, 0:1], scalar2=None, op0=ALU.add)
                        OP("dve", "tensor_scalar", [TpC, Tconstk], [TvcT], out=vcT[:, kvh, 32 * g:32 * g + 32],
                           in0=pC[:, 64 + 32 * kvh:96 + 32 * kvh], scalar1=constk[:, 1:2], scalar2=None, op0=ALU.add)
                    OP("dve", "tensor_copy", [Tkcv], [Tkcv], out=kcvbuf[:, :, 0:16], in_=kcvbuf[:, :, 512:528])

                for (co, dst, Td) in fm[:4]:
                    units.append(lambda co=co, dst=dst, Td=Td: u_fm(co, dst, Td))
                units.append(u_cmp)
                for (co, dst, Td) in fm[4:]:
                    units.append(lambda co=co, dst=dst, Td=Td: u_fm(co, dst, Td))
                for j in range(4):
                    slot = 4 * g + j
                    units.append(lambda j=j, slot=slot: u_tm(j, 768, vs[:, slot, :, 0:128], Tvs))
                    if g >= 5:
                        units.append(lambda j=j, slot=slot: u_tm(j, 1280, vw[:, slot - 20, :, 0:128], Tvw))
                return units

            for j in range(4):
                nt_tile(0, j)
            for g in range(8):
                units = mm_units(g)
                if g < 7:
                    n = len(units)
                    for j in range(4):
                        nt_tile(g + 1, j)
                        for u in units[(j * n) // 4:((j + 1) * n) // 4]:
                            u()
                else:
                    for u in units:
                        u()
            for tl in range(2):
                for kvh in range(2):
                    pm, Tpm = nextpm()
                    OP("pe", "transpose", [TvcT, Tident], [Tpm], out=pm[:, 0:128], in_=vcT[:, kvh, tl * 128:(tl + 1) * 128],
                       identity=ident[:])
                    OP("dve", "tensor_copy", [Tpm], [Tvcaug], out=vcaug[:, tl, kvh, 0:128], in_=pm[:, 0:128])
            if stop_after == 1:
                dump("ksT", ksT[:], TksT, [128, 2, S], BF16)
                dump("vs", vs[:], Tvs, [128, 32, 2, 130], BF16)
                dump("kwT", kwT[:], TkwT, [128, 2, 1536], BF16)
                dump("kcT", kcT[:], TkcT, [128, 2, 256], BF16)
                dump("vcaug", vcaug[:], Tvcaug, [128, 2, 2, 194], F32)
            P.barrier()
        if stop_after == 1:
            finish()
            return nc, dbg_outs

        qT, TqT = sb(sA, [128, 8, NQ], BF16, "qT")
        xh = [(xnTA, TxnTA), (xnTB, TxnTB)]
        with ExitStack() as s2:
            wst2 = [sb(s2, [128, 16, 128], F32, "wst2") for _ in range(2)]
            wb2 = [sb(s2, [128, 16, 128], BF16, "wb2") for _ in range(4)]
            bT, TbT = sb(s2, [128, 1026], F32, "bT")
            cT, TcT = sb(s2, [128, 1026], F32, "cT")
            hT, ThT = sb(s2, [128, 1026], F32, "hT")
            uu, Tuu = sb(s2, [128, 1026], F32, "uu")
            yy, Tyy = sb(s2, [128, 1024], F32, "yy")
            oo, Too = sb(s2, [128, 1024], F32, "oo")
            osq, Tosq = sb(s2, [128, 1024], F32, "osq")
            rs, Trs = sb(s2, [128, 1024], F32, "rs")
            pM2 = [psb(s2, [128, 512], F32, "pM2") for _ in range(4)]
            pH, TpH = psb(s2, [128, 512], F32, "pH")
            wrr = [0]
            mr2 = [0]

            def nextpm2():
                mr2[0] += 1
                return pM2[mr2[0] % 4]

            def load_w(c0, width=128):
                i = wrr[0]
                wrr[0] += 1
                stg, Tstg = wst2[i % 2]
                wb, Twb = wb2[i % 4]
                DMA("sp", stg[:, :, 0:width], w_in[:, c0:c0 + width].rearrange("(k p) c -> p k c", p=128), [], [Tstg])
                OP("pool", "tensor_tensor", [Tstg, Tgcol], [Twb], out=wb[:, :, 0:width], in0=stg[:, :, 0:width],
                   in1=gcol[:].unsqueeze(2).to_broadcast([128, 16, width]), op=ALU.mult)
                return wb, Twb

            def proj_fm(wb, Twb, dst_fn, Tdst):
                for half in range(2):
                    xg, Txg = xh[half]
                    pm, Tpm = nextpm2()
                    for kc in range(16):
                        OP("pe", "matmul", [Twb, Txg], [Tpm], out=pm[:], lhsT=wb[:, kc, :], rhs=xg[:, kc, :],
                           start=(kc == 0), stop=(kc == 15))
                    evac(dst_fn(half), pm[:], [Tpm], [Tdst])

            for h in range(8):
                wb, Twb = load_w(h * 128)
                proj_fm(wb, Twb, lambda half, h=h: qT[:, h, half * 512:(half + 1) * 512], TqT)
            wg, Twg = load_w(2560, 24)
            for tile in range(8):
                xg, Txg = xh[tile // 4]
                cs = (tile % 4) * 128
                pm, Tpm = nextpm2()
                for kc in range(16):
                    OP("pe", "matmul", [Twg, Txg], [Tpm], out=pm[:, 0:24], lhsT=xg[:, kc, cs:cs + 128], rhs=wg[:, kc, 0:24],
                       start=(kc == 0), stop=(kc == 15))
                OP("act", "activation", [Tpm], [Tgsig], out=gsig[:, tile, :], in_=pm[:, 0:24], func=AF.Sigmoid)
            for ch in range(8):
                wb_b, Twb_b = load_w(2584 + ch * 128)
                wb_c, Twb_c = load_w(3608 + ch * 128)
                wb_h, Twb_h = load_w(4632 + ch * 128)
                for (wb, Twb, dT, TdT) in ((wb_b, Twb_b, bT, TbT), (wb_c, Twb_c, cT, TcT), (wb_h, Twb_h, hT, ThT)):
                    proj_fm(wb, Twb, lambda half, dT=dT: dT[:, 2 + half * 512:2 + (half + 1) * 512], TdT)
                for hi, (wb, Twb, dT, TdT) in enumerate(((wb_c, Twb_c, cT, TcT), (wb_h, Twb_h, hT, ThT))):
                    for kc in range(16):
                        OP("pe", "matmul", [Twb, TxnTh], [TpH], out=pH[:, 2 * hi:2 * hi + 2], lhsT=wb[:, kc, :], rhs=xnTh[:, kc, :],
                           start=(kc == 0), stop=(kc == 15))
                    OP("dve", "tensor_copy", [TpH], [TdT], out=dT[:, 0:2], in_=pH[:, 2 * hi:2 * hi + 2])
                OP("dve", "tensor_tensor", [TcT, ThT], [Tuu], out=uu[:], in0=cT[:], in1=hT[:], op=ALU.mult)
                OP("dve", "tensor_scalar", [Tuu, Tcw, Tcbias], [Tyy], out=yy[:], in0=uu[:, 2:1026], scalar1=cw[:, 2, ch:ch + 1],
                   scalar2=cbias[:, ch:ch + 1], op0=ALU.mult, op1=ALU.add)
                OP("dve", "scalar_tensor_tensor", [Tuu, Tcw, Tyy], [Tyy], out=yy[:], in0=uu[:, 1:1025], scalar=cw[:, 1, ch:ch + 1],
                   in1=yy[:], op0=ALU.mult, op1=ALU.add)
                OP("dve", "scalar_tensor_tensor", [Tuu, Tcw, Tyy], [Tyy], out=yy[:], in0=uu[:, 0:1024], scalar=cw[:, 0, ch:ch + 1],
                   in1=yy[:], op0=ALU.mult, op1=ALU.add)
                OP("dve", "tensor_tensor", [TbT, Tyy], [Too], out=oo[:], in0=bT[:, 2:1026], in1=yy[:], op=ALU.mult)
                OP("pool", "tensor_tensor", [Too], [Tosq], out=osq[:], in0=oo[:], in1=oo[:], op=ALU.mult)
                for half in range(2):
                    pm, Tpm = nextpm2()
                    OP("pe", "matmul", [Tonesf, Tosq], [Tpm], out=pm[:], lhsT=onesf[:], rhs=osq[:, half * 512:(half + 1) * 512],
                       start=True, stop=True)
                    OP("dve", "tensor_scalar", [Tpm], [Trs], out=rs[:, half * 512:(half + 1) * 512], in0=pm[:], scalar1=1.0 / 128,
                       scalar2=EPS, op0=ALU.mult, op1=ALU.add)
                OP("act", "activation", [Trs], [Trs], out=rs[:], in_=rs[:], func=AF.Sqrt)
                OP("dve", "reciprocal", [Trs], [Trs], out=rs[:], in_=rs[:])
                OP("dve", "scalar_tensor_tensor", [Too, Tcgw, Trs], [TmixT], out=mixT[:, 8 + ch, :], in0=oo[:], scalar=cgw[:, ch:ch + 1],
                   in1=rs[:], op0=ALU.mult, op1=ALU.mult)
            if stop_after == 2:
                dump("qT", qT[:], TqT, [128, 8, NQ], BF16)
                dump("gsig", gsig[:], Tgsig, [128, 8, 24], F32)
                dump("mixc", mixT[:, 8:16, :], TmixT, [128, 8, NQ], BF16)
            P.barrier()
        if stop_after == 2:
            finish()
            return nc, dbg_outs
        with ExitStack() as s3:
            BT0, TBT0 = sb(s3, [128, 8, 128], F32, "BT0")
            BT1, TBT1 = sb(s3, [128, 8, 128], F32, "BT1")
            BT4, TBT4 = sb(s3, [128, 8, 128], F32, "BT4")
            BTC, TBTC = sb(s3, [128, 8, 128], F32, "BTC")
            CB, TCB = xnT[:].rearrange("p a b c -> p (a b c)").bitcast(F32).rearrange("p (h t) -> p h t", h=8), T("CB")
            eexp, Teexp = sb(s3, [128, 4096], BF16, "eexp")
            cbnf, Tcbnf = sb(s3, [128, 8, 64], F32, "cbnf")
            sadd, Tsadd = sb(s3, [128, 8, 64], F32, "sadd")
            cbm, Tcbm = sb(s3, [128, 8, 64], F32, "cbm")
            PT = [sb(s3, [128, 512], BF16, "PT") for _ in range(4)]
            ec, Tec = sb(s3, [128, 2, 512], F32, "ec")
            tmpS = [sb(s3, [128, 512], F32, "tmpS") for _ in range(2)]
            ocmp2 = [sb(s3, [128, 4, 194], F32, "ocmp") for _ in range(2)]
            osel, Tosel = sb(s3, [128, 4, 130], F32, "osel")
            owin, Towin = sb(s3, [128, 4, 130], F32, "owin")
            sm, Tsm = sb(s3, [128, 64], F32, "sm")
            imp, Timp = sb(s3, [128, 64], F32, "imp")
            score, Tscore = sb(s3, [128, 64], F32, "score")
            work, Twork = sb(s3, [128, 64], F32, "work")
            top8, Ttop8 = sb(s3, [128, 16], F32, "top8")
            negm, Tnegm = sb(s3, [128, 64], F32, "negm")
            negT = [sb(s3, [128, 128], BF16, "negT") for _ in range(2)]
            oc, Toc = sb(s3, [128, 4, 128], F32, "oc")
            on, Ton = sb(s3, [128, 4, 128], F32, "on")
            jk, Tjk = sb(s3, [128, 128], F32, "jk")
            pS = [psb(s3, [128, 512], F32, "pS") for _ in range(2)]
            pO = [psb(s3, [128, 512], F32, "pO") for _ in range(4)]
            pX = [psb(s3, [128, 512], F32, "pX") for _ in range(2)]
            Tgtd = T("gtd")
            if dbg:
                obr, Tobr = sb(s3, [128, 4, 128], F32, "obr")
            s3a = ExitStack()
            rb33, Trb33 = sb(s3a, [33, 8], F32, "rb33")
            rb31, Trb31 = sb(s3a, [8, 1], F32, "rb31")
            DMA("sp", rb31[:], rel_bias[31:32, :].rearrange("a h -> h a"), [], [Trb31], **NCD)
            oh33c = [sb(s3a, [33, 512], F32, "oh33c") for _ in range(2)]
            gtsc = [sb(s3a, [8, 512], F32, "gtsc") for _ in range(2)]
            hkc, Thkc = sb(s3a, [128, NQ], F32, "hkc")
            hk, Thk = hkc[:].rearrange("p (a b) -> p a b", a=8), Thkc

            OP("pool", "memset", [], [Teexp], ap=eexp[64:128, :], constant=0.0)
            DMA("sp", eexp[0:64, :], eexp_d, [], [Teexp])
            for (nT_, TnT_) in negT:
                OP("pool", "memset", [], [TnT_], ap=nT_[:], constant=0.0)
            DMA("sp", cbnf[:], cbnf_d, [], [Tcbnf])
            DMA("sp", sadd[:], sadd_d, [], [Tsadd])
            DMA("sp", cbm[:], cb_d, [], [Tcbm])
            OP("dve", "memset", [], [Trb33], ap=rb33[32:33, :], constant=1.0)
            DMA("sp", rb33[0:32, :], rel_bias, [], [Trb33])
            for c0 in range(0, MTOT, 512):
                wd = min(512, MTOT - c0)
                px, Tpx = pX[(c0 // 512) % 2]
                oh33, Toh33 = oh33c[(c0 // 512) % 2]
                gts, Tgts = gtsc[(c0 // 512) % 2]
                DMA("sp", oh33[:, 0:wd], oh33_d[:, c0:c0 + wd], [], [Toh33])
                OP("pe", "matmul", [Trb33, Toh33], [Tpx], out=px[0:8, 0:wd], lhsT=rb33[:, :], rhs=oh33[:, 0:wd], start=True, stop=True)
                OP("dve", "tensor_scalar", [Tpx, Trb31], [Tgts], out=gts[:, 0:wd], in0=px[0:8, 0:wd], scalar1=rb31[:, 0:1], scalar2=None, op0=ALU.subtract)
                DMA("sp", gtd[:, c0:c0 + wd], gts[:, 0:wd], [Tgts], [Tgtd])

            def hankel(off, pstep, ncol, heads=True):
                if heads:
                    return bass.AP(tensor=gtd.tensor, offset=off, ap=[[pstep, 128], [MTOT, 8], [1, ncol]])
                return bass.AP(tensor=gtd.tensor, offset=off, ap=[[pstep, 128], [1, ncol]])

            for (BT, TBT, off) in ((BT0, TBT0, 0), (BT1, TBT1, 128), (BT4, TBT4, MSEG0)):
                DMA("sp", hk, hankel(off, 1, 128), [Tgtd], [Thk])
                hkf = hkc
                BTf = BT[:].rearrange("p a b -> p (a b)")
                for half in range(2):
                    px, Tpx = pX[half]
                    OP("pe", "matmul", [Tjmat, Thk], [Tpx], out=px[:], lhsT=jmat[:], rhs=hkf[:, half * 512:(half + 1) * 512], start=True, stop=True)
                    OP("dve", "tensor_copy", [Tpx], [TBT], out=BTf[:, half * 512:(half + 1) * 512], in_=px[:])
            for h in range(8):
                DMA("sp", hkc[:], hankel(h * MTOT + MSEG0 + MSEG1, 16, NQ, heads=False), [Tgtd], [Thkc])
                for half in range(2):
                    px, Tpx = pX[half]
                    OP("pe", "matmul", [Tjmat, Thkc], [Tpx], out=px[:], lhsT=jmat[:], rhs=hkc[:, half * 512:(half + 1) * 512], start=True, stop=True)
                    OP("dve", "tensor_copy", [Tpx], [TCB], out=CB[:, h, half * 512:(half + 1) * 512], in_=px[:])
            OP("dve", "tensor_copy", [Tchcol], [TBTC], out=BTC[:], in_=chcol[:].unsqueeze(2).to_broadcast([128, 8, 128]))

            P.barrier()
            s3a.close()
            for (tab, c0) in ((peer_u, 0), (peer_v, D)):
                for r0 in range(0, 16384, 1024):
                    P.dma("pool", dict(out=uv16[r0:r0 + 1024, c0:c0 + D], in_=tab[r0:r0 + 1024, :]), reads=[], writes=[Tuv16], nowaw=True)
            srr = [0]
            prr = [0]
            trr = [0]
            nrr = [0]

            def nextS():
                srr[0] += 1
                return pS[srr[0] % 2]

            def nextPT():
                prr[0] += 1
                return PT[prr[0] % 4]

            def nextTmp():
                trr[0] += 1
                return tmpS[trr[0] % 2]

            pending = []
            sm2 = [(sm, Tsm), sb(s3, [128, 64], F32, "sm1")]
            combos = [(qt, kvh) for qt in range(8) for kvh in range(2)]

            def emit_tr(qt_, kvh_):
                px, Tpx = pX[1]
                for g in range(4):
                    OP("pe", "transpose", [Ton, Tident], [Tpx], out=px[:, g * 128:(g + 1) * 128], in_=on[:, g, :], identity=ident[:])
                for g in range(4):
                    OP("dve", "tensor_scalar", [Tpx, Tagw], [TmixT], out=mixT[:, 4 * kvh_ + g, qt_ * 128:(qt_ + 1) * 128], in0=px[:, g * 128:(g + 1) * 128],
                       scalar1=agw[:, 4 * kvh_ + g:4 * kvh_ + g + 1], scalar2=None, op0=ALU.mult)

            def cmp_and_topk(ci):
                qt, kvh = combos[ci]
                qs = slice(qt * 128, (qt + 1) * 128)
                hs = slice(4 * kvh, 4 * kvh + 4)
                q4 = qT[:, hs, qs]
                ocmp, Tocmp = ocmp2[ci % 2]
                smc, Tsmc = sm2[ci % 2]
                for tl in range(2):
                    ps, Tps = pX[tl]
                    OP("pe", "matmul", [TkcT, TqT], [Tps], out=ps[:], lhsT=kcT[:, kvh, tl * 128:(tl + 1) * 128], rhs=q4, start=True, stop=True)
                    if tl == 0:
                        OP("act", "activation", [Tps, Tcvalid], [Tec], out=ec[:, 0, :], in_=ps[:], func=AF.Exp, scale=SCALE, bias=cvalid[:, 0:1])
                    else:
                        tm, Ttm = nextTmp()
                        OP("dve", "scalar_tensor_tensor", [Tps, TCB], [Ttm], out=tm[:].rearrange("p (a b) -> p a b", a=4), in0=ps[:].rearrange("p (a b) -> p a b", a=4),
                           scalar=SCALE, in1=CB[:, hs, qs], op0=ALU.mult, op1=ALU.add)
                        OP("act", "activation", [Ttm, Tcvalid], [Tec], out=ec[:, 1, :], in_=tm[:], func=AF.Exp, bias=cvalid[:, 1:2])
                for g in range(4):
                    px, Tpx = pX[g // 2]
                    c0 = 256 * (g % 2)
                    for tl in range(2):
                        OP("pe", "matmul", [Tec, Tvcaug], [Tpx], out=px[:, c0:c0 + 194], lhsT=ec[:, tl, g * 128:(g + 1) * 128], rhs=vcaug[:, tl, kvh, :],
                           start=(tl == 0), stop=(tl == 1))
                    evac(ocmp[:, g, :], px[:, c0:c0 + 194], [Tpx], [Tocmp])
                OP("dve", "tensor_scalar", [Tocmp], [Tsmc], out=smc[:, 0:4], in0=ocmp[:, :, 128], scalar1=1e-30, scalar2=None, op0=ALU.max)
                OP("dve", "reciprocal", [Tsmc], [Tsmc], out=smc[:, 0:4], in_=smc[:, 0:4])
                OP("dve", "tensor_scalar", [Tocmp, Tsmc], [Timp], out=imp[:], in0=ocmp[:, 0, 130:194], scalar1=smc[:, 0:1], scalar2=None, op0=ALU.mult)
                for g in range(1, 4):
                    OP("dve", "scalar_tensor_tensor", [Tocmp, Tsmc, Timp], [Timp], out=imp[:], in0=ocmp[:, g, 130:194], scalar=smc[:, g:g + 1],
                       in1=imp[:], op0=ALU.mult, op1=ALU.add)
                OP("dve", "tensor_tensor", [Timp, Tcbnf], [Tscore], out=score[:], in0=imp[:], in1=cbnf[:, qt, :], op=ALU.mult)
                OP("dve", "tensor_tensor", [Tscore, Tsadd], [Tscore], out=score[:], in0=score[:], in1=sadd[:, qt, :], op=ALU.add)
                OP("dve", "max", [Tscore], [Ttop8], out=top8[:, 0:8], in_=score[:])
                OP("dve", "match_replace", [Tscore, Ttop8], [Twork], out=work[:], in_to_replace=top8[:, 0:8], in_values=score[:], imm_value=-1e30)
                OP("dve", "max", [Twork], [Ttop8], out=top8[:, 8:16], in_=work[:])
                OP("dve", "tensor_scalar", [Tscore, Ttop8], [Twork], out=work[:], in0=score[:], scalar1=top8[:, 15:16], scalar2=None, op0=ALU.is_ge)
                OP("dve", "tensor_tensor", [Twork, Tcbm], [Twork], out=work[:], in0=work[:], in1=cbm[:, qt, :], op=ALU.mult)
                OP("dve", "tensor_scalar", [Twork], [Tnegm], out=negm[:], in0=work[:], scalar1=-NEG, scalar2=NEG, op0=ALU.mult, op1=ALU.add)

            def neg_transpose(ci):
                px, Tpx = pX[0]
                OP("pe", "transpose", [Tnegm, Tident], [Tpx], out=px[0:64, 0:128], in_=negm[:], identity=ident[:])
                nT, TnT = negT[ci % 2]
                OP("act", "activation", [Tpx], [TnT], out=nT[0:64, :], in_=px[0:64, 0:128], func=AF.Copy)

            cmp_and_topk(0)
            neg_transpose(0)
            for ci, (qt, kvh) in enumerate(combos):
                    qs = slice(qt * 128, (qt + 1) * 128)
                    hs = slice(4 * kvh, 4 * kvh + 4)
                    q4 = qT[:, hs, qs]
                    ocmp, Tocmp = ocmp2[ci % 2]
                    sm, Tsm = sm2[ci % 2]
                    nT, TnT = negT[ci % 2]
                    def win_S(k):
                        wslot = qt + k
                        ps, Tps = nextS()
                        OP("pe", "matmul", [TkwT, TqT], [Tps], out=ps[:], lhsT=kwT[:, kvh, wslot * 128:(wslot + 1) * 128], rhs=q4, start=True, stop=True)
                        return ps, Tps
                    nxt = win_S(0)
                    for k in range(5):
                        BT, TBT = ((BT4, TBT4), (BTC, TBTC), (BTC, TBTC), (BT1, TBT1), (BT0, TBT0))[k]
                        wslot = qt + k
                        ps, Tps = nxt
                        if k < 4:
                            nxt = win_S(k + 1)
                        pt, Tpt = nextPT()
                        if k in (1, 2):
                            OP("act", "activation", [Tps, Tkvalid], [Tpt], out=pt[:], in_=ps[:], func=AF.Exp, scale=SCALE, bias=kvalid[:, 20 + wslot:21 + wslot])
                        else:
                            tm, Ttm = nextTmp()
                            OP("dve", "scalar_tensor_tensor", [Tps, TBT], [Ttm], out=tm[:].rearrange("p (a b) -> p a b", a=4), in0=ps[:].rearrange("p (a b) -> p a b", a=4),
                               scalar=SCALE, in1=BT[:, hs, :], op0=ALU.mult, op1=ALU.add)
                            OP("act", "activation", [Ttm, Tkvalid], [Tpt], out=pt[:], in_=tm[:], func=AF.Exp, bias=kvalid[:, 20 + wslot:21 + wslot])
                        for g in range(4):
                            po, Tpo = pO[g]
                            OP("pe", "matmul", [Tpt, Tvw], [Tpo], out=po[:, 0:130], lhsT=pt[:, g * 128:(g + 1) * 128], rhs=vw[:, wslot, kvh, :],
                               start=(k == 0), stop=(k == 4))
                    for g in range(4):
                        evac(owin[:, g, :], pO[g][0][:, 0:130], [pO[g][1]], [Towin])
                    last = 24 + qt
                    nTb = nT[:].unsqueeze(1).to_broadcast([128, 4, 128])

                    def sel_S(slot):
                        ps, Tps = nextS()
                        OP("pe", "matmul", [TksT, TqT], [Tps], out=ps[:], lhsT=ksT[:, kvh, slot * 128:(slot + 1) * 128], rhs=q4, start=True, stop=False)
                        OP("pe", "matmul", [Teexp, TnT], [Tps], out=ps[:], lhsT=eexp[:, slot * 128:(slot + 1) * 128], rhs=nTb, start=False, stop=True)
                        return ps, Tps
                    nxt = sel_S(0)
                    for slot in range(last + 1):
                        ps, Tps = nxt
                        if slot < last:
                            nxt = sel_S(slot + 1)
                        pt, Tpt = nextPT()
                        rel = slot - last
                        if rel >= -1:
                            BT, TBT = (BT0, TBT0) if rel == 0 else (BT1, TBT1)
                            tm, Ttm = nextTmp()
                            OP("dve", "scalar_tensor_tensor", [Tps, TBT], [Ttm], out=tm[:].rearrange("p (a b) -> p a b", a=4),
                               in0=ps[:].rearrange("p (a b) -> p a b", a=4), scalar=SCALE, in1=BT[:, hs, :], op0=ALU.mult, op1=ALU.add)
                            OP("act", "activation", [Ttm], [Tpt], out=pt[:], in_=tm[:], func=AF.Exp)
                        else:
                            OP("act", "activation", [Tps], [Tpt], out=pt[:], in_=ps[:], func=AF.Exp, scale=SCALE)
                        for g in range(4):
                            po, Tpo = pO[g]
                            OP("pe", "matmul", [Tpt, Tvs], [Tpo], out=po[:, 0:130], lhsT=pt[:, g * 128:(g + 1) * 128], rhs=vs[:, slot, kvh, :],
                               start=(slot == 0), stop=(slot == last))
                        if slot == 3 and ci + 1 < len(combos):
                            cmp_and_topk(ci + 1)
                        if slot == 8 and pending:
                            emit_tr(*pending.pop())
                    if ci + 1 < len(combos):
                        neg_transpose(ci + 1)
                    for g in range(4):
                        evac(osel[:, g, :], pO[g][0][:, 0:130], [pO[g][1]], [Tosel])
                    OP("dve", "reciprocal", [Tosel], [Tsm], out=sm[:, 4:8], in_=osel[:, :, 128])
                    OP("dve", "reciprocal", [Towin], [Tsm], out=sm[:, 8:12], in_=owin[:, :, 128])
                    gv = gsig[:, qt, 12 * kvh:12 * kvh + 12].rearrange("p (g b) -> p g b", b=3)
                    for br in range(3):
                        OP("dve", "tensor_tensor", [Tsm, Tgsig], [Tsm], out=sm[:, 12 + 4 * br:16 + 4 * br], in0=sm[:, 4 * br:4 * br + 4], in1=gv[:, :, br], op=ALU.mult)
                    if dbg and stop_after == 3 and qt in (0, 3, 7):
                        for br, (ob, Tob) in enumerate(((ocmp, Tocmp), (osel, Tosel), (owin, Towin))):
                            for g in range(4):
                                OP("dve", "tensor_scalar", [Tob, Tsm], [Tobr], out=obr[:, g, :], in0=ob[:, g, 0:128], scalar1=sm[:, 4 * br + g:4 * br + g + 1],
                                   scalar2=None, op0=ALU.mult)
                            dump("obr_%d_%d_%d" % (qt, kvh, br), obr[:], Tobr, [128, 4, 128], F32)
                    for g in range(4):
                        OP("dve", "tensor_scalar", [Tocmp, Tsm], [Toc], out=oc[:, g, :], in0=ocmp[:, g, 0:128], scalar1=sm[:, 12 + g:13 + g], scalar2=None, op0=ALU.mult)
                        OP("dve", "scalar_tensor_tensor", [Tosel, Tsm, Toc], [Toc], out=oc[:, g, :], in0=osel[:, g, 0:128], scalar=sm[:, 16 + g:17 + g], in1=oc[:, g, :],
                           op0=ALU.mult, op1=ALU.add)
                        OP("dve", "scalar_tensor_tensor", [Towin, Tsm, Toc], [Toc], out=oc[:, g, :], in0=owin[:, g, 0:128], scalar=sm[:, 20 + g:21 + g], in1=oc[:, g, :],
                           op0=ALU.mult, op1=ALU.add)
                        OP("dve", "scalar_tensor_tensor", [Toc], [Tjk, Tsm], out=jk[:], in0=oc[:, g, :], scalar=1.0, in1=oc[:, g, :],
                           op0=ALU.mult, op1=ALU.mult, accum_out=sm[:, 24 + g:25 + g])
                    OP("dve", "tensor_scalar", [Tsm], [Tsm], out=sm[:, 28:32], in0=sm[:, 24:28], scalar1=1.0 / 128, scalar2=EPS, op0=ALU.mult, op1=ALU.add)
                    OP("act", "activation", [Tsm], [Tsm], out=sm[:, 28:32], in_=sm[:, 28:32], func=AF.Ln)
                    OP("act", "activation", [Tsm], [Tsm], out=sm[:, 24:28], in_=sm[:, 28:32], func=AF.Exp, scale=-0.5)
                    OP("dve", "tensor_tensor", [Toc, Tsm], [Ton], out=on[:], in0=oc[:], in1=sm[:, 24:28].unsqueeze(2).to_broadcast([128, 4, 128]), op=ALU.mult)
                    pending.append((qt, kvh))
            emit_tr(*pending.pop())
            if stop_after == 3:
                dump("mixa", mixT[:, 0:8, :], TmixT, [128, 8, NQ], BF16)
            P.barrier()
        sA.close()
        if stop_after == 3:
            finish()
            return nc, dbg_outs
        h2, Th2 = sb(st, [128, 8, D], F32, "h2")
        for tile in range(8):
            DMA("sp", h2[:, tile, :], xs[3072 + tile * 128:3072 + (tile + 1) * 128, :], [], [Th2])
        with ExitStack() as s4:
            wos = [sb(s4, [128, 16, 512], F32, "wos") for _ in range(2)]
            wob = [sb(s4, [128, 16, 512], BF16, "wob") for _ in range(2)]
            pM4 = [psb(s4, [128, 512], F32, "pM4") for _ in range(4)]
            m4 = [0]
            for cc in range(4):
                stg, Tstg = wos[cc % 2]
                wb, Twb = wob[cc % 2]
                cs = slice(cc * 512, (cc + 1) * 512)
                DMA("sp", stg[:], w_out[:, cs].rearrange("(k p) c -> p k c", p=128), [], [Tstg])
                for hh in range(2):
                    OP("pool" if hh == 0 else "act", "tensor_copy" if hh == 0 else "activation", [Tstg], [Twb],
                       **(dict(out=wb[:, 8 * hh:8 * hh + 8, :], in_=stg[:, 8 * hh:8 * hh + 8, :]) if hh == 0 else
                          dict(out=wb[:, 8 * hh:8 * hh + 8, :], in_=stg[:, 8 * hh:8 * hh + 8, :], func=AF.Copy)))
                for tile in range(8):
                    m4[0] += 1
                    pm, Tpm = pM4[m4[0] % 4]
                    for kc in range(16):
                        OP("pe", "matmul", [TmixT, Twb], [Tpm], out=pm[:], lhsT=mixT[:, kc, tile * 128:(tile + 1) * 128], rhs=wb[:, kc, :],
                           start=(kc == 0), stop=(kc == 15))
                    OP("dve", "tensor_tensor", [Tpm, Th2], [Th2], out=h2[:, tile, cs], in0=pm[:], in1=h2[:, tile, cs], op=ALU.add)
            if stop_after == 4:
                dump("h2", h2[:], Th2, [128, 8, D], F32)
            P.barrier()
        if stop_after == 4:
            finish()
            return nc, dbg_outs

        xn2, Txn2 = sb(st, [128, 8, D], BF16, "xn2")
        eidx, Teidx = sb(st, [128, 8, 128], U32, "eidx")
        gates, Tgates = sb(st, [128, 8, 128], F32, "gates")
        iota16, Tiota = sb(st, [128, 16], F32, "iota16")
        DMA("sp", iota16[:], iota16_d, [], [Tiota])
        xn2T, Txn2T = mixT, T("xn2T")
        with ExitStack() as s4b:
            eqB, TeqB = sb(s4b, [128, 8, 16, 16], F32, "eqB")
            g2b, Tg2b = eqB[:].rearrange("p t a b -> p (t a b)"), TeqB
            sq4, Tsq4 = sb(s4b, [128, 2], F32, "sq4")
            SKT, TSKT = sb(s4b, [128, 16, 128], BF16, "SKT")
            wqs = [sb(s4b, [128, 16, 128], F32, "wqs")] * 2
            wqb = [sb(s4b, [128, 16, 128], BF16, "wqb") for _ in range(2)]
            skst, Tskst = wqs[0]
            skb, Tskb = wqb[0]
            qpT = [sb(s4b, [128, NQ], BF16, "qpT") for _ in range(2)]
            sw = [sb(s4b, [128, 128], F32, "sw") for _ in range(2)]
            sw2, Tsw2 = sb(s4b, [128, 128], F32, "sw2")
            sv, Tsv = sb(s4b, [128, 8, 16, 16], F32, "sv")
            si, Tsi = sb(s4b, [128, 8, 16, 16], U32, "si")
            pT4 = [psb(s4b, [128, 8, 128], BF16, "pT4") for _ in range(2)]
            pM5 = [psb(s4b, [128, 512], F32, "pM5") for _ in range(4)]
            pS5 = [psb(s4b, [128, 512], F32, "pS5") for _ in range(2)]
            DMA("sp", g2b, ffn_norm_w.partition_broadcast(128), [], [Tg2b], **NCD)
            for tile in range(8):
                OP("act", "activation", [Th2], [Txn2, Tsq4], out=xn2[:, tile, :], in_=h2[:, tile, :], func=AF.Square, accum_out=sq4[:, 0:1])
                rstd_from_sumsq(sq4, Tsq4, D)
                OP("dve", "scalar_tensor_tensor", [Th2, Tsq4, Tg2b], [Txn2], out=xn2[:, tile, :], in0=h2[:, tile, :], scalar=sq4[:, 0:1], in1=g2b,
                   op0=ALU.mult, op1=ALU.mult)
                for half in range(2):
                    pt, Tpt = pT4[half]
                    for k8 in range(8):
                        kc = half * 8 + k8
                        OP("pe", "transpose", [Txn2, Tidentb], [Tpt], out=pt[:, k8, :], in_=xn2[:, tile, kc * 128:(kc + 1) * 128], identity=identb[:])
                    evac(xn2T[:, half * 8:(half + 1) * 8, tile * 128:(tile + 1) * 128], pt[:], [Tpt], [Txn2T])
            DMA("sp", skst[:], peer_sk.rearrange("h n d -> n h d"), [], [Tskst])
            OP("pool", "tensor_copy", [Tskst], [Tskb], out=skb[:], in_=skst[:])
            for half in range(2):
                pt, Tpt = pT4[half]
                for k8 in range(8):
                    OP("pe", "transpose", [Tskb, Tidentb], [Tpt], out=pt[:, k8, :], in_=skb[:, half * 8 + k8, :], identity=identb[:])
                evac(SKT[:, half * 8:(half + 1) * 8, :], pt[:], [Tpt], [TSKT])
            candB, TcandB = sb(s4b, [128, 8, 256], F32, "candB")
            cand2B, Tcand2B = sb(s4b, [128, 256], F32, "cand2B")
            cvB, TcvB = sb(s4b, [128, 8, 16], F32, "cvB")
            ciB, TciB = sb(s4b, [128, 8, 16], U32, "ciB")
            pabB, TpabB = sb(s4b, [128, 2, 8, 16], U32, "pabB")
            rf, Trf = sb(s4b, [128, 8, 8, 16], F32, "rf")
            rsm, Trsm = sb(s4b, [128, 2, 8], F32, "rsm")
            B4 = [128, 8, 16, 16]

            def route_h(h):
                c1, c2 = 2 * h, 2 * h + 1
                OP("dve", "tensor_tensor", [Tsv], [TcandB], out=candB[:].rearrange("p t (a b) -> p t a b", a=16),
                   in0=sv[:, :, c1, :].unsqueeze(3).to_broadcast(B4), in1=sv[:, :, c2, :].unsqueeze(2).to_broadcast(B4), op=ALU.add)
                for t in range(8):
                    OP("dve", "max", [TcandB], [TcvB], out=cvB[:, t, 0:8], in_=candB[:, t, :])
                    OP("dve", "max_index", [TcandB, TcvB], [TciB], out=ciB[:, t, 0:8], in_max=cvB[:, t, 0:8], in_values=candB[:, t, :])
                    OP("dve", "match_replace", [TcandB, TcvB], [Tcand2B], out=cand2B[:], in_to_replace=cvB[:, t, 0:8], in_values=candB[:, t, :], imm_value=-1e30)
                    OP("dve", "max", [Tcand2B], [TcvB], out=cvB[:, t, 8:16], in_=cand2B[:])
                    OP("dve", "max_index", [Tcand2B, TcvB], [TciB], out=ciB[:, t, 8:16], in_max=cvB[:, t, 8:16], in_values=cand2B[:])
                OP("dve", "tensor_single_scalar", [TciB], [TpabB], out=pabB[:, 0], in_=ciB[:], scalar=4, op=ALU.logical_shift_right)
                OP("dve", "tensor_single_scalar", [TciB], [TpabB], out=pabB[:, 1], in_=ciB[:], scalar=15, op=ALU.bitwise_and)
                OP("dve", "tensor_copy", [TpabB], [Trf], out=rf[:, 0:2], in_=pabB[:])
                OP("dve", "tensor_copy", [Tsi], [Trf], out=rf[:, 2], in_=si[:, :, c1, :])
                OP("dve", "tensor_copy", [Tsi], [Trf], out=rf[:, 3], in_=si[:, :, c2, :])
                for (src, sif, dst) in ((0, 2, 4), (1, 3, 5)):
                    OP("dve", "tensor_tensor", [Trf, Tiota], [TeqB], out=eqB[:], in0=rf[:, src].unsqueeze(3).to_broadcast(B4),
                       in1=iota16[:].unsqueeze(1).unsqueeze(1).to_broadcast(B4), op=ALU.is_equal)
                    OP("dve", "tensor_tensor", [TeqB, Trf], [TeqB], out=eqB[:], in0=eqB[:], in1=rf[:, sif].unsqueeze(2).to_broadcast(B4), op=ALU.mult)
                    OP("dve", "tensor_reduce", [TeqB], [Trf], out=rf[:, dst], in_=eqB[:], axis=AX.X, op=ALU.add)
                OP("dve", "scalar_tensor_tensor", [Trf], [Trf], out=rf[:, 6], in0=rf[:, 4], scalar=128.0, in1=rf[:, 5], op0=ALU.mult, op1=ALU.add)
                OP("dve", "tensor_scalar", [Trf], [Trf], out=rf[:, 6], in0=rf[:, 6], scalar1=0.0, scalar2=16383.0, op0=ALU.max, op1=ALU.min)
                OP("dve", "tensor_copy", [Trf], [Teidx], out=eidx[:, :, 16 * h:16 * h + 16], in_=rf[:, 6])
                OP("dve", "tensor_tensor", [TcvB], [Trf], out=rf[:, 7], in0=cvB[:], in1=cvB[:, :, 0:1].to_broadcast([128, 8, 16]), op=ALU.subtract)
                OP("act", "activation", [Trf], [Trf], out=rf[:, 7], in_=rf[:, 7], func=AF.Exp)
                OP("dve", "tensor_reduce", [Trf], [Trsm], out=rsm[:, 0], in_=rf[:, 7], axis=AX.X, op=ALU.add)
                OP("dve", "reciprocal", [Trsm], [Trsm], out=rsm[:, 1], in_=rsm[:, 0])
                OP("dve", "tensor_tensor", [Trf, Trsm], [Tgates], out=gates[:, :, 16 * h:16 * h + 16], in0=rf[:, 7],
                   in1=rsm[:, 1].unsqueeze(2).to_broadcast([128, 8, 16]), op=ALU.mult)

            m5 = [0]
            for hc in range(16):
                stg, Tstg = wqs[hc % 2]
                wb, Twb = wqb[hc % 2]
                qp, Tqp = qpT[hc % 2]
                DMA("sp", stg[:], peer_wq[:, hc * 128:(hc + 1) * 128].rearrange("(k p) c -> p k c", p=128), [], [Tstg])
                OP("pool", "tensor_copy", [Tstg], [Twb], out=wb[:], in_=stg[:])
                for half in range(2):
                    m5[0] += 1
                    pm, Tpm = pM5[m5[0] % 4]
                    for kc in range(16):
                        OP("pe", "matmul", [Twb, Txn2T], [Tpm], out=pm[:], lhsT=wb[:, kc, :], rhs=xn2T[:, kc, half * 512:(half + 1) * 512],
                           start=(kc == 0), stop=(kc == 15))
                    evac(qp[:, half * 512:(half + 1) * 512], pm[:], [Tpm], [Tqp])
                for tile in range(8):
                    ps, Tps = pS5[tile % 2]
                    swt, Tswt = sw[tile % 2]
                    OP("pe", "matmul", [Tqp, TSKT], [Tps], out=ps[:, 0:128], lhsT=qp[:, tile * 128:(tile + 1) * 128], rhs=SKT[:, hc, :], start=True, stop=True)
                    OP("act", "activation", [Tps], [Tswt], out=swt[:], in_=ps[:, 0:128], func=AF.Copy)
                    OP("dve", "max", [Tswt], [Tsv], out=sv[:, tile, hc, 0:8], in_=swt[:])
                    OP("dve", "max_index", [Tswt, Tsv], [Tsi], out=si[:, tile, hc, 0:8], in_max=sv[:, tile, hc, 0:8], in_values=swt[:])
                    OP("dve", "match_replace", [Tswt, Tsv], [Tsw2], out=sw2[:], in_to_replace=sv[:, tile, hc, 0:8], in_values=swt[:], imm_value=-1e30)
                    OP("dve", "max", [Tsw2], [Tsv], out=sv[:, tile, hc, 8:16], in_=sw2[:])
                    OP("dve", "max_index", [Tsw2, Tsv], [Tsi], out=si[:, tile, hc, 8:16], in_max=sv[:, tile, hc, 8:16], in_values=sw2[:])
                if hc % 2 == 1:
                    route_h(hc // 2)
            if stop_after == 5:
                dump("eidx", eidx[:], Teidx, [128, 8, 128], U32)
                dump("gates", gates[:], Tgates, [128, 8, 128], F32)
            P.barrier()
        if stop_after == 5:
            finish()
            return nc, dbg_outs

        with ExitStack() as s5:
            gfin, Tgfin = sb(s5, [128, D], F32, "gfin")
            mixG = mixT[:].rearrange("p a b -> p (a b)").rearrange("p (k c) -> p k c", k=4)
            gb = [(mixG[:, i, :], T("gbm%d" % i)) for i in range(4)]
            gb += [(lambda t: (t[0][:], t[1]))(sb(s5, [128, 2 * D], BF16, "gb")) for _ in range(4)]
            acc, Tacc = sb(s5, [128, D], F32, "acc")
            junk5, Tjunk5 = sb(s5, [128, D], BF16, "junk5")
            aw2 = [sb(s5, [128, 128], F32, "aw") for _ in range(2)]
            ww2 = [sb(s5, [128, 128], F32, "ww") for _ in range(2)]
            t12 = [sb(s5, [128, 2], F32, "t1") for _ in range(2)]
            sq5, Tsq5 = sb(s5, [128, 2], F32, "sq5")
            dg = [sb(s5, [128, 128], BF16, "dg") for _ in range(8)]
            pV, TpV = psb(s5, [128, D], F32, "pV")
            DMA("sp", gfin[:], final_norm_w.partition_broadcast(128), [], [Tgfin], **NCD)
            GS = 2
            NG = 128 // GS
            gi = [0]
            bufs = {}

            def dots(tile, grp):
                a_, Ta_ = aw2[tile % 2]
                lst = []
                for j in range(GS):
                    slot = grp * GS + j
                    gi[0] += 1
                    g_, Tg_ = gb[gi[0] % 8]
                    P.dma("pool", dict(out=g_, out_offset=None, in_=uv16,
                                       in_offset=bass.IndirectOffsetOnAxis(ap=eidx[:, tile, slot:slot + 1], axis=0)),
                          reads=[Teidx, Tuv16], writes=[Tg_], name="indirect_dma_start")
                    OP("dve", "scalar_tensor_tensor", [Tg_, Txn2], [Tjunk5, Ta_], out=junk5[:], in0=g_[:, 0:D], scalar=1.0, in1=xn2[:, tile, :],
                       op0=ALU.mult, op1=ALU.mult, accum_out=a_[:, slot:slot + 1])
                    lst.append((g_, Tg_))
                bufs[(tile, grp)] = lst

            def gelu_pre(tile, grp):
                a_, Ta_ = aw2[tile % 2]
                t1, Tt1 = t12[grp % 2]
                av = a_[:, grp * GS:(grp + 1) * GS]
                OP("dve", "tensor_tensor", [Ta_], [Tt1], out=t1[:], in0=av, in1=av, op=ALU.mult)
                OP("dve", "tensor_tensor", [Tt1, Ta_], [Tt1], out=t1[:], in0=t1[:], in1=av, op=ALU.mult)
                OP("dve", "scalar_tensor_tensor", [Tt1, Ta_], [Tt1], out=t1[:], in0=t1[:], scalar=0.044715, in1=av, op0=ALU.mult, op1=ALU.add)
                OP("act", "activation", [Tt1], [Tt1], out=t1[:], in_=t1[:], func=AF.Tanh, scale=0.7978845608028654)

            def post_v(tile, grp):
                a_, Ta_ = aw2[tile % 2]
                w_, Tw_ = ww2[tile % 2]
                t1, Tt1 = t12[grp % 2]
                sl = slice(grp * GS, (grp + 1) * GS)
                OP("dve", "tensor_scalar", [Tt1], [Tt1], out=t1[:], in0=t1[:], scalar1=0.5, scalar2=0.5, op0=ALU.mult, op1=ALU.add)
                OP("dve", "tensor_tensor", [Tt1, Ta_], [Tt1], out=t1[:], in0=t1[:], in1=a_[:, sl], op=ALU.mult)
                OP("dve", "tensor_tensor", [Tt1, Tgates], [Tw_], out=w_[:, sl], in0=t1[:], in1=gates[:, tile, sl], op=ALU.mult)
                for j, (g_, Tg_) in enumerate(bufs.pop((tile, grp))):
                    slot = grp * GS + j
                    dk, Tdk = dg[slot % 8]
                    OP("act", "activation", [Tident, Tw_], [Tdk], out=dk[:], in_=ident[:], func=AF.Copy, scale=w_[:, slot:slot + 1])
                    for bq in range(4):
                        OP("pe", "matmul", [Tdk, Tg_], [TpV], out=pV[:, bq * 512:(bq + 1) * 512], lhsT=dk[:], rhs=g_[:, D + bq * 512:D + (bq + 1) * 512],
                           start=(slot == 0), stop=(slot == 127))

            def final(tile):
                OP("dve", "tensor_tensor", [TpV, Th2], [Tacc], out=acc[:], in0=pV[:], in1=h2[:, tile, :], op=ALU.add)
                OP("act", "activation", [Tacc], [Tjunk5, Tsq5], out=junk5[:], in_=acc[:], func=AF.Square, accum_out=sq5[:, 0:1])
                rstd_from_sumsq(sq5, Tsq5, D)
                OP("dve", "scalar_tensor_tensor", [Tacc, Tsq5, Tgfin], [Tacc], out=acc[:], in0=acc[:], scalar=sq5[:, 0:1], in1=gfin[:], op0=ALU.mult, op1=ALU.mult)
                DMA("sp", y_out[tile * 128:(tile + 1) * 128, :], acc[:], [Tacc], [Tout])

            prev = None
            for tile in range(8):
                for grp in range(NG):
                    dots(tile, grp)
                    gelu_pre(tile, grp)
                    if prev is not None:
                        post_v(*prev)
                        if prev[1] == NG - 1:
                            final(prev[0])
                    prev = (tile, grp)
            post_v(*prev)
            final(prev[0])
        finish()
        return nc, dbg_outs
        raise NotImplementedError
    return nc, dbg_outs


def _prep_inputs(inputs):
    x = np.asarray(inputs["x"], np.float32)
    cst = _consts()
    shared = {
        "attn_norm_w": np.ascontiguousarray(inputs["attn_norm_w"][0]),
        "w_in": np.ascontiguousarray(inputs["w_in"][0]),
        "w_cmp_k": np.ascontiguousarray(inputs["w_cmp_k"][0]),
        "w_cmp_v": np.ascontiguousarray(inputs["w_cmp_v"][0]),
        "cmp_pos": np.ascontiguousarray(inputs["cmp_pos"][0]),
        "conv_w": np.ascontiguousarray(inputs["conv_w"][0]),
        "conv_b": np.ascontiguousarray(inputs["conv_b"][0]),
        "attn_group_norm_w": np.ascontiguousarray(inputs["attn_group_norm_w"][0]),
        "conv_group_norm_w": np.ascontiguousarray(inputs["conv_group_norm_w"][0]),
        "w_out": np.ascontiguousarray(inputs["w_out"][0]),
        "rel_bias": np.ascontiguousarray(inputs["rel_bias"]),
        "ffn_norm_w": np.ascontiguousarray(inputs["ffn_norm_w"][0]),
        "peer_wq": np.ascontiguousarray(inputs["peer_wq"][0]),
        "peer_subkeys": np.ascontiguousarray(inputs["peer_subkeys"][0]).reshape(16, 128, 128),
        "peer_u": np.ascontiguousarray(inputs["peer_u"][0]),
        "peer_v": np.ascontiguousarray(inputs["peer_v"][0]),
        "final_norm_w": np.ascontiguousarray(inputs["final_norm_w"]),
    }
    shared = {k: np.asarray(v, np.float32) for k, v in shared.items()}
    shared.update(cst)
    in_maps = []
    for core in range(8):
        b, c = core // 4, core % 4
        q0 = 1024 * c
        ws = q0 - 3072
        xw = np.zeros((S, D), np.float32)
        lo = max(ws, 0)
        xw[lo - ws:, :] = x[b, lo:q0 + 1024, :]
        m = dict(shared)
        m["xs"] = xw
        m.update(_core_consts(c))
        in_maps.append(m)
    return in_maps


def kernel(**inputs):
    in_maps = _prep_inputs(inputs)
    nc, _ = build()
    res = run_bass_kernel_spmd(nc, in_maps, core_ids=list(range(8)))
    out = np.zeros((2, S, D), np.float32)
    for core in range(8):
        b, c = core // 4, core % 4
        out[b, 1024 * c:1024 * (c + 1), :] = res.results[core]["y"]
    return out
```
